# Optimizing a Trainium2 kernel written in Bass

```python
import math
import jax
import jax.numpy as jnp
from jax import lax
import numpy as np

D_MODEL = 1024
BATCH = 16
SEQ = 256
DEPTH = 4
DEC_BATCH = 2
DEC_SEQ = 2048
PAST_LEN = 512

GRID_W = 64
MIX_WIDTH = D_MODEL
MLSTM_WIDTH = MIX_WIDTH // 2
MLSTM_HEADS = 4
MLSTM_HEAD_DIM = MLSTM_WIDTH // MLSTM_HEADS
FOURIER_WIDTH = MIX_WIDTH - MLSTM_WIDTH
FOURIER_GROUPS = 4
FOURIER_GROUP_DIM = FOURIER_WIDTH // FOURIER_GROUPS
N_GATES = 4 * MLSTM_HEADS
Q_OFF = 0
K_OFF = MLSTM_WIDTH
V_OFF = 2 * MLSTM_WIDTH
O_OFF = 3 * MLSTM_WIDTH
G_OFF = 4 * MLSTM_WIDTH
F_OFF = G_OFF + N_GATES
IN_COLS = F_OFF + FOURIER_WIDTH
CHUNK = 128
N_EXPERTS = 32
TOP_K = 4
D_FF = D_MODEL
SWIGLU_LIMIT = 7.0
SWIGLU_ALPHA = 1.702
MOE_BLOCK = 128
DEEPNORM_ALPHA = (2 * DEPTH) ** 0.25
DEEPNORM_BETA = (8 * DEPTH) ** -0.25
LN_EPS = 1e-5

kernel_name = "hymba_mlstm_fnet_moe_deepnorm_diffusion_step"

F32 = jnp.float32


def layer_norm(x, w, b):
    xf = x.astype(F32)
    mu = jnp.mean(xf, axis=-1, keepdims=True)
    var = jnp.mean(jnp.square(xf - mu), axis=-1, keepdims=True)
    return ((xf - mu) * lax.rsqrt(var + LN_EPS) * w.astype(F32) + b.astype(F32)).astype(x.dtype)


def grid_pos_embed(rows, d):
    quarter = d // 4
    omega = 1.0 / (10000.0 ** (jnp.arange(quarter, dtype=F32) / quarter))
    r = jnp.repeat(jnp.arange(rows, dtype=F32), GRID_W)[:, None] * omega
    cc = jnp.tile(jnp.arange(GRID_W, dtype=F32), rows)[:, None] * omega
    return jnp.concatenate([jnp.sin(r), jnp.cos(r), jnp.sin(cc), jnp.cos(cc)], axis=-1)


def mlstm_scan(q, k, v, log_i, log_f, C0, n0, m0):
    B, H, N, DH = q.shape
    nc = N // CHUNK

    def to_chunks(a):
        return jnp.moveaxis(a.reshape(B, H, nc, CHUNK, *a.shape[3:]), 2, 0)

    causal = jnp.tril(jnp.ones((CHUNK, CHUNK), dtype=bool))

    def step(carry, xs):
        C, n, m = carry
        qc, kc, vc, lic, lfc = xs
        b = jnp.cumsum(lfc, axis=-1)
        dmat = jnp.where(causal, b[..., :, None] - b[..., None, :] + lic[..., None, :], -jnp.inf)
        inter = b + m[..., None]
        m_t = jnp.maximum(jnp.max(dmat, axis=-1), inter)
        w = jnp.exp(dmat - m_t[..., None])
        s = jnp.einsum('bhtd,bhsd->bhts', qc, kc) * w
        a = jnp.exp(inter - m_t)
        num = jnp.einsum('bhts,bhse->bhte', s, vc) + a[..., None] * jnp.einsum('bhtd,bhde->bhte', qc, C)
        den = jnp.sum(s, axis=-1) + a * jnp.einsum('bhtd,bhd->bht', qc, n)
        h = num / jnp.maximum(jnp.abs(den), jnp.exp(-m_t))[..., None]
        b_last = b[..., -1]
        log_w_end = b_last[..., None] - b + lic
        m_new = jnp.maximum(b_last + m, jnp.max(log_w_end, axis=-1))
        we = jnp.exp(log_w_end - m_new[..., None])
        decay = jnp.exp(b_last + m - m_new)
        C_new = decay[..., None, None] * C + jnp.einsum('bhs,bhsd,bhse->bhde', we, kc, vc)
        n_new = decay[..., None] * n + jnp.einsum('bhs,bhsd->bhd', we, kc)
        return (C_new, n_new, m_new), h

    xs = (to_chunks(q), to_chunks(k), to_chunks(v), to_chunks(log_i), to_chunks(log_f))
    (C, n, m), hs = lax.scan(step, (C0.astype(F32), n0.astype(F32), m0.astype(F32)), xs)
    h = jnp.moveaxis(hs, 0, 2).reshape(B, H, N, DH)
    return h, (C, n, m)


def fourier_mix(z):
    B, N, _ = z.shape
    zg = z.astype(F32).reshape(B, N, FOURIER_GROUPS, FOURIER_GROUP_DIM)
    out = jnp.fft.fft2(zg, axes=(1, 3), norm="ortho").real
    return out.reshape(B, N, FOURIER_WIDTH).astype(z.dtype)


def moe_ffn(u, router_w, router_b, w_gu, b_gu, w_down, b_down):
    shp = u.shape
    xt = u.reshape(-1, shp[-1])
    T = xt.shape[0]
    logits = xt.astype(F32) @ router_w.astype(F32) + router_b.astype(F32)
    top_val, top_idx = lax.top_k(logits, TOP_K)
    gates = jax.nn.softmax(top_val, axis=-1)
    TK = T * TOP_K
    flat_e = top_idx.reshape(TK)
    flat_tok = jnp.arange(TK, dtype=jnp.int32) // TOP_K
    flat_g = gates.reshape(TK)
    order = jnp.argsort(flat_e)
    sorted_e = flat_e[order]
    counts = jnp.bincount(flat_e, length=N_EXPERTS)
    padded = ((counts + MOE_BLOCK - 1) // MOE_BLOCK) * MOE_BLOCK
    start = jnp.cumsum(counts) - counts
    pad_end = jnp.cumsum(padded)
    pad_start = pad_end - padded
    dest = pad_start[sorted_e] + jnp.arange(TK, dtype=jnp.int32) - start[sorted_e]
    n_blocks = -(-TK // MOE_BLOCK) + N_EXPERTS
    P = n_blocks * MOE_BLOCK
    row_tok = jnp.full((P,), T, dtype=jnp.int32).at[dest].set(flat_tok[order])
    row_g = jnp.zeros((P,), F32).at[dest].set(flat_g[order])
    block_e = jnp.minimum(
        jnp.searchsorted(pad_end, jnp.arange(n_blocks, dtype=pad_end.dtype) * MOE_BLOCK, side='right'),
        N_EXPERTS - 1)
    x_pad = jnp.concatenate([xt, jnp.zeros((1, shp[-1]), xt.dtype)], axis=0)
    xb = x_pad[row_tok].reshape(n_blocks, MOE_BLOCK, shp[-1])

    def expert_block(args):
        xblk, e = args
        h = xblk @ w_gu[e] + b_gu[e]
        x_glu, x_lin = jnp.split(h, 2, axis=-1)
        x_glu = jnp.minimum(x_glu, SWIGLU_LIMIT)
        x_lin = jnp.clip(x_lin, -SWIGLU_LIMIT, SWIGLU_LIMIT)
        act = x_glu * jax.nn.sigmoid(SWIGLU_ALPHA * x_glu) * (x_lin + 1.0)
        return act @ w_down[e] + b_down[e]

    yb = lax.map(expert_block, (xb, block_e)).reshape(P, shp[-1])
    y = jax.ops.segment_sum(yb * row_g[:, None].astype(yb.dtype), row_tok, num_segments=T + 1)[:T]
    return y.reshape(shp).astype(u.dtype)


def trunk_layer(x, cond, C0, n0, m0, w_ada, b_ada, w_in, gate_bias, mlstm_norm_w, w_out,
                ln1_w, ln1_b, router_w, router_b, w_gu, b_gu, w_down, b_down, ln2_w, ln2_b):
    B, N, _ = x.shape
    mod = (jax.nn.silu(cond) @ w_ada + b_ada)[:, None, :]
    shift1, scale1, gate1, shift2, scale2, gate2 = jnp.split(mod, 6, axis=-1)

    u = x * (1.0 + scale1) + shift1
    p = u @ w_in

    def heads(a):
        return a.reshape(B, N, MLSTM_HEADS, MLSTM_HEAD_DIM).transpose(0, 2, 1, 3).astype(F32)

    q = heads(p[..., Q_OFF:K_OFF])
    k = heads(p[..., K_OFF:V_OFF]) * (MLSTM_HEAD_DIM ** -0.5)
    v = heads(p[..., V_OFF:O_OFF])
    o_gate = jax.nn.sigmoid(p[..., O_OFF:G_OFF])
    g = (p[..., G_OFF:F_OFF] + gate_bias).astype(F32).transpose(0, 2, 1)
    i_fw, f_fw, i_bw, f_bw = jnp.split(g, 4, axis=1)
    h_fw, st_fw = mlstm_scan(q, k, v, i_fw, jax.nn.log_sigmoid(f_fw), C0[:, 0], n0[:, 0], m0[:, 0])
    flip = lambda a: jnp.flip(a, axis=2)
    h_bw, st_bw = mlstm_scan(flip(q), flip(k), flip(v), flip(i_bw), jax.nn.log_sigmoid(flip(f_bw)),
                             C0[:, 1], n0[:, 1], m0[:, 1])
    h = (h_fw + flip(h_bw)).transpose(0, 2, 1, 3)
    mu = jnp.mean(h, axis=-1, keepdims=True)
    var = jnp.mean(jnp.square(h - mu), axis=-1, keepdims=True)
    h = (h - mu) * lax.rsqrt(var + LN_EPS) * mlstm_norm_w.astype(F32).reshape(MLSTM_HEADS, MLSTM_HEAD_DIM)
    h_m = h.reshape(B, N, MLSTM_WIDTH).astype(x.dtype) * o_gate
    f_m = fourier_mix(p[..., F_OFF:])
    mix = jnp.concatenate([h_m, f_m], axis=-1) @ w_out
    x = layer_norm(DEEPNORM_ALPHA * x + gate1 * mix, ln1_w, ln1_b)

    u2 = x * (1.0 + scale2) + shift2
    y = moe_ffn(u2, router_w, router_b, w_gu, b_gu, w_down, b_down)
    x = layer_norm(DEEPNORM_ALPHA * x + gate2 * y, ln2_w, ln2_b)

    C_f = jnp.stack([st_fw[0], st_bw[0]], axis=1)
    n_f = jnp.stack([st_fw[1], st_bw[1]], axis=1)
    m_f = jnp.stack([st_fw[2], st_bw[2]], axis=1)
    return x, (C_f, n_f, m_f)


def setup_inputs(seed: int = 0) -> dict:
    key = jax.random.key(seed)
    ks = jax.random.split(key, 24)
    nrm = lambda k, s: jax.random.normal(k, s, F32)
    H, DH = MLSTM_HEADS, MLSTM_HEAD_DIM
    gate_bias = jnp.concatenate([
        0.1 * nrm(ks[0], (DEPTH, H)),
        3.0 + 0.5 * nrm(ks[1], (DEPTH, H)),
        0.1 * nrm(ks[2], (DEPTH, H)),
        3.0 + 0.5 * nrm(ks[3], (DEPTH, H)),
    ], axis=-1)
    return {
        "x_prompt": nrm(ks[4], (BATCH, SEQ, D_MODEL)),
        "x_sample": nrm(ks[5], (DEC_BATCH, DEC_SEQ, D_MODEL)),
        "state_C": 0.1 * nrm(ks[6], (DEC_BATCH, DEPTH, 2, H, DH, DH)),
        "state_n": 0.1 * nrm(ks[7], (DEC_BATCH, DEPTH, 2, H, DH)),
        "state_m": jax.random.uniform(ks[8], (DEC_BATCH, DEPTH, 2, H), F32, 0.0, 3.0),
        "c": nrm(ks[9], (DEC_BATCH, D_MODEL)),
        "c_ctx": nrm(ks[10], (D_MODEL,)),
        "w_ada": 0.5 * D_MODEL ** -0.5 * nrm(ks[11], (DEPTH, D_MODEL, 6 * D_MODEL)),
        "b_ada": 0.02 * nrm(ks[12], (DEPTH, 6 * D_MODEL)),
        "w_in": D_MODEL ** -0.5 * nrm(ks[13], (DEPTH, D_MODEL, IN_COLS)),
        "gate_bias": gate_bias,
        "mlstm_norm_w": 1.0 + 0.02 * nrm(ks[14], (DEPTH, MLSTM_WIDTH)),
        "w_out": DEEPNORM_BETA * MIX_WIDTH ** -0.5 * nrm(ks[15], (DEPTH, MIX_WIDTH, D_MODEL)),
        "ln1_w": 1.0 + 0.02 * nrm(ks[16], (DEPTH, D_MODEL)),
        "ln1_b": 0.02 * nrm(ks[17], (DEPTH, D_MODEL)),
        "router_w": D_MODEL ** -0.5 * nrm(ks[18], (DEPTH, D_MODEL, N_EXPERTS)),
        "router_b": 0.01 * nrm(ks[19], (DEPTH, N_EXPERTS)),
        "w_gate_up": D_MODEL ** -0.5 * nrm(ks[20], (DEPTH, N_EXPERTS, D_MODEL, 2 * D_FF)),
        "b_gate_up": 0.02 * nrm(ks[21], (DEPTH, N_EXPERTS, 2 * D_FF)),
        "w_down": DEEPNORM_BETA * D_FF ** -0.5 * nrm(ks[22], (DEPTH, N_EXPERTS, D_FF, D_MODEL)),
        "b_down": 0.02 * nrm(ks[23], (DEPTH, N_EXPERTS, D_MODEL)),
        "ln2_w": 1.0 + 0.02 * nrm(jax.random.fold_in(key, 101), (DEPTH, D_MODEL)),
        "ln2_b": 0.02 * nrm(jax.random.fold_in(key, 102), (DEPTH, D_MODEL)),
    }


def reference(x_prompt, x_sample, state_C, state_n, state_m, c, c_ctx, w_ada, b_ada, w_in,
              gate_bias, mlstm_norm_w, w_out, ln1_w, ln1_b, router_w, router_b, w_gate_up,
              b_gate_up, w_down, b_down, ln2_w, ln2_b):
    H, DH = MLSTM_HEADS, MLSTM_HEAD_DIM

    def layer_params(l):
        return (w_ada[l], b_ada[l], w_in[l], gate_bias[l], mlstm_norm_w[l], w_out[l], ln1_w[l],
                ln1_b[l], router_w[l], router_b[l], w_gate_up[l], b_gate_up[l], w_down[l],
                b_down[l], ln2_w[l], ln2_b[l])

    bp = x_prompt.shape[0]
    zC = jnp.zeros((bp, 2, H, DH, DH), F32)
    zn = jnp.zeros((bp, 2, H, DH), F32)
    zm = jnp.zeros((bp, 2, H), F32)
    cond_ctx = c_ctx[None, :]
    x = x_prompt
    Cs, ns, ms = [], [], []
    for l in range(DEPTH):
        x, (C_l, n_l, m_l) = trunk_layer(x, cond_ctx, zC, zn, zm, *layer_params(l))
        Cs.append(C_l)
        ns.append(n_l)
        ms.append(m_l)
    y_prompt = x
    new_state_C = jnp.stack(Cs, axis=1)
    new_state_n = jnp.stack(ns, axis=1)
    new_state_m = jnp.stack(ms, axis=1)

    rows = x_sample.shape[1] // GRID_W
    z = x_sample + grid_pos_embed(rows, D_MODEL).astype(x_sample.dtype)[None]
    for l in range(DEPTH):
        z, _ = trunk_layer(z, c, state_C[:, l], state_n[:, l], state_m[:, l], *layer_params(l))
    y_sample = z
    return (y_prompt, y_sample, new_state_C, new_state_n, new_state_m)
```

```python
from contextlib import ExitStack
import types
import numpy as np
import ml_dtypes
import concourse.bass as bass
import concourse.mybir as mybir
from concourse.bass_utils import run_bass_kernel_spmd

F32 = mybir.dt.float32
BF16 = mybir.dt.bfloat16
AF = mybir.ActivationFunctionType
ALU = mybir.AluOpType

D = 1024
DEPTH = 4
NE = 32
DH = 128
ALPHA = (2 * DEPTH) ** 0.25
LN_EPS = 1e-5
LIM = 7.0
SALPHA = 1.702
IN_COLS = 2576
G_OFF = 2048
F_OFF = 2064

COMPUTE = ("pe", "dve", "act", "pool")
ENGS = ("pe", "dve", "act", "pool", "sp")
EPOCH = 30000


class Tok:
    __slots__ = ("name", "last_w", "readers", "dma_total", "sem", "pending")

    def __init__(self, name):
        self.name = name
        self.last_w = None
        self.readers = []
        self.dma_total = 0
        self.sem = None
        self.pending = []


def _snapshot(fn):
    if getattr(fn, "__closure__", None) is None:
        return fn
    cells = []
    for c in fn.__closure__:
        try:
            cells.append(types.CellType(c.cell_contents))
        except ValueError:
            cells.append(c)
    return types.FunctionType(fn.__code__, fn.__globals__, fn.__name__, fn.__defaults__, tuple(cells))


class Op:
    __slots__ = ("eng", "fn", "waits", "signal", "sig_idx", "dma_key", "dma_val", "inc")

    def __init__(self, eng, fn):
        self.eng = eng
        self.fn = _snapshot(fn)
        self.waits = []
        self.signal = False
        self.sig_idx = None
        self.dma_key = None
        self.dma_val = None
        self.inc = 16


class Prog:
    def __init__(self, nc):
        self.nc = nc
        self.ops = {e: [] for e in ENGS}

    def tok(self, name="t"):
        return Tok(name)

    def toks(self, n, name="t"):
        return [Tok(name) for _ in range(n)]

    def alias(self, new, olds):
        for o in olds:
            if o.last_w is not None:
                new.pending.append(o.last_w)
            new.pending.extend(o.readers)

    def _add(self, eng, fn, reads, writes, dma_key=None, inc=16):
        o = Op(eng, fn)
        deps = []
        for t in reads:
            if t.last_w is not None:
                deps.append(t.last_w)
        for t in writes:
            if t.last_w is not None:
                deps.append(t.last_w)
            deps.extend(t.readers)
            if t.pending:
                deps.extend(t.pending)
                t.pending = []
        seen = set()
        for d in deps:
            if d is o or id(d) in seen:
                continue
            seen.add(id(d))
            if d.dma_key is not None:
                o.waits.append(("d", d.dma_key, d.dma_key.dma_total))
            else:
                if d.eng == "pe" and eng == "pe":
                    continue
                d.signal = True
                o.waits.append(("c", d.eng, d))
        for t in reads:
            t.readers.append(o)
        for t in writes:
            t.last_w = o
            t.readers = []
        if dma_key is not None:
            o.dma_key = dma_key
            o.inc = inc
            dma_key.dma_total += inc
            o.dma_val = dma_key.dma_total
        self.ops[eng].append(o)
        return o

    def op(self, eng, fn, reads=(), writes=()):
        return self._add(eng, fn, list(reads), list(writes))

    def dma(self, eng, fn, reads=(), writes=(), key=None, inc=16):
        if key is None:
            key = list(writes)[0]
        return self._add(eng, fn, list(reads), list(writes), dma_key=key, inc=inc)

    def emit(self, stack, final_waits=()):
        nc = self.nc
        nsig = {}
        for e in ENGS:
            k = 0
            for o in self.ops[e]:
                if o.dma_key is None and o.signal:
                    k += 1
                    o.sig_idx = k
            nsig[e] = k
        esems = {}
        for e in ENGS:
            n_ep = max(1, (nsig[e] + EPOCH - 1) // EPOCH)
            esems[e] = [stack.enter_context(nc.semaphore(f"s_{e}{i}")) for i in range(n_ep)]
        nk = 0
        for e in ENGS:
            for o in self.ops[e]:
                if o.dma_key is not None and o.dma_key.sem is None:
                    o.dma_key.sem = stack.enter_context(nc.semaphore(f"d_{nk}"))
                    nk += 1
        self.n_sems = sum(len(v) for v in esems.values()) + nk
        block = stack.enter_context(nc.Block())
        ops = self.ops

        def run(e, eng):
            waited_c = {}
            waited_d = {}
            for o in ops[e]:
                for w in o.waits:
                    if w[0] == "c":
                        d = w[2]
                        ep, v = divmod(d.sig_idx - 1, EPOCH)
                        v += 1
                        kk = (d.eng, ep)
                        if waited_c.get(kk, 0) >= v:
                            continue
                        if any(k2[0] == d.eng and k2[1] > ep for k2 in waited_c):
                            continue
                        waited_c[kk] = v
                        eng.wait_ge(esems[d.eng][ep], v)
                    else:
                        t, v = w[1], w[2]
                        if waited_d.get(id(t), 0) >= v:
                            continue
                        waited_d[id(t)] = v
                        eng.wait_ge(t.sem, v)
                ins = o.fn(eng)
                if o.dma_key is not None:
                    ins.then_inc(o.dma_key.sem, o.inc)
                elif o.signal:
                    ins.then_inc(esems[e][(o.sig_idx - 1) // EPOCH], 1)
            if e == "sp":
                for t in final_waits:
                    eng.wait_ge(t.sem, t.dma_total)

        @block.sync
        def _(eng):
            run("sp", eng)

        @block.tensor
        def _(eng):
            run("pe", eng)

        @block.vector
        def _(eng):
            run("dve", eng)

        @block.scalar
        def _(eng):
            run("act", eng)

        @block.gpsimd
        def _(eng):
            run("pool", eng)


def input_specs(DEPTH, NEX):
  return [
    ("xp", [512, D], F32), ("xs_own", [512, D], F32), ("pos_own", [512, D], F32),
    ("xs_full", [2048, D], F32), ("pos_full", [2048, D], F32),
    ("sC", [DEPTH, 8, 128, 128], F32), ("sn", [DEPTH, 128, 8], F32), ("smb", [DEPTH, 128, 8], F32),
    ("cvT", [128, 16], F32), ("maskf", [128, 16], F32), ("maskb", [128, 16], F32),
    ("w_ada", [DEPTH, D, 6 * D], F32), ("b_ada2", [DEPTH, 2, 6 * D], F32),
    ("w_in", [DEPTH, D, IN_COLS], F32), ("gbias", [DEPTH, 128, 16], F32), ("nwT", [DEPTH, 128, 4], F32),
    ("w_out", [DEPTH, D, D], F32), ("ln1w", [DEPTH, 128, D], F32), ("ln1b", [DEPTH, 128, D], F32),
    ("router_w", [DEPTH, D, NE], F32), ("rbb", [DEPTH, 128, NE], F32),
    ("w_gu", [DEPTH, NEX, D, 2 * D], F32), ("bguT", [DEPTH, 128, 16, NE], F32),
    ("w_down", [DEPTH, NEX, D, D], F32), ("b_down", [DEPTH, NE, D], F32),
    ("ln2w", [DEPTH, 128, D], F32), ("ln2b", [DEPTH, 128, D], F32),
    ("cn_own", [2048, 512], BF16), ("sn_own", [2048, 512], BF16),
    ("c256", [256, 256], BF16), ("s256", [256, 256], BF16),
    ("ccp", [128, 128], BF16), ("nscp", [128, 128], BF16), ("ccs", [128, 128], BF16), ("nscs", [128, 128], BF16),
    ("ident_b", [128, 128], BF16), ("ident_f", [128, 128], F32), ("triU", [128, 128], F32), ("triL", [128, 128], F32),
    ("selr", [2, 2, 128], F32), ("sel8", [8, 2], F32), ("NMU4", [128, 4, 128], F32), ("NML4", [128, 4, 128], F32),
  ]


def output_specs(DEPTH):
  return [
    ("yp", [512, D], F32), ("ys", [512, D], F32),
    ("nC", [2, DEPTH, 8, 128, 128], F32), ("nn", [2, DEPTH, 8, 128], F32), ("nm", [2, DEPTH, 8], F32),
  ]


def build_program(NL=DEPTH, NEX=NE, do_cc=True, stop_after=None):
    nc = bass.Bass("TRN2", target_bir_lowering=False)
    P = Prog(nc)
    DI = {n: nc.dram_tensor(n, list(s), d, kind="ExternalInput").ap() for n, s, d in input_specs(NL, max(NEX, 1))}
    DO = {n: nc.dram_tensor(n, list(s), d, kind="ExternalOutput").ap() for n, s, d in output_specs(NL)}
    if _DBG:
        DO["dbg_mix"] = nc.dram_tensor("dbg_mix", [128, 8, 512], BF16, kind="ExternalOutput").ap()
        DO["dbg_h1"] = nc.dram_tensor("dbg_h1", [128, 2, 512], F32, kind="ExternalOutput").ap()
        DO["dbg_h2"] = nc.dram_tensor("dbg_h2", [128, 2, 512], F32, kind="ExternalOutput").ap()
        DO["dbg_gt"] = nc.dram_tensor("dbg_gt", [128, 2, 48], F32, kind="ExternalOutput").ap()
        DO["dbg_stx"] = nc.dram_tensor("dbg_stx", [128, 2, 4, 128], BF16, kind="ExternalOutput").ap()
        DO["dbg_scr"] = nc.dram_tensor("dbg_scr", [128, 512], F32, kind="ExternalOutput").ap()
        DO["dbg_qk"] = nc.dram_tensor("dbg_qk", [128, 2, 4, 256], BF16, kind="ExternalOutput").ap()
        DO["dbg_nd"] = nc.dram_tensor("dbg_nd", [128, 2, 129], F32, kind="ExternalOutput").ap()
        DO["dbg_sm2"] = nc.dram_tensor("dbg_sm2", [128, 32], F32, kind="ExternalOutput").ap()
        DO["dbg_x1"] = nc.dram_tensor("dbg_x1", [128, 8, D], F32, kind="ExternalOutput").ap()
    gins = [nc.dram_tensor(f"gin{i}", [128, D], F32) for i in range(4)]
    gouts = [nc.dram_tensor(f"gout{i}", [512, D], F32) for i in range(4)]
    t_gin, t_gout = P.toks(4, "gin"), P.toks(4, "gout")
    out_toks = []

    with ExitStack() as st:
        def sb(name, shape, dt):
            return st.enter_context(nc.sbuf_tensor("sb_" + name, list(shape), dt))

        PS = [st.enter_context(nc.psum_tensor(f"ps{i}", [128, 512], F32)) for i in range(8)]
        tPS = P.toks(8, "ps")
        bank_ctr = [0]
        PLONG, t_PLONG = PS[7], tPS[7]

        def bank():
            i = bank_ctr[0] % 7
            bank_ctr[0] += 1
            return PS[i], tPS[i]

        consts = {}
        for n, shp, dt in [("ident_b", [128, 128], BF16), ("ident_f", [128, 128], F32), ("triU", [128, 128], F32),
                           ("triL", [128, 128], F32), ("ccp", [128, 128], BF16), ("nscp", [128, 128], BF16),
                           ("ccs", [128, 128], BF16), ("nscs", [128, 128], BF16), ("cvT", [128, 16], F32),
                           ("maskf", [128, 16], F32), ("maskb", [128, 16], F32), ("selr", [2, 2, 128], F32), ("sel8", [8, 2], F32), ("NMU4", [128, 4, 128], F32), ("NML4", [128, 4, 128], F32)]:
            t = sb("c_" + n, shp, dt)
            tk = P.tok(n)
            P.dma("sp", lambda e, t=t, n=n: e.dma_start(out=t[:], in_=DI[n]), writes=[tk])
            consts[n] = (t, tk)
        ident_b, t_idb = consts["ident_b"]
        ident_f, t_idf = consts["ident_f"]
        triU, t_triU = consts["triU"]
        triL, t_triL = consts["triL"]
        cvT, t_cvT = consts["cvT"]
        maskf, t_maskf = consts["maskf"]
        maskb, t_maskb = consts["maskb"]
        selr, t_selr = consts["selr"]
        sel8, t_sel8 = consts["sel8"]
        NMU4, t_NMU4 = consts["NMU4"]
        NML4, t_NML4 = consts["NML4"]
        c256 = sb("c256", [128, 2, 256], BF16)
        s256 = sb("s256", [128, 2, 256], BF16)
        t_c256, t_s256 = P.tok(), P.tok()
        P.dma("sp", lambda e: e.dma_start(out=c256[:], in_=DI["c256"].rearrange("(m p) n -> p m n", p=128)), writes=[t_c256])
        P.dma("sp", lambda e: e.dma_start(out=s256[:], in_=DI["s256"].rearrange("(m p) n -> p m n", p=128)), writes=[t_s256])
        ones_f = sb("ones_f", [128, 128], F32)
        ones_b = sb("ones_b", [128, 128], BF16)
        eps_t = sb("eps_t", [128, 1], F32)
        t_ones = P.tok()
        P.op("pool", lambda e: e.memset(ones_f[:], 1.0), writes=[t_ones])
        P.op("pool", lambda e: e.memset(ones_b[:], 1.0), writes=[t_ones])
        P.op("pool", lambda e: e.memset(eps_t[:], LN_EPS), writes=[t_ones])
        sT = sb("sT", [128, 16], F32)
        t_sT = P.tok()
        P.op("act", lambda e: e.activation(out=sT[:], in_=cvT[:], func=AF.Silu), reads=[t_cvT], writes=[t_sT])

        X = sb("X", [128, 8, D], F32)
        tX = P.toks(8, "X")
        for i in range(4):
            P.dma("sp", lambda e, i=i: e.dma_start(out=X[:, i, :], in_=DI["xp"][i * 128:(i + 1) * 128, :]), writes=[tX[i]])
        ptmp = sb("ptmp", [128, 2, D], F32)
        t_ptmp = P.toks(2)
        for i in range(4):
            P.dma("sp", lambda e, i=i: e.dma_start(out=X[:, 4 + i, :], in_=DI["xs_own"][i * 128:(i + 1) * 128, :]), writes=[tX[4 + i]])
            P.dma("sp", lambda e, i=i: e.dma_start(out=ptmp[:, i % 2, :], in_=DI["pos_own"][i * 128:(i + 1) * 128, :]), writes=[t_ptmp[i % 2]])
            P.op("pool", lambda e, i=i: e.tensor_tensor(out=X[:, 4 + i, :], in0=X[:, 4 + i, :], in1=ptmp[:, i % 2, :], op=ALU.add),
                 reads=[tX[4 + i], t_ptmp[i % 2]], writes=[tX[4 + i]])

        STG_N = 2
        stg = [sb(f"stg{i}", [128, 8, 256], F32) for i in range(STG_N)]
        t_stg = P.toks(STG_N, "stg")
        stg_ctr = [0]

        def stage_load(src_ap):
            i = stg_ctr[0] % STG_N
            stg_ctr[0] += 1
            P.dma("sp", lambda e: e.dma_start(out=stg[i][:], in_=src_ap.rearrange("(kc p) c -> p kc c", p=128)), writes=[t_stg[i]])
            return stg[i], t_stg[i]

        cast_ctr = [0]

        def cast_eng():
            cast_ctr[0] += 1
            return "dve" if cast_ctr[0] % 2 else "act"

        def do_copy(eng, out, in_, reads, writes):
            if eng == "act":
                P.op("act", lambda e: e.copy(out=out, in_=in_), reads=reads, writes=writes)
            else:
                P.op(eng, lambda e: e.tensor_copy(out=out, in_=in_), reads=reads, writes=writes)

        nwT = sb("nwT", [128, 4], F32)
        t_nwT = P.tok()
        gbias = sb("gbias", [128, 16], F32)
        t_gbias = P.tok()
        modT = sb("modT", [128, 6, 8, 2], F32)
        t_modT = P.tok("modT")
        gb = sb("gb", [128, 2, D], F32)
        t_gb = P.tok("gb")
        lnw = sb("lnw", [128, D], F32)
        lnb = sb("lnb", [128, D], F32)
        t_lnw, t_lnb = P.tok(), P.tok()
        rw = sb("rw", [128, 8, NE], F32)
        t_rw = P.tok()
        rbb = sb("rbb", [128, NE], F32)
        t_rbb = P.tok()
        bguT = sb("bguT", [128, 16, NE], F32)
        t_bguT = P.tok()
        bdn = sb("bdn", [NE, D], F32)
        t_bdn = P.tok()
        zscr = nc.dram_tensor("zscr", [2048, 512], BF16)
        t_zscr = P.tok("zscr")
        Sst = sb("Sst", [128, 8, 129], F32)
        t_S = P.toks(8, "S")
        Sbf = sb("Sbf", [128, 8, 129], BF16)
        t_Sbf = P.toks(8, "Sbf")
        smb = sb("smb", [128, 8], F32)
        t_smb = P.tok()
        arena = sb("arena", [128, 16384], BF16)
        uT = arena[:, 0:4096].rearrange("p (a b) -> p a b", a=8)
        t_uT = P.toks(4, "uT")
        qT = arena[:, 4096:6144].rearrange("p (a b) -> p a b", a=4)
        kT = arena[:, 6144:8192].rearrange("p (a b) -> p a b", a=4)
        t_qT, t_kT = P.tok("qT"), P.tok("kT")
        ktm = arena[:, 12288:14336].rearrange("p (a b) -> p a b", a=4)
        t_ktm = P.toks(4, "ktm")
        vext = sb("vext", [128, 4, 4, 129], BF16)
        t_vext = P.toks(4, "vext")
        vsc = sb("vsc", [128, 2, 4, 129], BF16)
        t_vsc = P.tok("vsc")
        og = arena[:, 14336:16384].rearrange("p (a b) -> p a b", a=4)
        t_og = P.toks(4, "og")
        zown = sb("zown", [128, 2, 512], BF16)
        t_zown = P.toks(2, "zown")
        zst, t_zst = zown, t_zown
        gt = sb("gt", [128, 4, 6, 8], F32)
        mst = sb("mst", [128, 8], F32)
        t_mst = P.tok("mst")
        scr = sb("scr", [128, 512], F32)
        t_scr = P.tok("scr")
        nd = sb("nd", [128, 2, 129], F32)
        t_nd = P.toks(2, "nd")
        t_gt = P.toks(4, "gt")
        STx = sb("STx", [128, 2, 4, 128], BF16)
        t_STx = P.toks(2, "STx")
        arena2 = sb("arena2", [128, 2048], F32)
        hacc = arena2[:, :].rearrange("p (a b) -> p a b", a=4)
        t_hacc = P.toks(4, "hacc")
        hmT = arena[:, 8192:12288].rearrange("p (a b) -> p a b", a=8)
        t_hmT = P.toks(4, "hmT")
        t_fmT = P.tok("fmT")
        small = sb("small", [128, 64], F32)
        t_small = P.tok("small")
        xt = ptmp
        t_xt = t_ptmp
        xtb = sb("xtb", [128, 1, D], BF16)
        t_xtb = P.toks(1, "xtb")
        dtab = sb("dtab", [128, 2, 2, 512], BF16)
        t_dtab = P.toks(2, "dtab")
        p12 = sb("p12", [128, 2, 2, 512], BF16)
        t_p12 = P.toks(2, "p12")

        u2T = sb("u2T", [128, 8, 1024], BF16)
        t_u2T = P.toks(8, "u2T")
        u2f = sb("u2f", [128, 8, 128], F32)
        t_u2f = P.tok("u2f")
        Gall = sb("Gall", [128, 8, NE], F32)
        t_G = P.toks(8, "G")
        arena3 = sb("arena3", [128, 1032], F32)
        GT = arena3[0:NE, 0:1024]
        t_GT = P.toks(8, "GT")
        WB_N = 2
        wb = [sb(f"wb{i}", [128, 8, 512], BF16) for i in range(WB_N)]
        t_wb = P.toks(WB_N, "wb")
        wb_ctr = [0]
        gsb = arena[:, 8192:12288].rearrange("p (a b) -> p a b", a=4)
        t_gs = P.toks(8, "gs")
        actT = arena[:, 0:8192].rearrange("p (a b) -> p a b", a=8)
        t_actT = [P.toks(2, "actT") for _ in range(8)]
        tmpA = arena2[:, 0:1024].rearrange("p (a b) -> p a b", a=2)
        t_tmpA = P.toks(2, "tmpA")
        tmpB = sb("tmpB", [128, 2, 512], BF16)
        t_tmpB = P.toks(2, "tmpB")
        tmpC = arena2[:, 1024:2048].rearrange("p (a b) -> p a b", a=2)
        t_tmpC = P.toks(2, "tmpC")

        SCALE_K = DH ** -0.5

        def load_cast(dst, t_dst, src2d, ncols, scale_ap=None, scale_rows=0):
            c0 = 0
            while c0 < ncols:
                w = min(256, ncols - c0)
                s_t, s_k = stage_load_w(src2d, c0, w)
                eng = cast_eng()
                if scale_ap is None:
                    do_copy(eng, dst[:, :, c0:c0 + w], s_t[:, :, 0:w], [s_k], [t_dst])
                else:
                    for kc in range(8):
                        if kc < scale_rows:
                            P.op("pool", lambda e, kc=kc, c0=c0, w=w, s_t=s_t: e.tensor_scalar(
                                out=dst[:, kc, c0:c0 + w], in0=s_t[:, kc, 0:w], scalar1=scale_ap[:, kc:kc + 1], scalar2=None,
                                op0=ALU.mult), reads=[s_k, t_nwT], writes=[t_dst])
                        else:
                            do_copy("pool", dst[:, kc, c0:c0 + w], s_t[:, kc, 0:w], [s_k], [t_dst])
                c0 += w

        def stage_load_w(src2d, c0, w):
            i = stg_ctr[0] % STG_N
            stg_ctr[0] += 1
            P.dma("sp", lambda e: e.dma_start(out=stg[i][:, :, 0:w], in_=src2d[:, c0:c0 + w].rearrange("(kc p) c -> p kc c", p=128)),
                  writes=[t_stg[i]])
            return stg[i], t_stg[i]

        BIG = 30000.0

        def gate_math(pg, t_pg, slot):
            G_ = gt[:, slot]
            tk = t_gt[slot]
            P.op("dve", lambda e: e.tensor_tensor(out=small[:, 0:16], in0=pg, in1=gbias[:], op=ALU.add),
                 reads=[t_pg, t_gbias], writes=[t_small])
            P.op("act", lambda e: e.activation(out=small[:, 16:20], in_=small[:, 4:8], func=AF.Exp, scale=-1.0), reads=[t_small], writes=[t_small])
            P.op("act", lambda e: e.activation(out=small[:, 20:24], in_=small[:, 12:16], func=AF.Exp, scale=-1.0), reads=[t_small], writes=[t_small])
            P.op("act", lambda e: e.activation(out=small[:, 24:32], in_=small[:, 16:24], func=AF.Ln, bias=1.0), reads=[t_small], writes=[t_small])
            P.op("dve", lambda e: e.tensor_scalar(out=small[:, 48:56], in0=small[:, 24:32], scalar1=-1.0, scalar2=None, op0=ALU.mult),
                 reads=[t_small], writes=[t_small])
            P.op("dve", lambda e: e.tensor_copy(out=small[:, 56:60], in_=small[:, 0:4]), reads=[t_small], writes=[t_small])
            P.op("dve", lambda e: e.tensor_copy(out=small[:, 60:64], in_=small[:, 8:12]), reads=[t_small], writes=[t_small])
            pb, tpb = bank()
            P.op("pe", lambda e: e.matmul(pb[:, 0:4], lhsT=triU[:], rhs=small[:, 48:52], start=True, stop=True), reads=[t_small, t_triU], writes=[tpb])
            P.op("pe", lambda e: e.matmul(pb[:, 4:8], lhsT=triL[:], rhs=small[:, 52:56], start=True, stop=True), reads=[t_small, t_triL], writes=[tpb])
            P.op("pe", lambda e: e.matmul(pb[:, 8:16], lhsT=ones_f[:], rhs=small[:, 48:56], start=True, stop=True), reads=[t_small, t_ones], writes=[tpb])
            P.op("dve", lambda e: e.tensor_tensor(out=G_[:, 0, :], in0=small[:, 56:64], in1=pb[:, 0:8], op=ALU.subtract), reads=[t_small, tpb], writes=[tk])
            P.op("act", lambda e: e.copy(out=G_[:, 1, :], in_=pb[:, 0:8]), reads=[tpb], writes=[tk])
            P.op("act", lambda e: e.copy(out=G_[:, 2, :], in_=pb[:, 8:16]), reads=[tpb], writes=[tk])
            for dr in range(2):
                for h in range(4):
                    P.op("dve", lambda e, dr=dr, h=h: e.tensor_scalar(out=hnb[:, h * 128:(h + 1) * 128], in0=ident_f[:], scalar1=G_[:, 0, dr * 4 + h:dr * 4 + h + 1], scalar2=None, op0=ALU.mult),
                         reads=[tk, t_idf], writes=[t_hnb])
                pq, tpq = bank()
                P.op("pe", lambda e, pq=pq: e.matmul(pq[:, 0:512], lhsT=ones_f[:], rhs=hnb[:, 0:512], start=True, stop=True), reads=[t_hnb, t_ones], writes=[tpq])
                P.op("dve", lambda e, dr=dr, pq=pq: e.tensor_reduce(out=G_[:, 3, dr * 4:dr * 4 + 4], in_=pq[:, 0:512].rearrange("p (h s) -> p h s", h=4), axis=mybir.AxisListType.X, op=ALU.max),
                     reads=[tpq], writes=[tk])
                nm_, tnm_ = (NML4, t_NML4) if dr == 0 else (NMU4, t_NMU4)
                P.op("dve", lambda e, pq=pq, nm_=nm_: e.tensor_tensor(out=scr[:, 0:512], in0=pq[:, 0:512], in1=nm_[:].rearrange("p h s -> p (h s)"), op=ALU.add),
                     reads=[tpq, tnm_], writes=[t_scr])
                P.op("dve", lambda e, dr=dr: e.tensor_reduce(out=G_[:, 5, dr * 4:dr * 4 + 4], in_=scr[:, 0:512].rearrange("p (h s) -> p h s", h=4), axis=mybir.AxisListType.X, op=ALU.max),
                     reads=[t_scr], writes=[tk])
            P.op("dve", lambda e: e.tensor_tensor(out=small[:, 40:48], in0=G_[:, 0, :], in1=G_[:, 3, :], op=ALU.subtract), reads=[tk], writes=[t_small])
            P.op("act", lambda e: e.activation(out=G_[:, 4, :], in_=small[:, 40:48], func=AF.Exp), reads=[t_small], writes=[tk])

        def scaled_v(slot, dr):
            for h in range(4):
                P.op("act", lambda e, h=h: e.activation(
                    out=vsc[:, dr, h, :], in_=vext[:, slot, h, :], func=AF.Identity, scale=gt[:, slot, 4, dr * 4 + h:dr * 4 + h + 1]),
                    reads=[t_vext[slot], t_gt[slot]], writes=[t_vsc])

        def d_matmuls(slot, dr):
            res = []
            pbs = [bank() for _ in range(2)]
            for h in range(4):
                pb, tpb = pbs[h // 3]
                o = (h % 3) * 132
                P.op("pe", lambda e, pb=pb, o=o, h=h: e.matmul(pb[:, o:o + 129], lhsT=ktm[:, slot, h * 128:(h + 1) * 128], rhs=vsc[:, dr, h, :], start=True, stop=True),
                     reads=[t_ktm[slot], t_vsc], writes=[tpb])
                res.append((pb[:, o:o + 129], tpb))
            return res

        def state_update(slot, dr, mask_col=None, t_mask=None):
            scaled_v(slot, dr)
            Dl = d_matmuls(slot, dr)
            c0 = dr * 4
            Gm = gt[:, slot, 3, c0:c0 + 4]
            BLc = gt[:, slot, 2, c0:c0 + 4]
            mcur = mst[:, c0:c0 + 4]
            if mask_col is not None:
                P.op("dve", lambda e: e.tensor_scalar(out=sm2[:, 0:4], in0=Gm, scalar1=BIG, scalar2=mask_col, op0=ALU.add, op1=ALU.mult), reads=[t_gt[slot], t_mask], writes=[t_sm2])
                P.op("dve", lambda e: e.tensor_scalar(out=sm2[:, 0:4], in0=sm2[:, 0:4], scalar1=-BIG, scalar2=None, op0=ALU.add), reads=[t_sm2], writes=[t_sm2])
                P.op("dve", lambda e: e.tensor_scalar(out=sm2[:, 4:8], in0=BLc, scalar1=mask_col, scalar2=None, op0=ALU.mult), reads=[t_gt[slot], t_mask], writes=[t_sm2])
            else:
                P.op("dve", lambda e: e.tensor_copy(out=sm2[:, 0:4], in_=Gm), reads=[t_gt[slot]], writes=[t_sm2])
                P.op("dve", lambda e: e.tensor_copy(out=sm2[:, 4:8], in_=BLc), reads=[t_gt[slot]], writes=[t_sm2])
            P.op("dve", lambda e: e.tensor_tensor(out=sm2[:, 8:12], in0=mcur, in1=sm2[:, 0:4], op=ALU.max), reads=[t_mst, t_sm2], writes=[t_sm2])
            P.op("dve", lambda e: e.tensor_tensor(out=sm2[:, 12:16], in0=mcur, in1=sm2[:, 8:12], op=ALU.subtract), reads=[t_mst, t_sm2], writes=[t_sm2])
            P.op("dve", lambda e: e.tensor_tensor(out=sm2[:, 16:20], in0=sm2[:, 0:4], in1=sm2[:, 8:12], op=ALU.subtract), reads=[t_sm2], writes=[t_sm2])
            P.op("act", lambda e: e.activation(out=sm2[:, 12:20], in_=sm2[:, 12:20], func=AF.Exp), reads=[t_sm2], writes=[t_sm2])
            for h in range(4):
                u = c0 + h
                dps, tdps = Dl[h]
                P.op("dve", lambda e, u=u, h=h: e.tensor_scalar(out=Sst[:, u, :], in0=Sst[:, u, :], scalar1=sm2[:, 12 + h:13 + h], scalar2=None, op0=ALU.mult),
                     reads=[t_S[u], t_sm2], writes=[t_S[u]])
                P.op("dve", lambda e, u=u, h=h, dps=dps: e.scalar_tensor_tensor(out=Sst[:, u, :], in0=dps, scalar=sm2[:, 16 + h:17 + h], in1=Sst[:, u, :], op0=ALU.mult, op1=ALU.add),
                     reads=[tdps, t_S[u], t_sm2], writes=[t_S[u]])
                P.op("act", lambda e, u=u: e.copy(out=Sbf[:, u, :], in_=Sst[:, u, :]), reads=[t_S[u]], writes=[t_Sbf[u]])
            P.op("dve", lambda e: e.tensor_tensor(out=mcur, in0=sm2[:, 4:8], in1=sm2[:, 8:12], op=ALU.add), reads=[t_sm2], writes=[t_mst])

        def make_uT(src_f32, t_src, cond, vec_shift, vec_scale, dst_fn, t_dst, want_f32=None, t_f32=None):
            xb_i = 0
            P.op("dve", lambda e: e.tensor_copy(out=xtb[:, xb_i, :], in_=src_f32), reads=[t_src], writes=[t_xtb[xb_i]])
            for half in range(2):
                pb, tpb = bank()
                pbb = pb[:].bitcast(BF16)
                for q in range(4):
                    kc = half * 4 + q
                    P.op("pe", lambda e, q=q, kc=kc, pbb=pbb: e.transpose(pbb[:, q * 128:(q + 1) * 128], xtb[:, xb_i, kc * 128:(kc + 1) * 128], ident_b[:]),
                         reads=[t_xtb[xb_i], t_idb], writes=[tpb])
                for q in range(4):
                    kc = half * 4 + q
                    P.op("act", lambda e, q=q, kc=kc, pbb=pbb: e.activation(
                        out=dst_fn(kc), in_=pbb[:, q * 128:(q + 1) * 128], func=AF.Identity,
                        scale=modT[:, vec_scale, kc, cond:cond + 1], bias=modT[:, vec_shift, kc, cond:cond + 1]),
                        reads=[tpb, t_modT], writes=[t_dst])

        def load_wpiece(src2d, c0, ncols, row_scale=False, eng=None, into=None):
            if into is None:
                i = wb_ctr[0] % WB_N
                wb_ctr[0] += 1
                buf, tk = wb[i], t_wb[i]
            else:
                buf, tk = into
            cc = 0
            while cc < ncols:
                w = min(256, ncols - cc)
                s_t, s_k = stage_load_w(src2d, c0 + cc, w)
                if not row_scale:
                    do_copy(eng or cast_eng(), buf[:, :, cc:cc + w], s_t[:, :, 0:w], [s_k], [tk])
                else:
                    do_copy("act", buf[:, 4:8, cc:cc + w], s_t[:, 4:8, 0:w], [s_k], [tk])
                    for kc in range(4):
                        P.op("dve", lambda e, kc=kc, cc=cc, w=w, s_t=s_t, buf=buf: e.tensor_scalar(
                            out=buf[:, kc, cc:cc + w], in0=s_t[:, kc, 0:w], scalar1=nwT[:, kc:kc + 1], scalar2=None, op0=ALU.mult),
                            reads=[s_k, t_nwT], writes=[tk])
                cc += w
            return buf, tk

        sm2 = sb("sm2", [128, 32], F32)
        t_sm2 = P.tok("sm2")
        st6 = sb("st6", [128, 4, 6], F32)
        t_st6 = P.tok("st6")
        Cout = arena3[:, :].rearrange("p (a b) -> p a b", a=8)
        t_Cout = P.tok("Cout")
        hnb = sb("hnb", [128, 512], F32)
        t_hnb = P.tok("hnb")
        hmb = sb("hmb", [128, 512], BF16)
        t_hmb = P.tok("hmb")
        lg = sb("lg", [128, 4, NE], F32)
        t_lg = P.tok("lg")
        P.op("pool", lambda e: e.memset(vext[:, :, :, 128:129], 1.0), writes=t_vext)

        def layer_norm_inplace(xi, tw, tb):
            xv = X[:, xi, :]
            for hf in range(2):
                P.op("dve", lambda e, hf=hf: e.bn_stats(out=st6[:, hf, :], in_=X[:, xi, hf * 512:(hf + 1) * 512]), reads=[tX[xi]], writes=[t_st6])
            P.op("dve", lambda e: e.bn_aggr(out=sm2[:, 0:2], in_=st6[:, 0:2, :].rearrange("p a b -> p (a b)")), reads=[t_st6], writes=[t_sm2])
            P.op("act", lambda e: e.activation(out=sm2[:, 2:3], in_=sm2[:, 1:2], func=AF.Sqrt, bias=eps_t[:], scale=1.0), reads=[t_sm2, t_ones], writes=[t_sm2])
            P.op("dve", lambda e: e.reciprocal(out=sm2[:, 3:4], in_=sm2[:, 2:3]), reads=[t_sm2], writes=[t_sm2])
            P.op("dve", lambda e: e.scalar_tensor_tensor(out=sm2[:, 4:5], in0=sm2[:, 0:1], scalar=-1.0, in1=sm2[:, 3:4], op0=ALU.mult, op1=ALU.mult),
                 reads=[t_sm2], writes=[t_sm2])
            P.op("act", lambda e: e.activation(out=xv, in_=xv, func=AF.Identity, scale=sm2[:, 3:4], bias=sm2[:, 4:5]), reads=[tX[xi], t_sm2], writes=[tX[xi]])
            P.op("dve", lambda e: e.tensor_tensor(out=xv, in0=xv, in1=lnw[:], op=ALU.mult), reads=[tX[xi], tw], writes=[tX[xi]])
            P.op("dve", lambda e: e.tensor_tensor(out=xv, in0=xv, in1=lnb[:], op=ALU.add), reads=[tX[xi], tb], writes=[tX[xi]])

        def mod_vectors(l, vecs):
            pmT, t_pmT = PLONG, t_PLONG
            for v in vecs:
                for sub in range(4):
                    pc = v * 4 + sub
                    s_t, s_k = stage_load_w(DI["w_ada"][l], pc * 256, 256)
                    pr, tpr = bank()
                    for kc in range(8):
                        P.op("pe", lambda e, kc=kc, s_t=s_t, pr=pr: e.matmul(pr[0:2, 0:256], lhsT=sT[:, 2 * kc:2 * kc + 2], rhs=s_t[:, kc, :],
                                                                            start=(kc == 0), stop=(kc == 7)), reads=[s_k, t_sT], writes=[tpr])
                    mr = tmpA[0:2, pc % 2, 0:256]
                    tmr = t_tmpA[pc % 2]
                    bp = tmpC[0:2, pc % 2, 0:256]
                    tbp = t_tmpC[pc % 2]
                    P.dma("sp", lambda e, pc=pc, bp=bp: e.dma_start(out=bp, in_=DI["b_ada2"][l][:, pc * 256:(pc + 1) * 256]), writes=[tbp])
                    P.op("dve", lambda e, pr=pr, mr=mr, bp=bp: e.tensor_tensor(out=mr, in0=pr[0:2, 0:256], in1=bp, op=ALU.add),
                         reads=[tpr, tbp], writes=[tmr])
                    if v in (2, 5):
                        for cond in range(2):
                            pg_, tpg_ = bank()
                            P.op("pe", lambda e, cond=cond, pg_=pg_, mr=mr: e.matmul(pg_[:, 0:256], lhsT=selr[0:2, cond, :], rhs=mr, start=True, stop=True),
                                 reads=[tmr, t_selr], writes=[tpg_])
                            P.op("act", lambda e, cond=cond, sub=sub, pg_=pg_: e.copy(out=gb[:, cond, sub * 256:(sub + 1) * 256], in_=pg_[:, 0:256]),
                                 reads=[tpg_], writes=[t_gb])
                    else:
                        for q in range(2):
                            ch = sub * 2 + q
                            c0 = (v * 8 + ch) * 2
                            P.op("pe", lambda e, c0=c0, q=q, mr=mr: e.transpose(pmT[:, c0:c0 + 2], mr[:, q * 128:(q + 1) * 128], ident_f[0:2, 0:2]),
                                 reads=[tmr, t_idf], writes=[t_pmT])
            for v in vecs:
                if v in (2, 5):
                    continue
                if v in (1, 4):
                    P.op("dve", lambda e, v=v: e.tensor_scalar(out=modT[:, v].rearrange("p a b -> p (a b)"), in0=pmT[:, v * 16:(v + 1) * 16], scalar1=1.0, scalar2=None, op0=ALU.add),
                         reads=[t_pmT], writes=[t_modT])
                else:
                    P.op("dve", lambda e, v=v: e.tensor_copy(out=modT[:, v].rearrange("p a b -> p (a b)"), in_=pmT[:, v * 16:(v + 1) * 16]),
                         reads=[t_pmT], writes=[t_modT])

        def tm_proj(slot, wbuf, twb, ncols, evac):
            pb, tpb = bank()
            for kc in range(8):
                P.op("pe", lambda e, kc=kc, pb=pb, wbuf=wbuf: e.matmul(pb[:, 0:ncols], lhsT=uT[:, kc, slot * 128:(slot + 1) * 128], rhs=wbuf[:, kc, 0:ncols],
                                                                      start=(kc == 0), stop=(kc == 7)), reads=[t_uT[slot], twb], writes=[tpb])
            evac(pb, tpb)

        gpc = sb("gpc", [128, 8, 16], BF16)
        t_gpc = P.tok("gpc")
        zpc = arena[:, 8192:12288].rearrange("p (a b) -> p a b", a=8)
        t_zpc = P.tok("zpc")

        cur_l = [0]

        def process_item(l, tiles, cond, is_sample, seq_out):
            T = len(tiles)
            N = 128 * T
            for tau, xi in enumerate(tiles):
                make_uT(X[:, xi, :], tX[xi], cond, 0, 1, lambda kc, tau=tau: uT[:, kc, tau * 128:(tau + 1) * 128], t_uT[tau])
            def ev_k(pb, tpb, tau):
                P.op("act", lambda e: e.mul(out=ktm[:, tau, :], in_=pb[:, 0:512], mul=SCALE_K), reads=[tpb], writes=[t_ktm[tau]])

            def ev_v(pb, tpb, tau):
                P.op("dve", lambda e: e.tensor_copy(out=vext[:, tau, :, 0:128], in_=pb[:, 0:512].rearrange("p (h d) -> p h d", h=4)),
                     reads=[tpb], writes=[t_vext[tau]])

            def ev_o(pb, tpb, tau):
                P.op("act", lambda e: e.activation(out=og[:, tau, :], in_=pb[:, 0:512], func=AF.Sigmoid), reads=[tpb], writes=[t_og[tau]])

            def ev_g(pb, tpb, tau):
                gate_math(pb[:, 0:16], tpb, tau)

            def ev_z(pb, tpb, tau):
                P.op("act", lambda e: e.copy(out=zown[:, tau, :], in_=pb[:, 0:512]), reads=[tpb], writes=[t_zown[tau]])

            plist = [("q", 0, 512, None), ("k", 512, 512, ev_k), ("v", 1024, 512, ev_v), ("o", 1536, 512, ev_o), ("g", G_OFF, 16, ev_g)]
            if not is_sample:
                plist.append(("z", F_OFF, 512, ev_z))
            nxt = load_wpiece(DI["w_in"][l], plist[0][1], plist[0][2])
            for pi, (pname, col0, ncols, evac) in enumerate(plist):
                wbuf, twb = nxt
                if pi + 1 < len(plist):
                    nxt = load_wpiece(DI["w_in"][l], plist[pi + 1][1], plist[pi + 1][2])
                if pname in ("q", "k"):
                    for h in range(4):
                        pb, tpb = bank()
                        for kc in range(8):
                            P.op("pe", lambda e, kc=kc, h=h, pb=pb, wbuf=wbuf: e.matmul(pb[:, 0:N], lhsT=wbuf[:, kc, h * 128:(h + 1) * 128], rhs=uT[:, kc, 0:N],
                                                                                       start=(kc == 0), stop=(kc == 7)), reads=t_uT[0:T] + [twb], writes=[tpb])
                        if pname == "q":
                            P.op("act", lambda e, h=h, pb=pb: e.copy(out=qT[:, h, 0:N], in_=pb[:, 0:N]), reads=[tpb], writes=[t_qT])
                        else:
                            P.op("act", lambda e, h=h, pb=pb: e.mul(out=kT[:, h, 0:N], in_=pb[:, 0:N], mul=SCALE_K), reads=[tpb], writes=[t_kT])
                if evac is not None:
                    for tau in range(T):
                        tm_proj(tau, wbuf, twb, ncols, lambda pb, tpb, tau=tau, evac=evac: evac(pb, tpb, tau))
            if not is_sample:
                P.op("pool", lambda e: e.memset(Sst[:], 0.0), writes=t_S)
                P.op("pool", lambda e: e.memset(Sbf[:], 0.0), writes=t_Sbf)
                P.op("pool", lambda e: e.memset(mst[:], 0.0), writes=[t_mst])
            else:
                for u in range(8):
                    P.op("act", lambda e, u=u: e.copy(out=Sbf[:, u, :], in_=Sst[:, u, :]), reads=[t_S[u]], writes=[t_Sbf[u]])
            for dr in range(2):
                order = list(range(T)) if dr == 0 else list(range(T - 1, -1, -1))
                nm_, tnm_ = (NMU4, t_NMU4) if dr == 0 else (NML4, t_NML4)
                c0 = dr * 4
                for oi, tau in enumerate(order):
                    sx = (dr * T + oi) % 2
                    G_ = gt[:, tau]
                    P.op("dve", lambda e, G_=G_: e.tensor_tensor(out=sm2[:, 20:24], in0=G_[:, 5, c0:c0 + 4], in1=mst[:, c0:c0 + 4], op=ALU.max), reads=[t_gt[tau], t_mst], writes=[t_sm2])
                    P.op("dve", lambda e: e.tensor_tensor(out=sm2[:, 24:28], in0=mst[:, c0:c0 + 4], in1=sm2[:, 20:24], op=ALU.subtract), reads=[t_mst, t_sm2], writes=[t_sm2])
                    P.op("dve", lambda e, G_=G_: e.scalar_tensor_tensor(out=sm2[:, 28:32], in0=G_[:, 1, c0:c0 + 4], scalar=-1.0, in1=sm2[:, 20:24], op0=ALU.mult, op1=ALU.subtract),
                         reads=[t_gt[tau], t_sm2], writes=[t_sm2])
                    P.op("act", lambda e: e.activation(out=sm2[:, 24:32], in_=sm2[:, 24:32], func=AF.Exp), reads=[t_sm2], writes=[t_sm2])
                    for h in range(4):
                        P.op("dve", lambda e, h=h: e.tensor_scalar(out=hnb[:, h * 128:(h + 1) * 128], in0=ident_f[:], scalar1=sm2[:, 20 + h:21 + h], scalar2=-1.0, op0=ALU.mult, op1=ALU.mult),
                             reads=[t_sm2, t_idf], writes=[t_hnb])
                    pr_, tpr_ = bank()
                    P.op("pe", lambda e, pr_=pr_: e.matmul(pr_[:, 0:512], lhsT=ones_f[:], rhs=hnb[:, 0:512], start=True, stop=False), reads=[t_hnb, t_ones], writes=[tpr_])
                    P.op("pe", lambda e, pr_=pr_, nm_=nm_: e.matmul(pr_[:, 0:512], lhsT=ident_f[:], rhs=nm_[:].rearrange("p h s -> p (h s)"), start=False, stop=True),
                         reads=[tnm_, t_idf], writes=[tpr_])
                    for h in range(4):
                        P.op("act", lambda e, h=h, pr_=pr_, G_=G_: e.activation(out=scr[:, h * 128:(h + 1) * 128], in_=pr_[:, h * 128:(h + 1) * 128], func=AF.Exp,
                                                                           bias=G_[:, 0, c0 + h:c0 + h + 1], scale=1.0), reads=[tpr_, t_gt[tau]], writes=[t_scr])
                    pst, tpst = bank()
                    for h in range(4):
                        P.op("pe", lambda e, h=h, pst=pst: e.matmul(pst[:, h * 128:(h + 1) * 128], lhsT=kT[:, h, tau * 128:(tau + 1) * 128],
                                                                   rhs=qT[:, h, tau * 128:(tau + 1) * 128], start=True, stop=True),
                             reads=[t_kT, t_qT], writes=[tpst])
                    P.op("dve", lambda e, pst=pst: e.tensor_tensor(out=STx[:, sx].rearrange("p h t -> p (h t)"), in0=pst[:, 0:512], in1=scr[:, 0:512], op=ALU.mult),
                         reads=[tpst, t_scr], writes=[t_STx[sx]])
                    for h in range(4):
                        u = c0 + h
                        ni = h % 2
                        po, tpo = bank()
                        P.op("pe", lambda e, h=h, po=po: e.matmul(po[:, 0:129], lhsT=STx[:, sx, h, :], rhs=vext[:, tau, h, :], start=True, stop=True),
                             reads=[t_STx[sx], t_vext[tau]], writes=[tpo])
                        P.op("pe", lambda e, h=h, u=u, po=po: e.matmul(po[:, 132:261], lhsT=qT[:, h, tau * 128:(tau + 1) * 128], rhs=Sbf[:, u, :], start=True, stop=True),
                             reads=[t_qT, t_Sbf[u]], writes=[tpo])
                        P.op("act", lambda e, h=h, po=po, ni=ni: e.activation(out=nd[:, ni, :], in_=po[:, 132:261], func=AF.Identity, scale=sm2[:, 24 + h:25 + h]),
                             reads=[tpo, t_sm2], writes=[t_nd[ni]])
                        P.op("dve", lambda e, po=po, ni=ni: e.tensor_tensor(out=nd[:, ni, :], in0=po[:, 0:129], in1=nd[:, ni, :], op=ALU.add), reads=[tpo, t_nd[ni]], writes=[t_nd[ni]])
                        P.op("dve", lambda e, ni=ni: e.scalar_tensor_tensor(out=sm2[:, 8:9], in0=nd[:, ni, 128:129], scalar=-1.0, in1=nd[:, ni, 128:129], op0=ALU.mult, op1=ALU.max),
                             reads=[t_nd[ni]], writes=[t_sm2])
                        P.op("dve", lambda e, h=h: e.tensor_tensor(out=sm2[:, 9:10], in0=sm2[:, 8:9], in1=sm2[:, 28 + h:29 + h], op=ALU.max), reads=[t_sm2], writes=[t_sm2])
                        P.op("dve", lambda e: e.reciprocal(out=sm2[:, 10:11], in_=sm2[:, 9:10]), reads=[t_sm2], writes=[t_sm2])
                        if dr == 0:
                            P.op("dve", lambda e, h=h, ni=ni: e.tensor_scalar(out=hacc[:, tau, h * 128:(h + 1) * 128], in0=nd[:, ni, 0:128], scalar1=sm2[:, 10:11], scalar2=None, op0=ALU.mult),
                                 reads=[t_nd[ni], t_sm2], writes=[t_hacc[tau]])
                        else:
                            P.op("dve", lambda e, h=h, ni=ni: e.scalar_tensor_tensor(out=hacc[:, tau, h * 128:(h + 1) * 128], in0=nd[:, ni, 0:128], scalar=sm2[:, 10:11],
                                                                                 in1=hacc[:, tau, h * 128:(h + 1) * 128], op0=ALU.mult, op1=ALU.add),
                                 reads=[t_nd[ni], t_sm2, t_hacc[tau]], writes=[t_hacc[tau]])
                    if (not is_sample) or oi < T - 1:
                        state_update(tau, dr)
                if _DBG and l == 0 and seq_out == 1:
                    tkh = P.tok("dbgh")
                    out_toks.append(tkh)
                    P.dma("sp", lambda e, dr=dr: e.dma_start(out=DO["dbg_h1" if dr == 0 else "dbg_h2"], in_=hacc[:, 0:2, :]), reads=t_hacc[0:2], writes=[tkh])
                    if dr == 0:
                        for nm2, src, rd in [("dbg_stx", STx[:], t_STx), ("dbg_scr", scr[:], [t_scr]), ("dbg_nd", nd[:], t_nd), ("dbg_sm2", sm2[:], [t_sm2])]:
                            tkq = P.tok(nm2)
                            out_toks.append(tkq)
                            P.dma("sp", lambda e, nm2=nm2, src=src: e.dma_start(out=DO[nm2], in_=src), reads=rd, writes=[tkq])
                        tkq = P.tok("dbgqk")
                        out_toks.append(tkq)
                        P.dma("sp", lambda e: e.dma_start(out=DO["dbg_qk"][:, 0], in_=qT[:, :, 0:256]), reads=[t_qT], writes=[tkq])
                        P.dma("sp", lambda e: e.dma_start(out=DO["dbg_qk"][:, 1], in_=kT[:, :, 0:256]), reads=[t_kT], writes=[tkq])
                        tkg = P.tok("dbgg")
                        out_toks.append(tkg)
                        P.dma("sp", lambda e: e.dma_start(out=DO["dbg_gt"], in_=gt[:, 0:2].rearrange("p a b c -> p a (b c)")), reads=t_gt[0:2], writes=[tkg])
            if not is_sample:
                tk_nm, tk_nC, tk_nn = P.tok("nm"), P.tok("nC"), P.tok("nn")
                out_toks.extend([tk_nm, tk_nC, tk_nn])
                P.dma("sp", lambda e: e.dma_start(out=DO["nm"][seq_out, l].rearrange("(o u) -> o u", o=1), in_=mst[0:1, 0:8]), reads=[t_mst], writes=[tk_nm])
                P.dma("sp", lambda e: e.dma_start(out=DO["nC"][seq_out, l].rearrange("u d e -> d u e"), in_=Sst[:, :, 0:128]), reads=t_S, writes=[tk_nC])
                P.dma("sp", lambda e: e.dma_start(out=DO["nn"][seq_out, l].rearrange("u d -> d u"), in_=Sst[:, :, 128], allow_slow_non_contiguous=True), reads=t_S, writes=[tk_nn])
            for tau in range(T):
                for h in range(4):
                    P.op("dve", lambda e, h=h: e.bn_stats(out=st6[:, h, :], in_=hacc[:, tau, h * 128:(h + 1) * 128]), reads=[t_hacc[tau]], writes=[t_st6])
                for h in range(4):
                    P.op("dve", lambda e, h=h: e.bn_aggr(out=sm2[:, 2 * h:2 * h + 2], in_=st6[:, h, :]), reads=[t_st6], writes=[t_sm2])
                P.op("act", lambda e: e.activation(out=sm2[:, 8:12], in_=sm2[:, 0:8].rearrange("p (h t) -> p h t", t=2)[:, :, 1], func=AF.Sqrt, bias=eps_t[:], scale=1.0),
                     reads=[t_sm2, t_ones], writes=[t_sm2])
                P.op("dve", lambda e: e.reciprocal(out=sm2[:, 12:16], in_=sm2[:, 8:12]), reads=[t_sm2], writes=[t_sm2])
                P.op("dve", lambda e: e.scalar_tensor_tensor(out=sm2[:, 16:20], in0=sm2[:, 0:8].rearrange("p (h t) -> p h t", t=2)[:, :, 0], scalar=-1.0, in1=sm2[:, 12:16],
                                                             op0=ALU.mult, op1=ALU.mult), reads=[t_sm2], writes=[t_sm2])
                for h in range(4):
                    P.op("act", lambda e, h=h: e.activation(out=hnb[:, h * 128:(h + 1) * 128], in_=hacc[:, tau, h * 128:(h + 1) * 128], func=AF.Identity,
                                                            scale=sm2[:, 12 + h:13 + h], bias=sm2[:, 16 + h:17 + h]), reads=[t_hacc[tau], t_sm2], writes=[t_hnb])
                P.op("dve", lambda e: e.tensor_tensor(out=hmb[:], in0=hnb[:], in1=og[:, tau, :], op=ALU.mult), reads=[t_hnb, t_og[tau]], writes=[t_hmb])
                pb, tpb = bank()
                pbb = pb[:].bitcast(BF16)
                for h in range(4):
                    P.op("pe", lambda e, h=h, pbb=pbb: e.transpose(pbb[:, h * 128:(h + 1) * 128], hmb[:, h * 128:(h + 1) * 128], ident_b[:]), reads=[t_hmb, t_idb], writes=[tpb])
                P.op("act", lambda e, pbb=pbb: e.copy(out=hmT[:, 0:4, tau * 128:(tau + 1) * 128], in_=pbb[:, 0:512].rearrange("p (h t) -> p h t", h=4)),
                     reads=[tpb], writes=[t_hmT[tau]])
            if not is_sample:
                for g in range(4):
                    pbs = []
                    for cs, (tab, ttab) in enumerate(((c256, t_c256), (s256, t_s256))):
                        pb, tpb = bank()
                        for mc in range(2):
                            P.op("pe", lambda e, mc=mc, g=g, pb=pb, tab=tab: e.matmul(pb[:, 0:256], lhsT=zown[:, mc, g * 128:(g + 1) * 128], rhs=tab[:, mc, :],
                                                                                     start=(mc == 0), stop=(mc == 1)), reads=[t_zown[mc], ttab], writes=[tpb])
                        P.op("act" if cs else "dve", (lambda e, cs=cs, g=g, pb=pb: e.copy(out=p12[:, g % 2, cs, 0:256], in_=pb[:, 0:256])) if cs else
                             (lambda e, cs=cs, g=g, pb=pb: e.tensor_copy(out=p12[:, g % 2, cs, 0:256], in_=pb[:, 0:256])), reads=[tpb], writes=[t_p12[g % 2]])
                    py, tpy = bank()
                    P.op("pe", lambda e, g=g, py=py: e.matmul(py[:, 0:256], lhsT=consts["ccp"][0][:], rhs=p12[:, g % 2, 0, 0:256], start=True, stop=False),
                         reads=[t_p12[g % 2], consts["ccp"][1]], writes=[tpy])
                    P.op("pe", lambda e, g=g, py=py: e.matmul(py[:, 0:256], lhsT=consts["nscp"][0][:], rhs=p12[:, g % 2, 1, 0:256], start=False, stop=True),
                         reads=[t_p12[g % 2], consts["nscp"][1]], writes=[tpy])
                    P.op("act", lambda e, g=g, py=py: e.copy(out=hmT[:, 4 + g, 0:256], in_=py[:, 0:256]), reads=[tpy], writes=[t_fmT] + t_hmT[0:2])
            else:
                for gp in range(2):
                    pbs = [bank() for _ in range(4)]
                    for mc in range(16):
                        bi = mc % 2
                        P.dma("sp", lambda e, mc=mc, bi=bi: e.dma_start(out=dtab[:, bi, 0, :], in_=DI["cn_own"][mc * 128:(mc + 1) * 128, :]), writes=[t_dtab[bi]])
                        P.dma("sp", lambda e, mc=mc, bi=bi: e.dma_start(out=dtab[:, bi, 1, :], in_=DI["sn_own"][mc * 128:(mc + 1) * 128, :]), writes=[t_dtab[bi]])
                        P.dma("sp", lambda e, mc=mc, bi=bi: e.dma_start(out=zst[:, bi, :], in_=zscr.ap()[mc * 128:(mc + 1) * 128, :]), reads=[t_zscr], writes=[t_zst[bi]])
                        for gl in range(2):
                            g = gp * 2 + gl
                            for cs in range(2):
                                pb, tpb = pbs[gl * 2 + cs]
                                P.op("pe", lambda e, mc=mc, g=g, cs=cs, pb=pb, bi=bi: e.matmul(pb[:, 0:512], lhsT=zst[:, bi, g * 128:(g + 1) * 128], rhs=dtab[:, bi, cs, :],
                                                                                              start=(mc == 0), stop=(mc == 15)), reads=[t_zst[bi], t_dtab[bi]], writes=[tpb])
                    for gl in range(2):
                        g = gp * 2 + gl
                        for cs in range(2):
                            pb, tpb = pbs[gl * 2 + cs]
                            if cs:
                                P.op("act", lambda e, gl=gl, cs=cs, pb=pb: e.copy(out=p12[:, gl, cs, :], in_=pb[:, 0:512]), reads=[tpb], writes=[t_p12[gl]])
                            else:
                                P.op("dve", lambda e, gl=gl, cs=cs, pb=pb: e.tensor_copy(out=p12[:, gl, cs, :], in_=pb[:, 0:512]), reads=[tpb], writes=[t_p12[gl]])
                        py, tpy = bank()
                        P.op("pe", lambda e, gl=gl, py=py: e.matmul(py[:, 0:512], lhsT=consts["ccs"][0][:], rhs=p12[:, gl, 0, :], start=True, stop=False),
                             reads=[t_p12[gl], consts["ccs"][1]], writes=[tpy])
                        P.op("pe", lambda e, gl=gl, py=py: e.matmul(py[:, 0:512], lhsT=consts["nscs"][0][:], rhs=p12[:, gl, 1, :], start=False, stop=True),
                             reads=[t_p12[gl], consts["nscs"][1]], writes=[tpy])
                        P.op("act", lambda e, g=g, py=py: e.copy(out=hmT[:, 4 + g, 0:512], in_=py[:, 0:512]), reads=[tpy], writes=[t_fmT] + t_hmT)
            if _DBG and l == 0 and seq_out == 1:
                tkd = P.tok("dbgmix")
                out_toks.append(tkd)
                P.dma("sp", lambda e: e.dma_start(out=DO["dbg_mix"], in_=hmT[:, :, :]), reads=t_hmT + [t_fmT], writes=[tkd])
            wo = [load_wpiece(DI["w_out"][l], hf * 512, 512, row_scale=True) for hf in range(2)]
            for tau, xi in enumerate(tiles):
                for hf in range(2):
                    wbuf, twb = wo[hf]
                    pb, tpb = bank()
                    for fc in range(8):
                        P.op("pe", lambda e, fc=fc, pb=pb, wbuf=wbuf: e.matmul(pb[:, 0:512], lhsT=hmT[:, fc, tau * 128:(tau + 1) * 128], rhs=wbuf[:, fc, :],
                                                                              start=(fc == 0), stop=(fc == 7)), reads=[t_hmT[tau], t_fmT, twb], writes=[tpb])
                    P.op("dve", lambda e, hf=hf, pb=pb: e.tensor_tensor(out=xt[:, 0, hf * 512:(hf + 1) * 512], in0=pb[:, 0:512], in1=gb[:, cond, hf * 512:(hf + 1) * 512], op=ALU.mult),
                         reads=[tpb, t_gb], writes=[t_xt[0]])
                P.op("dve", lambda e, xi=xi: e.scalar_tensor_tensor(out=X[:, xi, :], in0=X[:, xi, :], scalar=ALPHA, in1=xt[:, 0, :], op0=ALU.mult, op1=ALU.add),
                     reads=[tX[xi], t_xt[0]], writes=[tX[xi]])
                layer_norm_inplace(xi, t_lnw, t_lnb)

        for l in range(NL):
            cur_l[0] = l
            if l > 0:
                moe_toks = t_gs + [t for pair in t_actT for t in pair]
                for tk in t_uT + [t_qT, t_kT, t_fmT] + t_hmT + t_ktm + t_og:
                    P.alias(tk, moe_toks)
                P.alias(t_Cout, t_GT)
            P.dma("sp", lambda e, l=l: e.dma_start(out=nwT[:], in_=DI["nwT"][l]), writes=[t_nwT])
            P.dma("sp", lambda e, l=l: e.dma_start(out=gbias[:], in_=DI["gbias"][l]), writes=[t_gbias])
            P.dma("sp", lambda e, l=l: e.dma_start(out=rw[:], in_=DI["router_w"][l].rearrange("(kc p) n -> p kc n", p=128)), writes=[t_rw])
            P.dma("sp", lambda e, l=l: e.dma_start(out=rbb[:], in_=DI["rbb"][l]), writes=[t_rbb])
            P.dma("sp", lambda e, l=l: e.dma_start(out=bguT[:], in_=DI["bguT"][l]), writes=[t_bguT])
            P.dma("sp", lambda e, l=l: e.dma_start(out=bdn[:], in_=DI["b_down"][l]), writes=[t_bdn])
            P.dma("sp", lambda e, l=l: e.dma_start(out=smb[:], in_=DI["smb"][l]), writes=[t_smb])
            P.dma("sp", lambda e, l=l: e.dma_start(out=lnw[:], in_=DI["ln1w"][l]), writes=[t_lnw])
            P.dma("sp", lambda e, l=l: e.dma_start(out=lnb[:], in_=DI["ln1b"][l]), writes=[t_lnb])
            mod_vectors(l, [0, 1, 2])

            P.dma("sp", lambda e, l=l: e.dma_start(out=Sst[:, :, 0:128], in_=DI["sC"][l].rearrange("u d e -> d u e")), writes=t_S)
            P.dma("sp", lambda e, l=l: e.dma_start(out=sm2[:, 20:28], in_=DI["sn"][l]), writes=[t_sm2])
            P.op("dve", lambda e: e.tensor_copy(out=Sst[:, :, 128], in_=sm2[:, 20:28]), reads=[t_sm2], writes=t_S)
            P.op("dve", lambda e: e.tensor_copy(out=mst[:], in_=smb[:]), reads=[t_smb], writes=[t_mst])
            kpc = load_wpiece(DI["w_in"][l], 512, 512, into=(wb[0], t_wb[0]))
            vpc = load_wpiece(DI["w_in"][l], 1024, 512, into=(wb[1], t_wb[1]))
            gpiece = load_wpiece(DI["w_in"][l], G_OFF, 16, into=(gpc, t_gpc))
            P.alias(t_zpc, t_hmT + [t_fmT] + t_gs)
            zpiece = load_wpiece(DI["w_in"][l], F_OFF, 512, into=(zpc, t_zpc))
            steps = [(0, c) for c in range(16)] + [(1, c) for c in range(15, -1, -1)]

            def front(gi):
                sp_dir, c = steps[gi]
                slot = gi % 4
                xs = gi % 2
                if l == 0:
                    P.dma("sp", lambda e: e.dma_start(out=xt[:, xs, :], in_=DI["xs_full"][c * 128:(c + 1) * 128, :]), writes=[t_xt[xs]])
                    for hf in range(2):
                        P.dma("sp", lambda e, hf=hf: e.dma_start(out=scr[:, :], in_=DI["pos_full"][c * 128:(c + 1) * 128, hf * 512:(hf + 1) * 512]), writes=[t_scr])
                        P.op("dve", lambda e, hf=hf: e.tensor_tensor(out=xt[:, xs, hf * 512:(hf + 1) * 512], in0=xt[:, xs, hf * 512:(hf + 1) * 512], in1=scr[:], op=ALU.add),
                             reads=[t_xt[xs], t_scr], writes=[t_xt[xs]])
                else:
                    P.dma("sp", lambda e: e.dma_start(out=xt[:, xs, :], in_=gouts[c % 4].ap()[(c // 4) * 128:(c // 4 + 1) * 128, :]), reads=[t_gout[c % 4]], writes=[t_xt[xs]])
                make_uT(xt[:, xs, :], t_xt[xs], 1, 0, 1, lambda kc: uT[:, kc, slot * 128:(slot + 1) * 128], t_uT[slot])

                def ev_k(pb, tpb):
                    P.op("act", lambda e: e.mul(out=ktm[:, slot, :], in_=pb[:, 0:512], mul=SCALE_K), reads=[tpb], writes=[t_ktm[slot]])

                def ev_v(pb, tpb):
                    P.op("dve", lambda e: e.tensor_copy(out=vext[:, slot, :, 0:128], in_=pb[:, 0:512].rearrange("p (h d) -> p h d", h=4)),
                         reads=[tpb], writes=[t_vext[slot]])

                def ev_g(pb, tpb):
                    gate_math(pb[:, 0:16], tpb, slot)

                def ev_z(pb, tpb):
                    zb = c % 2
                    P.op("act", lambda e: e.copy(out=zown[:, zb, :], in_=pb[:, 0:512]), reads=[tpb], writes=[t_zown[zb]])
                    P.dma("sp", lambda e: e.dma_start(out=zscr.ap()[c * 128:(c + 1) * 128, :], in_=zown[:, zb, :]), reads=[t_zown[zb]], writes=[t_zscr])

                tm_proj(slot, kpc[0], kpc[1], 512, ev_k)
                tm_proj(slot, vpc[0], vpc[1], 512, ev_v)
                tm_proj(slot, gpiece[0], gpiece[1], 16, ev_g)
                if sp_dir == 0:
                    tm_proj(slot, zpiece[0], zpiece[1], 512, ev_z)

            def back(gi):
                sp_dir, c = steps[gi]
                mk, tmk = (maskf, t_maskf) if sp_dir == 0 else (maskb, t_maskb)
                state_update(gi % 4, sp_dir, mk[:, c:c + 1], tmk)

            front(0)
            for gi in range(len(steps)):
                if gi + 1 < len(steps):
                    front(gi + 1)
                back(gi)

            for tkz in t_hmT + [t_fmT]:
                P.alias(tkz, [t_zpc])
            for tk in t_hacc:
                P.alias(tk, t_tmpA + t_tmpC)
            process_item(l, [4, 5, 6, 7], 1, True, None)
            process_item(l, [0, 1], 0, False, 0)
            process_item(l, [2, 3], 0, False, 1)

            if _DBG and l == 0:
                tkd2 = P.tok("dbgx1")
                out_toks.append(tkd2)
                P.dma("sp", lambda e: e.dma_start(out=DO["dbg_x1"], in_=X[:, :, :]), reads=tX, writes=[tkd2])
            mixer_toks = t_uT + [t_qT, t_kT, t_fmT] + t_hmT + t_ktm + t_og
            for tk in t_gs + [t for pair in t_actT for t in pair]:
                P.alias(tk, mixer_toks)
            for tk in t_tmpA + t_tmpC:
                P.alias(tk, t_hacc)
            for tk in t_GT:
                P.alias(tk, [t_Cout])
            mod_vectors(l, [3, 4, 5])
            P.dma("sp", lambda e, l=l: e.dma_start(out=lnw[:], in_=DI["ln2w"][l]), writes=[t_lnw])
            P.dma("sp", lambda e, l=l: e.dma_start(out=lnb[:], in_=DI["ln2b"][l]), writes=[t_lnb])
            for xi in range(8):
                cond = 0 if xi < 4 else 1
                for half in range(2):
                    pb, tpb = bank()
                    for q in range(4):
                        kc = half * 4 + q
                        P.op("pe", lambda e, q=q, kc=kc, pb=pb, xi=xi: e.transpose(pb[:, q * 128:(q + 1) * 128], X[:, xi, kc * 128:(kc + 1) * 128], ident_f[:]),
                             reads=[tX[xi], t_idf], writes=[tpb])
                    for q in range(4):
                        kc = half * 4 + q
                        P.op("act", lambda e, q=q, kc=kc, pb=pb, cond=cond: e.activation(out=u2f[:, kc, :], in_=pb[:, q * 128:(q + 1) * 128], func=AF.Identity,
                                                                                         scale=modT[:, 4, kc, cond:cond + 1], bias=modT[:, 3, kc, cond:cond + 1]),
                             reads=[tpb, t_modT], writes=[t_u2f])
                P.op("act", lambda e, xi=xi: e.copy(out=u2T[:, :, xi * 128:(xi + 1) * 128], in_=u2f[:]), reads=[t_u2f], writes=[t_u2T[xi]])
                pl, tpl = bank()
                for kc in range(8):
                    P.op("pe", lambda e, kc=kc, pl=pl: e.matmul(pl[:, 0:NE], lhsT=u2f[:, kc, :], rhs=rw[:, kc, :], start=(kc == 0), stop=(kc == 7)),
                         reads=[t_u2f, t_rw], writes=[tpl])
                P.op("dve", lambda e, pl=pl: e.tensor_tensor(out=lg[:, 0, :], in0=pl[:, 0:NE], in1=rbb[:], op=ALU.add), reads=[tpl, t_rbb], writes=[t_lg])
                P.op("dve", lambda e: e.max(out=sm2[:, 0:8], in_=lg[:, 0, :]), reads=[t_lg], writes=[t_sm2])
                P.op("dve", lambda e: e.tensor_scalar(out=lg[:, 1, :], in0=lg[:, 0, :], scalar1=sm2[:, 3:4], scalar2=None, op0=ALU.is_ge), reads=[t_lg, t_sm2], writes=[t_lg])
                P.op("dve", lambda e: e.tensor_scalar(out=sm2[:, 8:9], in0=sm2[:, 0:1], scalar1=-1.0, scalar2=None, op0=ALU.mult), reads=[t_sm2], writes=[t_sm2])
                P.op("act", lambda e: e.activation(out=lg[:, 2, :], in_=lg[:, 0, :], func=AF.Exp, bias=sm2[:, 8:9], scale=1.0), reads=[t_lg, t_sm2], writes=[t_lg])
                P.op("dve", lambda e: e.tensor_tensor(out=lg[:, 3, :], in0=lg[:, 2, :], in1=lg[:, 1, :], op=ALU.mult), reads=[t_lg], writes=[t_lg])
                P.op("dve", lambda e: e.reduce_sum(out=sm2[:, 9:10], in_=lg[:, 3, :], axis=mybir.AxisListType.X), reads=[t_lg], writes=[t_sm2])
                P.op("dve", lambda e: e.reciprocal(out=sm2[:, 10:11], in_=sm2[:, 9:10]), reads=[t_sm2], writes=[t_sm2])
                P.op("dve", lambda e, xi=xi: e.tensor_scalar(out=Gall[:, xi, :], in0=lg[:, 3, :], scalar1=sm2[:, 10:11], scalar2=None, op0=ALU.mult),
                     reads=[t_lg, t_sm2], writes=[t_G[xi]])
                pg2, tpg2 = bank()
                P.op("pe", lambda e, xi=xi, pg2=pg2: e.transpose(pg2[0:NE, 0:128], Gall[:, xi, :], ident_f[:]), reads=[t_G[xi], t_idf], writes=[tpg2])
                P.op("act", lambda e, xi=xi, pg2=pg2: e.copy(out=GT[:, xi * 128:(xi + 1) * 128], in_=pg2[0:NE, 0:128]), reads=[tpg2], writes=[t_GT[xi]])
                P.op("dve", lambda e, xi=xi: e.tensor_scalar(out=X[:, xi, :], in0=X[:, xi, :], scalar1=ALPHA, scalar2=None, op0=ALU.mult), reads=[tX[xi]], writes=[tX[xi]])
                for hf in range(2):
                    pbb_, tpbb_ = bank()
                    P.op("pe", lambda e, xi=xi, hf=hf, pbb_=pbb_: e.matmul(pbb_[:, 0:512], lhsT=GT[:, xi * 128:(xi + 1) * 128], rhs=bdn[:, hf * 512:(hf + 1) * 512], start=True, stop=True),
                         reads=[t_GT[xi], t_bdn], writes=[tpbb_])
                    P.op("dve", lambda e, hf=hf, pbb_=pbb_, cond=cond: e.tensor_tensor(out=xt[:, 1, hf * 512:(hf + 1) * 512], in0=pbb_[:, 0:512], in1=gb[:, cond, hf * 512:(hf + 1) * 512], op=ALU.mult),
                         reads=[tpbb_, t_gb], writes=[t_xt[1]])
                P.op("dve", lambda e, xi=xi: e.tensor_tensor(out=X[:, xi, :], in0=X[:, xi, :], in1=xt[:, 1, :], op=ALU.add), reads=[tX[xi], t_xt[1]], writes=[tX[xi]])
            pieces = []
            for ex in range(NEX):
                pieces += [("glu", ex, 0, 0), ("lin", ex, 0, 1024), ("glu", ex, 1, 512), ("lin", ex, 1, 1536), ("down", ex, 0, 0), ("down", ex, 1, 512)]

            def fetch(pc):
                kind, ex, fb, c0 = pc
                src = DI["w_down"][l, ex] if kind == "down" else DI["w_gu"][l, ex]
                return load_wpiece(src, c0, 512, eng="act")

            nxt = fetch(pieces[0]) if pieces else None
            for pi, pc in enumerate(pieces):
                kind, ex, fb, c0 = pc
                wbuf, twb = nxt
                if pi + 1 < len(pieces):
                    nxt = fetch(pieces[pi + 1])
                if kind == "glu":
                    for fcl in range(4):
                        fc = fb * 4 + fcl
                        for th in range(2):
                            pb, tpb = bank()
                            for kc in range(8):
                                P.op("pe", lambda e, kc=kc, fcl=fcl, th=th, pb=pb, wbuf=wbuf: e.matmul(pb[:, 0:512], lhsT=wbuf[:, kc, fcl * 128:(fcl + 1) * 128], rhs=u2T[:, kc, th * 512:(th + 1) * 512],
                                                                                                   start=(kc == 0), stop=(kc == 7)), reads=[twb] + t_u2T[th * 4:th * 4 + 4], writes=[tpb])
                            i2 = (fcl * 2 + th) % 2
                            P.op("dve", lambda e, fc=fc, ex=ex, pb=pb, i2=i2: e.tensor_scalar(out=tmpA[:, i2, :], in0=pb[:, 0:512], scalar1=bguT[:, fc, ex:ex + 1], scalar2=LIM, op0=ALU.add, op1=ALU.min),
                                 reads=[tpb, t_bguT], writes=[t_tmpA[i2]])
                            P.op("act", lambda e, i2=i2: e.activation(out=tmpB[:, i2, :], in_=tmpA[:, i2, :], func=AF.Sigmoid, scale=SALPHA), reads=[t_tmpA[i2]], writes=[t_tmpB[i2]])
                            P.op("dve", lambda e, fcl=fcl, th=th, i2=i2: e.tensor_tensor(out=gsb[:, fcl, th * 512:(th + 1) * 512], in0=tmpA[:, i2, :], in1=tmpB[:, i2, :], op=ALU.mult),
                                 reads=[t_tmpA[i2], t_tmpB[i2]], writes=[t_gs[fcl * 2 + th]])
                elif kind == "lin":
                    for fcl in range(4):
                        fc = fb * 4 + fcl
                        for th in range(2):
                            pb, tpb = bank()
                            for kc in range(8):
                                P.op("pe", lambda e, kc=kc, fcl=fcl, th=th, pb=pb, wbuf=wbuf: e.matmul(pb[:, 0:512], lhsT=wbuf[:, kc, fcl * 128:(fcl + 1) * 128], rhs=u2T[:, kc, th * 512:(th + 1) * 512],
                                                                                                   start=(kc == 0), stop=(kc == 7)), reads=[twb] + t_u2T[th * 4:th * 4 + 4], writes=[tpb])
                            i2 = (fcl * 2 + th) % 2
                            P.op("act", lambda e, fc=fc, ex=ex, pb=pb, i2=i2: e.activation(out=tmpC[:, i2, :], in_=pb[:, 0:512], func=AF.Identity, bias=bguT[:, 8 + fc, ex:ex + 1], scale=1.0),
                                 reads=[tpb, t_bguT], writes=[t_tmpC[i2]])
                            P.op("dve", lambda e, i2=i2: e.tensor_scalar(out=tmpC[:, i2, :], in0=tmpC[:, i2, :], scalar1=LIM, scalar2=-LIM, op0=ALU.min, op1=ALU.max),
                                 reads=[t_tmpC[i2]], writes=[t_tmpC[i2]])
                            P.op("dve", lambda e, fc=fc, fcl=fcl, th=th, i2=i2: e.scalar_tensor_tensor(out=actT[:, fc, th * 512:(th + 1) * 512], in0=tmpC[:, i2, :], scalar=1.0, in1=gsb[:, fcl, th * 512:(th + 1) * 512],
                                                                                                  op0=ALU.add, op1=ALU.mult),
                                 reads=[t_gs[fcl * 2 + th], t_tmpC[i2]], writes=[t_actT[fc][th]])
                else:
                    dh = fb
                    for xi in range(8):
                        cond = 0 if xi < 4 else 1
                        pb, tpb = bank()
                        for fc in range(8):
                            P.op("pe", lambda e, fc=fc, xi=xi, pb=pb, wbuf=wbuf: e.matmul(pb[:, 0:512], lhsT=actT[:, fc, xi * 128:(xi + 1) * 128], rhs=wbuf[:, fc, :],
                                                                                         start=(fc == 0), stop=(fc == 7)), reads=[twb, t_actT[fc][xi // 4]], writes=[tpb])
                        i2 = xi % 2
                        P.op("dve", lambda e, xi=xi, ex=ex, pb=pb, i2=i2, cond=cond, dh=dh: e.scalar_tensor_tensor(out=tmpA[:, i2, :], in0=pb[:, 0:512], scalar=Gall[:, xi, ex:ex + 1],
                                                                                                         in1=gb[:, cond, dh * 512:(dh + 1) * 512], op0=ALU.mult, op1=ALU.mult),
                             reads=[tpb, t_G[xi], t_gb], writes=[t_tmpA[i2]])
                        P.op("dve", lambda e, xi=xi, i2=i2, dh=dh: e.tensor_tensor(out=X[:, xi, dh * 512:(dh + 1) * 512], in0=X[:, xi, dh * 512:(dh + 1) * 512], in1=tmpA[:, i2, :], op=ALU.add),
                             reads=[tX[xi], t_tmpA[i2]], writes=[tX[xi]])
            for xi in range(8):
                layer_norm_inplace(xi, t_lnw, t_lnb)

            if l < NL - 1:
                for i in range(4):
                    P.dma("pool", lambda e, i=i: e.dma_start(out=gins[i].ap(), in_=X[:, 4 + i, :]), reads=[tX[4 + i]], writes=[t_gin[i]])
                    P.dma("pool", lambda e, i=i: e.collective_compute("AllGather", ALU.bypass, replica_groups=[[0, 1, 2, 3], [4, 5, 6, 7]],
                                                                      ins=[gins[i].ap().opt()], outs=[gouts[i].ap().opt()]), reads=[t_gin[i]], writes=[t_gout[i]], inc=1)
            else:
                for i in range(4):
                    tk1, tk2 = P.tok("yp"), P.tok("ys")
                    out_toks.extend([tk1, tk2])
                    P.dma("sp", lambda e, i=i: e.dma_start(out=DO["yp"][i * 128:(i + 1) * 128, :], in_=X[:, i, :]), reads=[tX[i]], writes=[tk1])
                    P.dma("sp", lambda e, i=i: e.dma_start(out=DO["ys"][i * 128:(i + 1) * 128, :], in_=X[:, 4 + i, :]), reads=[tX[4 + i]], writes=[tk2])

        P.emit(st, final_waits=out_toks)
    return nc


def _grid_pos_embed(rows, d):
    quarter = d // 4
    omega = (1.0 / (10000.0 ** (np.arange(quarter, dtype=np.float32) / np.float32(quarter)))).astype(np.float32)
    r = np.repeat(np.arange(rows, dtype=np.float32), 64)[:, None] * omega
    cc = np.tile(np.arange(64, dtype=np.float32), rows)[:, None] * omega
    return np.concatenate([np.sin(r), np.cos(r), np.sin(cc), np.cos(cc)], axis=-1).astype(np.float32)


_CACHE = {}
_DBG = False
_CFG = {"NL": 4, "NEX": NE}


def kernel(x_prompt, x_sample, state_C, state_n, state_m, c, c_ctx, w_ada, b_ada, w_in, gate_bias, mlstm_norm_w,
           w_out, ln1_w, ln1_b, router_w, router_b, w_gate_up, b_gate_up, w_down, b_down, ln2_w, ln2_b):
    f32 = np.float32
    bf = ml_dtypes.bfloat16
    A = lambda a: np.ascontiguousarray(np.asarray(a), dtype=f32)
    x_prompt, x_sample = A(x_prompt), A(x_sample)
    NL, NEX = _CFG["NL"], _CFG["NEX"]
    key = (NL, NEX)
    if key not in _CACHE:
        _CACHE[key] = build_program(NL, NEX)
    nc = _CACHE[key]
    DEPTH = NL
    pos = _grid_pos_embed(2048 // 64, D)
    n = np.arange(2048, dtype=np.float64)
    ang = 2 * np.pi * ((n[:, None] * n[None, :]) % 2048) / 2048
    CN, SN = np.cos(ang), np.sin(ang)
    n2 = np.arange(256, dtype=np.float64)
    ang2 = 2 * np.pi * ((n2[:, None] * n2[None, :]) % 256) / 256
    n3 = np.arange(128, dtype=np.float64)
    ang3 = 2 * np.pi * ((n3[:, None] * n3[None, :]) % 128) / 128
    CC, SC = np.cos(ang3), np.sin(ang3)
    sp_, ss_ = 1.0 / np.sqrt(256 * 128.0), 1.0 / np.sqrt(2048 * 128.0)
    ar = np.arange(128)
    triU = (ar[:, None] <= ar[None, :]).astype(f32)
    triL = (ar[:, None] >= ar[None, :]).astype(f32)
    selr = np.zeros((2, 2, 128), f32)
    selr[0, 0] = 1
    selr[1, 1] = 1
    sel8 = np.zeros((8, 2), f32)
    sel8[0:4, 0] = 1
    sel8[4:8, 1] = 1
    shared = {
        "pos_full": pos,
        "w_ada": A(w_ada)[:NL], "b_ada2": np.ascontiguousarray(np.broadcast_to(A(b_ada)[:NL, None, :], (DEPTH, 2, 6 * D))),
        "w_in": A(w_in)[:NL], "gbias": np.ascontiguousarray(np.broadcast_to(A(gate_bias)[:NL, None, :], (DEPTH, 128, 16))),
        "nwT": np.ascontiguousarray(A(mlstm_norm_w)[:NL].reshape(DEPTH, 4, 128).transpose(0, 2, 1)),
        "w_out": A(w_out)[:NL],
        "ln1w": np.ascontiguousarray(np.broadcast_to(A(ln1_w)[:NL, None, :], (DEPTH, 128, D))),
        "ln1b": np.ascontiguousarray(np.broadcast_to(A(ln1_b)[:NL, None, :], (DEPTH, 128, D))),
        "router_w": A(router_w)[:NL], "rbb": np.ascontiguousarray(np.broadcast_to(A(router_b)[:NL, None, :], (DEPTH, 128, NE))),
        "w_gu": A(w_gate_up)[:NL, :max(NEX, 1)], "bguT": np.ascontiguousarray(A(b_gate_up)[:NL].reshape(DEPTH, NE, 16, 128).transpose(0, 3, 2, 1)),
        "w_down": A(w_down)[:NL, :max(NEX, 1)], "b_down": A(b_down)[:NL],
        "ln2w": np.ascontiguousarray(np.broadcast_to(A(ln2_w)[:NL, None, :], (DEPTH, 128, D))),
        "ln2b": np.ascontiguousarray(np.broadcast_to(A(ln2_b)[:NL, None, :], (DEPTH, 128, D))),
        "c256": np.cos(ang2).astype(bf), "s256": np.sin(ang2).astype(bf),
        "ccp": (CC * sp_).astype(bf), "nscp": (-SC * sp_).astype(bf), "ccs": (CC * ss_).astype(bf), "nscs": (-SC * ss_).astype(bf),
        "ident_b": np.eye(128, dtype=f32).astype(bf), "ident_f": np.eye(128, dtype=f32), "triU": triU, "triL": triL,
        "selr": selr, "sel8": sel8,
        "NMU4": np.ascontiguousarray(np.broadcast_to(((triU - 1.0) * 30000.0)[:, None, :], (128, 4, 128))).astype(f32),
        "NML4": np.ascontiguousarray(np.broadcast_to(((triL - 1.0) * 30000.0)[:, None, :], (128, 4, 128))).astype(f32),
    }
    state_C, state_n, state_m, c, c_ctx = A(state_C), A(state_n), A(state_m), A(c), A(c_ctx)
    in_maps = []
    for core in range(8):
        b, j = divmod(core, 4)
        cv = np.stack([c_ctx, c[b]], 0)
        cvT = np.ascontiguousarray(cv.reshape(2, 8, 128).transpose(2, 1, 0).reshape(128, 16))
        mf = np.zeros((128, 16), f32)
        mb = np.zeros((128, 16), f32)
        mf[:, :4 * j] = 1
        mb[:, 4 * j + 4:] = 1
        m = dict(shared)
        m.update({
            "xp": np.ascontiguousarray(x_prompt[2 * core:2 * core + 2].reshape(512, D)),
            "xs_own": np.ascontiguousarray(x_sample[b, 512 * j:512 * j + 512]),
            "pos_own": np.ascontiguousarray(pos[512 * j:512 * j + 512]),
            "xs_full": np.ascontiguousarray(x_sample[b]),
            "sC": np.ascontiguousarray(state_C[b][:NL].reshape(DEPTH, 8, 128, 128)),
            "sn": np.ascontiguousarray(state_n[b][:NL].reshape(DEPTH, 8, 128).transpose(0, 2, 1)),
            "smb": np.ascontiguousarray(np.broadcast_to(state_m[b][:NL].reshape(DEPTH, 1, 8), (DEPTH, 128, 8))),
            "cvT": cvT, "maskf": mf, "maskb": mb,
            "cn_own": np.ascontiguousarray(CN[:, 512 * j:512 * j + 512]).astype(bf),
            "sn_own": np.ascontiguousarray(SN[:, 512 * j:512 * j + 512]).astype(bf),
        })
        in_maps.append(m)
    res = run_bass_kernel_spmd(nc, in_maps, core_ids=list(range(8)))
    R = res.results
    y_prompt = np.concatenate([R[k]["yp"].reshape(2, 256, D) for k in range(8)], 0).astype(f32)
    y_sample = np.stack([np.concatenate([R[4 * b + j]["ys"] for j in range(4)], 0) for b in range(2)], 0).astype(f32)
    nC = np.concatenate([R[k]["nC"] for k in range(8)], 0).reshape(16, DEPTH, 2, 4, 128, 128).astype(f32)
    nn = np.concatenate([R[k]["nn"] for k in range(8)], 0).reshape(16, DEPTH, 2, 4, 128).astype(f32)
    nm = np.concatenate([R[k]["nm"] for k in range(8)], 0).reshape(16, DEPTH, 2, 4).astype(f32)
    if _DBG:
      _CACHE["dbg2"] = {k2: [np.asarray(R[k][k2]) for k in range(8)] for k2 in ["dbg_h1", "dbg_h2", "dbg_gt", "dbg_stx", "dbg_scr", "dbg_qk", "dbg_nd", "dbg_sm2"]}
      _CACHE["dbg"] = {"mix": [np.asarray(R[k]["dbg_mix"]) for k in range(8)], "x1": [np.asarray(R[k]["dbg_x1"]) for k in range(8)]}
    return (y_prompt, y_sample, nC, nn, nm)
```

```python
from contextlib import ExitStack
import types
import numpy as np
import ml_dtypes
import concourse.bass as bass
import concourse.mybir as mybir
from concourse.bass_utils import run_bass_kernel_spmd

F32 = mybir.dt.float32
BF16 = mybir.dt.bfloat16
AF = mybir.ActivationFunctionType
ALU = mybir.AluOpType

D = 1024
DEPTH = 4
NE = 32
DH = 128
ALPHA = (2 * DEPTH) ** 0.25
LN_EPS = 1e-5
LIM = 7.0
SALPHA = 1.702
IN_COLS = 2576
G_OFF = 2048
F_OFF = 2064

COMPUTE = ("pe", "dve", "act", "pool")
ENGS = ("pe", "dve", "act", "pool", "sp")
EPOCH = 30000


class Tok:
    __slots__ = ("name", "last_w", "readers", "dma_total", "sem", "pending")

    def __init__(self, name):
        self.name = name
        self.last_w = None
        self.readers = []
        self.dma_total = 0
        self.sem = None
        self.pending = []


def _snapshot(fn):
    if getattr(fn, "__closure__", None) is None:
        return fn
    cells = []
    for c in fn.__closure__:
        try:
            cells.append(types.CellType(c.cell_contents))
        except ValueError:
            cells.append(c)
    return types.FunctionType(fn.__code__, fn.__globals__, fn.__name__, fn.__defaults__, tuple(cells))


class Op:
    __slots__ = ("eng", "fn", "waits", "signal", "sig_idx", "dma_key", "dma_val", "inc")

    def __init__(self, eng, fn):
        self.eng = eng
        self.fn = _snapshot(fn)
        self.waits = []
        self.signal = False
        self.sig_idx = None
        self.dma_key = None
        self.dma_val = None
        self.inc = 16


class Prog:
    def __init__(self, nc):
        self.nc = nc
        self.ops = {e: [] for e in ENGS}

    def tok(self, name="t"):
        return Tok(name)

    def toks(self, n, name="t"):
        return [Tok(name) for _ in range(n)]

    def alias(self, new, olds):
        for o in olds:
            if o.last_w is not None:
                new.pending.append(o.last_w)
            new.pending.extend(o.readers)

    def _add(self, eng, fn, reads, writes, dma_key=None, inc=16):
        o = Op(eng, fn)
        deps = []
        for t in reads:
            if t.last_w is not None:
                deps.append(t.last_w)
        for t in writes:
            if t.last_w is not None:
                deps.append(t.last_w)
            deps.extend(t.readers)
            if t.pending:
                deps.extend(t.pending)
                t.pending = []
        seen = set()
        for d in deps:
            if d is o or id(d) in seen:
                continue
            seen.add(id(d))
            if d.dma_key is not None:
                o.waits.append(("d", d.dma_key, d.dma_key.dma_total))
            else:
                if d.eng == "pe" and eng == "pe":
                    continue
                d.signal = True
                o.waits.append(("c", d.eng, d))
        for t in reads:
            t.readers.append(o)
        for t in writes:
            t.last_w = o
            t.readers = []
        if dma_key is not None:
            o.dma_key = dma_key
            o.inc = inc
            dma_key.dma_total += inc
            o.dma_val = dma_key.dma_total
        self.ops[eng].append(o)
        return o

    def op(self, eng, fn, reads=(), writes=()):
        return self._add(eng, fn, list(reads), list(writes))

    def dma(self, eng, fn, reads=(), writes=(), key=None, inc=16):
        if key is None:
            key = list(writes)[0]
        return self._add(eng, fn, list(reads), list(writes), dma_key=key, inc=inc)

    def emit(self, stack, final_waits=()):
        nc = self.nc
        nsig = {}
        for e in ENGS:
            k = 0
            for o in self.ops[e]:
                if o.dma_key is None and o.signal:
                    k += 1
                    o.sig_idx = k
            nsig[e] = k
        esems = {}
        for e in ENGS:
            n_ep = max(1, (nsig[e] + EPOCH - 1) // EPOCH)
            esems[e] = [stack.enter_context(nc.semaphore(f"s_{e}{i}")) for i in range(n_ep)]
        nk = 0
        for e in ENGS:
            for o in self.ops[e]:
                if o.dma_key is not None and o.dma_key.sem is None:
                    o.dma_key.sem = stack.enter_context(nc.semaphore(f"d_{nk}"))
                    nk += 1
        self.n_sems = sum(len(v) for v in esems.values()) + nk
        block = stack.enter_context(nc.Block())
        ops = self.ops

        def run(e, eng):
            waited_c = {}
            waited_d = {}
            for o in ops[e]:
                for w in o.waits:
                    if w[0] == "c":
                        d = w[2]
                        ep, v = divmod(d.sig_idx - 1, EPOCH)
                        v += 1
                        kk = (d.eng, ep)
                        if waited_c.get(kk, 0) >= v:
                            continue
                        if any(k2[0] == d.eng and k2[1] > ep for k2 in waited_c):
                            continue
                        waited_c[kk] = v
                        eng.wait_ge(esems[d.eng][ep], v)
                    else:
                        t, v = w[1], w[2]
                        if waited_d.get(id(t), 0) >= v:
                            continue
                        waited_d[id(t)] = v
                        eng.wait_ge(t.sem, v)
                ins = o.fn(eng)
                if o.dma_key is not None:
                    ins.then_inc(o.dma_key.sem, o.inc)
                elif o.signal:
                    ins.then_inc(esems[e][(o.sig_idx - 1) // EPOCH], 1)
            if e == "sp":
                for t in final_waits:
                    eng.wait_ge(t.sem, t.dma_total)

        @block.sync
        def _(eng):
            run("sp", eng)

        @block.tensor
        def _(eng):
            run("pe", eng)

        @block.vector
        def _(eng):
            run("dve", eng)

        @block.scalar
        def _(eng):
            run("act", eng)

        @block.gpsimd
        def _(eng):
            run("pool", eng)


def input_specs(DEPTH, NEX):
  return [
    ("xp", [512, D], F32), ("xs_own", [512, D], F32), ("pos_own", [512, D], F32),
    ("xs_full", [2048, D], F32), ("pos_full", [2048, D], F32),
    ("sC", [DEPTH, 8, 128, 128], F32), ("sn", [DEPTH, 128, 8], F32), ("smb", [DEPTH, 128, 8], F32),
    ("cvT", [128, 16], F32), ("maskf", [128, 16], F32), ("maskb", [128, 16], F32),
    ("w_ada", [DEPTH, D, 6 * D], F32), ("b_ada2", [DEPTH, 2, 6 * D], F32),
    ("w_in", [DEPTH, D, IN_COLS], F32), ("gbias", [DEPTH, 128, 16], F32), ("nwT", [DEPTH, 128, 4], F32),
    ("w_out", [DEPTH, D, D], F32), ("ln1w", [DEPTH, 128, D], F32), ("ln1b", [DEPTH, 128, D], F32),
    ("router_w", [DEPTH, D, NE], F32), ("rbb", [DEPTH, 128, NE], F32),
    ("w_gu", [DEPTH, NEX, D, 2 * D], F32), ("bguT", [DEPTH, 128, 16, NE], F32),
    ("w_down", [DEPTH, NEX, D, D], F32), ("b_down", [DEPTH, NE, D], F32),
    ("ln2w", [DEPTH, 128, D], F32), ("ln2b", [DEPTH, 128, D], F32),
    ("cn_own", [2048, 512], BF16), ("sn_own", [2048, 512], BF16),
    ("c256", [256, 256], BF16), ("s256", [256, 256], BF16),
    ("ccp", [128, 128], BF16), ("nscp", [128, 128], BF16), ("ccs", [128, 128], BF16), ("nscs", [128, 128], BF16),
    ("ident_b", [128, 128], BF16), ("ident_f", [128, 128], F32), ("triU", [128, 128], F32), ("triL", [128, 128], F32),
    ("selr", [2, 2, 128], F32), ("sel8", [8, 2], F32), ("NMU4", [128, 4, 128], F32), ("NML4", [128, 4, 128], F32),
  ]


def output_specs(DEPTH):
  return [
    ("yp", [512, D], F32), ("ys", [512, D], F32),
    ("nC", [2, DEPTH, 8, 128, 128], F32), ("nn", [2, DEPTH, 8, 128], F32), ("nm", [2, DEPTH, 8], F32),
  ]


def build_program(NL=DEPTH, NEX=NE, do_cc=True, stop_after=None):
    nc = bass.Bass("TRN2", target_bir_lowering=False)
    P = Prog(nc)
    DI = {n: nc.dram_tensor(n, list(s), d, kind="ExternalInput").ap() for n, s, d in input_specs(NL, max(NEX, 1))}
    DO = {n: nc.dram_tensor(n, list(s), d, kind="ExternalOutput").ap() for n, s, d in output_specs(NL)}
    if _DBG:
        DO["dbg_mix"] = nc.dram_tensor("dbg_mix", [128, 8, 512], BF16, kind="ExternalOutput").ap()
        DO["dbg_h1"] = nc.dram_tensor("dbg_h1", [128, 2, 512], F32, kind="ExternalOutput").ap()
        DO["dbg_h2"] = nc.dram_tensor("dbg_h2", [128, 2, 512], F32, kind="ExternalOutput").ap()
        DO["dbg_gt"] = nc.dram_tensor("dbg_gt", [128, 2, 48], F32, kind="ExternalOutput").ap()
        DO["dbg_stx"] = nc.dram_tensor("dbg_stx", [128, 2, 4, 128], BF16, kind="ExternalOutput").ap()
        DO["dbg_scr"] = nc.dram_tensor("dbg_scr", [128, 512], F32, kind="ExternalOutput").ap()
        DO["dbg_qk"] = nc.dram_tensor("dbg_qk", [128, 2, 4, 256], BF16, kind="ExternalOutput").ap()
        DO["dbg_nd"] = nc.dram_tensor("dbg_nd", [128, 2, 129], F32, kind="ExternalOutput").ap()
        DO["dbg_sm2"] = nc.dram_tensor("dbg_sm2", [128, 32], F32, kind="ExternalOutput").ap()
        DO["dbg_x1"] = nc.dram_tensor("dbg_x1", [128, 8, D], F32, kind="ExternalOutput").ap()
    gins = [nc.dram_tensor(f"gin{i}", [128, D], F32) for i in range(4)]
    gouts = [nc.dram_tensor(f"gout{i}", [512, D], F32) for i in range(4)]
    t_gin, t_gout = P.toks(4, "gin"), P.toks(4, "gout")
    out_toks = []

    with ExitStack() as st:
        def sb(name, shape, dt):
            return st.enter_context(nc.sbuf_tensor("sb_" + name, list(shape), dt))

        PS = [st.enter_context(nc.psum_tensor(f"ps{i}", [128, 512], F32)) for i in range(8)]
        tPS = P.toks(8, "ps")
        bank_ctr = [0]
        PLONG, t_PLONG = PS[7], tPS[7]

        def bank():
            i = bank_ctr[0] % 7
            bank_ctr[0] += 1
            return PS[i], tPS[i]

        consts = {}
        for n, shp, dt in [("ident_b", [128, 128], BF16), ("ident_f", [128, 128], F32), ("triU", [128, 128], F32),
                           ("triL", [128, 128], F32), ("ccp", [128, 128], BF16), ("nscp", [128, 128], BF16),
                           ("ccs", [128, 128], BF16), ("nscs", [128, 128], BF16), ("cvT", [128, 16], F32),
                           ("maskf", [128, 16], F32), ("maskb", [128, 16], F32), ("selr", [2, 2, 128], F32), ("sel8", [8, 2], F32), ("NMU4", [128, 4, 128], F32), ("NML4", [128, 4, 128], F32)]:
            t = sb("c_" + n, shp, dt)
            tk = P.tok(n)
            P.dma("sp", lambda e, t=t, n=n: e.dma_start(out=t[:], in_=DI[n]), writes=[tk])
            consts[n] = (t, tk)
        ident_b, t_idb = consts["ident_b"]
        ident_f, t_idf = consts["ident_f"]
        triU, t_triU = consts["triU"]
        triL, t_triL = consts["triL"]
        cvT, t_cvT = consts["cvT"]
        maskf, t_maskf = consts["maskf"]
        maskb, t_maskb = consts["maskb"]
        selr, t_selr = consts["selr"]
        sel8, t_sel8 = consts["sel8"]
        NMU4, t_NMU4 = consts["NMU4"]
        NML4, t_NML4 = consts["NML4"]
        c256 = sb("c256", [128, 2, 256], BF16)
        s256 = sb("s256", [128, 2, 256], BF16)
        t_c256, t_s256 = P.tok(), P.tok()
        P.dma("sp", lambda e: e.dma_start(out=c256[:], in_=DI["c256"].rearrange("(m p) n -> p m n", p=128)), writes=[t_c256])
        P.dma("sp", lambda e: e.dma_start(out=s256[:], in_=DI["s256"].rearrange("(m p) n -> p m n", p=128)), writes=[t_s256])
        ones_f = sb("ones_f", [128, 128], F32)
        ones_b = sb("ones_b", [128, 128], BF16)
        eps_t = sb("eps_t", [128, 1], F32)
        t_ones = P.tok()
        P.op("pool", lambda e: e.memset(ones_f[:], 1.0), writes=[t_ones])
        P.op("pool", lambda e: e.memset(ones_b[:], 1.0), writes=[t_ones])
        P.op("pool", lambda e: e.memset(eps_t[:], LN_EPS), writes=[t_ones])
        sT = sb("sT", [128, 16], F32)
        t_sT = P.tok()
        P.op("act", lambda e: e.activation(out=sT[:], in_=cvT[:], func=AF.Silu), reads=[t_cvT], writes=[t_sT])

        X = sb("X", [128, 8, D], F32)
        tX = P.toks(8, "X")
        for i in range(4):
            P.dma("sp", lambda e, i=i: e.dma_start(out=X[:, i, :], in_=DI["xp"][i * 128:(i + 1) * 128, :]), writes=[tX[i]])
        ptmp = sb("ptmp", [128, 2, D], F32)
        t_ptmp = P.toks(2)
        for i in range(4):
            P.dma("sp", lambda e, i=i: e.dma_start(out=X[:, 4 + i, :], in_=DI["xs_own"][i * 128:(i + 1) * 128, :]), writes=[tX[4 + i]])
            P.dma("sp", lambda e, i=i: e.dma_start(out=ptmp[:, i % 2, :], in_=DI["pos_own"][i * 128:(i + 1) * 128, :]), writes=[t_ptmp[i % 2]])
            P.op("pool", lambda e, i=i: e.tensor_tensor(out=X[:, 4 + i, :], in0=X[:, 4 + i, :], in1=ptmp[:, i % 2, :], op=ALU.add),
                 reads=[tX[4 + i], t_ptmp[i % 2]], writes=[tX[4 + i]])

        STG_N = 2
        stg = [sb(f"stg{i}", [128, 8, 256], F32) for i in range(STG_N)]
        t_stg = P.toks(STG_N, "stg")
        stg_ctr = [0]

        def stage_load(src_ap):
            i = stg_ctr[0] % STG_N
            stg_ctr[0] += 1
            P.dma("sp", lambda e: e.dma_start(out=stg[i][:], in_=src_ap.rearrange("(kc p) c -> p kc c", p=128)), writes=[t_stg[i]])
            return stg[i], t_stg[i]

        cast_ctr = [0]

        def cast_eng():
            cast_ctr[0] += 1
            return "dve" if cast_ctr[0] % 2 else "act"

        def do_copy(eng, out, in_, reads, writes):
            if eng == "act":
                P.op("act", lambda e: e.copy(out=out, in_=in_), reads=reads, writes=writes)
            else:
                P.op(eng, lambda e: e.tensor_copy(out=out, in_=in_), reads=reads, writes=writes)

        nwT = sb("nwT", [128, 4], F32)
        t_nwT = P.tok()
        gbias = sb("gbias", [128, 16], F32)
        t_gbias = P.tok()
        modT = sb("modT", [128, 6, 8, 2], F32)
        t_modT = P.tok("modT")
        gb = sb("gb", [128, 2, D], F32)
        t_gb = P.tok("gb")
        lnw = sb("lnw", [128, D], F32)
        lnb = sb("lnb", [128, D], F32)
        t_lnw, t_lnb = P.tok(), P.tok()
        rw = sb("rw", [128, 8, NE], F32)
        t_rw = P.tok()
        rbb = sb("rbb", [128, NE], F32)
        t_rbb = P.tok()
        bguT = sb("bguT", [128, 16, NE], F32)
        t_bguT = P.tok()
        bdn = sb("bdn", [NE, D], F32)
        t_bdn = P.tok()
        zscr = nc.dram_tensor("zscr", [2048, 512], BF16)
        t_zscr = P.tok("zscr")
        dscr = nc.dram_tensor("dscr", [16, 128, 528], F32)
        t_dscr = P.tok("dscr")
        Sst = sb("Sst", [128, 8, 129], F32)
        t_S = P.toks(8, "S")
        Sbf = sb("Sbf", [128, 8, 129], BF16)
        t_Sbf = P.toks(8, "Sbf")
        smb = sb("smb", [128, 8], F32)
        t_smb = P.tok()
        arena = sb("arena", [128, 16384], BF16)
        uT = arena[:, 0:4096].rearrange("p (a b) -> p a b", a=8)
        t_uT = P.toks(4, "uT")
        qT = arena[:, 4096:6144].rearrange("p (a b) -> p a b", a=4)
        kT = arena[:, 6144:8192].rearrange("p (a b) -> p a b", a=4)
        t_qT, t_kT = P.tok("qT"), P.tok("kT")
        ktm = arena[:, 12288:14336].rearrange("p (a b) -> p a b", a=4)
        t_ktm = P.toks(4, "ktm")
        vext = sb("vext", [128, 4, 4, 129], BF16)
        t_vext = P.toks(4, "vext")
        vsc = sb("vsc", [128, 2, 4, 129], BF16)
        t_vsc = P.tok("vsc")
        og = arena[:, 14336:16384].rearrange("p (a b) -> p a b", a=4)
        t_og = P.toks(4, "og")
        zown = sb("zown", [128, 2, 512], BF16)
        t_zown = P.toks(2, "zown")
        zst, t_zst = zown, t_zown
        gt = sb("gt", [128, 4, 6, 8], F32)
        mst = sb("mst", [128, 8], F32)
        t_mst = P.tok("mst")
        scr = sb("scr", [128, 512], F32)
        t_scr = P.tok("scr")
        nd = sb("nd", [128, 2, 129], F32)
        t_nd = P.toks(2, "nd")
        t_gt = P.toks(4, "gt")
        STx = sb("STx", [128, 2, 4, 128], BF16)
        t_STx = P.toks(2, "STx")
        arena2 = sb("arena2", [128, 2048], F32)
        hacc = arena2[:, :].rearrange("p (a b) -> p a b", a=4)
        t_hacc = P.toks(4, "hacc")
        hmT = arena[:, 8192:12288].rearrange("p (a b) -> p a b", a=8)
        t_hmT = P.toks(4, "hmT")
        t_fmT = P.tok("fmT")
        small = sb("small", [128, 64], F32)
        t_small = P.tok("small")
        xt = ptmp
        t_xt = t_ptmp
        xtb = sb("xtb", [128, 1, D], BF16)
        t_xtb = P.toks(1, "xtb")
        dtab = sb("dtab", [128, 2, 2, 512], BF16)
        t_dtab = P.toks(2, "dtab")
        p12 = sb("p12", [128, 2, 2, 512], BF16)
        t_p12 = P.toks(2, "p12")

        u2T = sb("u2T", [128, 8, 1024], BF16)
        t_u2T = P.toks(8, "u2T")
        u2f = sb("u2f", [128, 8, 128], F32)
        t_u2f = P.tok("u2f")
        Gall = sb("Gall", [128, 8, NE], F32)
        t_G = P.toks(8, "G")
        arena3 = sb("arena3", [128, 1032], F32)
        GT = arena3[0:NE, 0:1024]
        t_GT = P.toks(8, "GT")
        WB_N = 2
        wb = [sb(f"wb{i}", [128, 8, 512], BF16) for i in range(WB_N)]
        t_wb = P.toks(WB_N, "wb")
        wb_ctr = [0]
        gsb = arena[:, 8192:12288].rearrange("p (a b) -> p a b", a=4)
        t_gs = P.toks(8, "gs")
        actT = arena[:, 0:8192].rearrange("p (a b) -> p a b", a=8)
        t_actT = [P.toks(2, "actT") for _ in range(8)]
        tmpA = arena2[:, 0:1024].rearrange("p (a b) -> p a b", a=2)
        t_tmpA = P.toks(2, "tmpA")
        tmpB = sb("tmpB", [128, 2, 512], BF16)
        t_tmpB = P.toks(2, "tmpB")
        tmpC = arena2[:, 1024:2048].rearrange("p (a b) -> p a b", a=2)
        t_tmpC = P.toks(2, "tmpC")

        SCALE_K = DH ** -0.5

        def load_cast(dst, t_dst, src2d, ncols, scale_ap=None, scale_rows=0):
            c0 = 0
            while c0 < ncols:
                w = min(256, ncols - c0)
                s_t, s_k = stage_load_w(src2d, c0, w)
                eng = cast_eng()
                if scale_ap is None:
                    do_copy(eng, dst[:, :, c0:c0 + w], s_t[:, :, 0:w], [s_k], [t_dst])
                else:
                    for kc in range(8):
                        if kc < scale_rows:
                            P.op("pool", lambda e, kc=kc, c0=c0, w=w, s_t=s_t: e.tensor_scalar(
                                out=dst[:, kc, c0:c0 + w], in0=s_t[:, kc, 0:w], scalar1=scale_ap[:, kc:kc + 1], scalar2=None,
                                op0=ALU.mult), reads=[s_k, t_nwT], writes=[t_dst])
                        else:
                            do_copy("pool", dst[:, kc, c0:c0 + w], s_t[:, kc, 0:w], [s_k], [t_dst])
                c0 += w

        def stage_load_w(src2d, c0, w):
            i = stg_ctr[0] % STG_N
            stg_ctr[0] += 1
            P.dma("sp", lambda e: e.dma_start(out=stg[i][:, :, 0:w], in_=src2d[:, c0:c0 + w].rearrange("(kc p) c -> p kc c", p=128)),
                  writes=[t_stg[i]])
            return stg[i], t_stg[i]

        BIG = 30000.0

        def gate_math(pg, t_pg, slot):
            G_ = gt[:, slot]
            tk = t_gt[slot]
            P.op("dve", lambda e: e.tensor_tensor(out=small[:, 0:16], in0=pg, in1=gbias[:], op=ALU.add),
                 reads=[t_pg, t_gbias], writes=[t_small])
            P.op("act", lambda e: e.activation(out=small[:, 16:20], in_=small[:, 4:8], func=AF.Exp, scale=-1.0), reads=[t_small], writes=[t_small])
            P.op("act", lambda e: e.activation(out=small[:, 20:24], in_=small[:, 12:16], func=AF.Exp, scale=-1.0), reads=[t_small], writes=[t_small])
            P.op("act", lambda e: e.activation(out=small[:, 24:32], in_=small[:, 16:24], func=AF.Ln, bias=1.0), reads=[t_small], writes=[t_small])
            P.op("dve", lambda e: e.tensor_scalar(out=small[:, 48:56], in0=small[:, 24:32], scalar1=-1.0, scalar2=None, op0=ALU.mult),
                 reads=[t_small], writes=[t_small])
            P.op("dve", lambda e: e.tensor_copy(out=small[:, 56:60], in_=small[:, 0:4]), reads=[t_small], writes=[t_small])
            P.op("dve", lambda e: e.tensor_copy(out=small[:, 60:64], in_=small[:, 8:12]), reads=[t_small], writes=[t_small])
            pb, tpb = bank()
            P.op("pe", lambda e: e.matmul(pb[:, 0:4], lhsT=triU[:], rhs=small[:, 48:52], start=True, stop=True), reads=[t_small, t_triU], writes=[tpb])
            P.op("pe", lambda e: e.matmul(pb[:, 4:8], lhsT=triL[:], rhs=small[:, 52:56], start=True, stop=True), reads=[t_small, t_triL], writes=[tpb])
            P.op("pe", lambda e: e.matmul(pb[:, 8:16], lhsT=ones_f[:], rhs=small[:, 48:56], start=True, stop=True), reads=[t_small, t_ones], writes=[tpb])
            P.op("dve", lambda e: e.tensor_tensor(out=G_[:, 0, :], in0=small[:, 56:64], in1=pb[:, 0:8], op=ALU.subtract), reads=[t_small, tpb], writes=[tk])
            P.op("act", lambda e: e.copy(out=G_[:, 1, :], in_=pb[:, 0:8]), reads=[tpb], writes=[tk])
            P.op("act", lambda e: e.copy(out=G_[:, 2, :], in_=pb[:, 8:16]), reads=[tpb], writes=[tk])
            for dr in range(2):
                for h in range(4):
                    P.op("dve", lambda e, dr=dr, h=h: e.tensor_scalar(out=hnb[:, h * 128:(h + 1) * 128], in0=ident_f[:], scalar1=G_[:, 0, dr * 4 + h:dr * 4 + h + 1], scalar2=None, op0=ALU.mult),
                         reads=[tk, t_idf], writes=[t_hnb])
                pq, tpq = bank()
                P.op("pe", lambda e, pq=pq: e.matmul(pq[:, 0:512], lhsT=ones_f[:], rhs=hnb[:, 0:512], start=True, stop=True), reads=[t_hnb, t_ones], writes=[tpq])
                P.op("dve", lambda e, dr=dr, pq=pq: e.tensor_reduce(out=G_[:, 3, dr * 4:dr * 4 + 4], in_=pq[:, 0:512].rearrange("p (h s) -> p h s", h=4), axis=mybir.AxisListType.X, op=ALU.max),
                     reads=[tpq], writes=[tk])
                nm_, tnm_ = (NML4, t_NML4) if dr == 0 else (NMU4, t_NMU4)
                P.op("dve", lambda e, pq=pq, nm_=nm_: e.tensor_tensor(out=scr[:, 0:512], in0=pq[:, 0:512], in1=nm_[:].rearrange("p h s -> p (h s)"), op=ALU.add),
                     reads=[tpq, tnm_], writes=[t_scr])
                P.op("dve", lambda e, dr=dr: e.tensor_reduce(out=G_[:, 5, dr * 4:dr * 4 + 4], in_=scr[:, 0:512].rearrange("p (h s) -> p h s", h=4), axis=mybir.AxisListType.X, op=ALU.max),
                     reads=[t_scr], writes=[tk])
            P.op("dve", lambda e: e.tensor_tensor(out=small[:, 40:48], in0=G_[:, 0, :], in1=G_[:, 3, :], op=ALU.subtract), reads=[tk], writes=[t_small])
            P.op("act", lambda e: e.activation(out=G_[:, 4, :], in_=small[:, 40:48], func=AF.Exp), reads=[t_small], writes=[tk])

        def scaled_v(slot, dr):
            for h in range(4):
                P.op("act", lambda e, h=h: e.activation(
                    out=vsc[:, dr, h, :], in_=vext[:, slot, h, :], func=AF.Identity, scale=gt[:, slot, 4, dr * 4 + h:dr * 4 + h + 1]),
                    reads=[t_vext[slot], t_gt[slot]], writes=[t_vsc])

        def d_matmuls(slot, dr):
            res = []
            pbs = [bank() for _ in range(2)]
            for h in range(4):
                pb, tpb = pbs[h // 3]
                o = (h % 3) * 132
                P.op("pe", lambda e, pb=pb, o=o, h=h: e.matmul(pb[:, o:o + 129], lhsT=ktm[:, slot, h * 128:(h + 1) * 128], rhs=vsc[:, dr, h, :], start=True, stop=True),
                     reads=[t_ktm[slot], t_vsc], writes=[tpb])
                res.append((pb[:, o:o + 129], tpb))
            return res

        def state_update(slot, dr, mask_col=None, t_mask=None):
            scaled_v(slot, dr)
            Dl = d_matmuls(slot, dr)
            c0 = dr * 4
            apply_update(dr, Dl, gt[:, slot, 3, c0:c0 + 4], gt[:, slot, 2, c0:c0 + 4], t_gt[slot], mask_col, t_mask)

        def apply_update(dr, Dl, Gm, BLc, t_src, mask_col=None, t_mask=None):
            c0 = dr * 4
            mcur = mst[:, c0:c0 + 4]
            if mask_col is not None:
                P.op("dve", lambda e: e.tensor_scalar(out=sm2[:, 0:4], in0=Gm, scalar1=BIG, scalar2=mask_col, op0=ALU.add, op1=ALU.mult), reads=[t_src, t_mask], writes=[t_sm2])
                P.op("dve", lambda e: e.tensor_scalar(out=sm2[:, 0:4], in0=sm2[:, 0:4], scalar1=-BIG, scalar2=None, op0=ALU.add), reads=[t_sm2], writes=[t_sm2])
                P.op("dve", lambda e: e.tensor_scalar(out=sm2[:, 4:8], in0=BLc, scalar1=mask_col, scalar2=None, op0=ALU.mult), reads=[t_src, t_mask], writes=[t_sm2])
            else:
                P.op("dve", lambda e: e.tensor_copy(out=sm2[:, 0:4], in_=Gm), reads=[t_src], writes=[t_sm2])
                P.op("dve", lambda e: e.tensor_copy(out=sm2[:, 4:8], in_=BLc), reads=[t_src], writes=[t_sm2])
            P.op("dve", lambda e: e.tensor_tensor(out=sm2[:, 8:12], in0=mcur, in1=sm2[:, 0:4], op=ALU.max), reads=[t_mst, t_sm2], writes=[t_sm2])
            P.op("dve", lambda e: e.tensor_tensor(out=sm2[:, 12:16], in0=mcur, in1=sm2[:, 8:12], op=ALU.subtract), reads=[t_mst, t_sm2], writes=[t_sm2])
            P.op("dve", lambda e: e.tensor_tensor(out=sm2[:, 16:20], in0=sm2[:, 0:4], in1=sm2[:, 8:12], op=ALU.subtract), reads=[t_sm2], writes=[t_sm2])
            P.op("act", lambda e: e.activation(out=sm2[:, 12:20], in_=sm2[:, 12:20], func=AF.Exp), reads=[t_sm2], writes=[t_sm2])
            for h in range(4):
                u = c0 + h
                dps, tdps = Dl[h]
                P.op("dve", lambda e, u=u, h=h: e.tensor_scalar(out=Sst[:, u, :], in0=Sst[:, u, :], scalar1=sm2[:, 12 + h:13 + h], scalar2=None, op0=ALU.mult),
                     reads=[t_S[u], t_sm2], writes=[t_S[u]])
                P.op("dve", lambda e, u=u, h=h, dps=dps: e.scalar_tensor_tensor(out=Sst[:, u, :], in0=dps, scalar=sm2[:, 16 + h:17 + h], in1=Sst[:, u, :], op0=ALU.mult, op1=ALU.add),
                     reads=[tdps, t_S[u], t_sm2], writes=[t_S[u]])
                P.op("act", lambda e, u=u: e.copy(out=Sbf[:, u, :], in_=Sst[:, u, :]), reads=[t_S[u]], writes=[t_Sbf[u]])
            P.op("dve", lambda e: e.tensor_tensor(out=mcur, in0=sm2[:, 4:8], in1=sm2[:, 8:12], op=ALU.add), reads=[t_sm2], writes=[t_mst])

        def make_uT(src_f32, t_src, cond, vec_shift, vec_scale, dst_fn, t_dst, want_f32=None, t_f32=None):
            xb_i = 0
            P.op("dve", lambda e: e.tensor_copy(out=xtb[:, xb_i, :], in_=src_f32), reads=[t_src], writes=[t_xtb[xb_i]])
            for half in range(2):
                pb, tpb = bank()
                pbb = pb[:].bitcast(BF16)
                for q in range(4):
                    kc = half * 4 + q
                    P.op("pe", lambda e, q=q, kc=kc, pbb=pbb: e.transpose(pbb[:, q * 128:(q + 1) * 128], xtb[:, xb_i, kc * 128:(kc + 1) * 128], ident_b[:]),
                         reads=[t_xtb[xb_i], t_idb], writes=[tpb])
                for q in range(4):
                    kc = half * 4 + q
                    P.op("act", lambda e, q=q, kc=kc, pbb=pbb: e.activation(
                        out=dst_fn(kc), in_=pbb[:, q * 128:(q + 1) * 128], func=AF.Identity,
                        scale=modT[:, vec_scale, kc, cond:cond + 1], bias=modT[:, vec_shift, kc, cond:cond + 1]),
                        reads=[tpb, t_modT], writes=[t_dst])

        def load_wpiece(src2d, c0, ncols, row_scale=False, eng=None, into=None):
            if into is None:
                i = wb_ctr[0] % WB_N
                wb_ctr[0] += 1
                buf, tk = wb[i], t_wb[i]
            else:
                buf, tk = into
            cc = 0
            while cc < ncols:
                w = min(256, ncols - cc)
                s_t, s_k = stage_load_w(src2d, c0 + cc, w)
                if not row_scale:
                    do_copy(eng or cast_eng(), buf[:, :, cc:cc + w], s_t[:, :, 0:w], [s_k], [tk])
                else:
                    do_copy("act", buf[:, 4:8, cc:cc + w], s_t[:, 4:8, 0:w], [s_k], [tk])
                    for kc in range(4):
                        P.op("dve", lambda e, kc=kc, cc=cc, w=w, s_t=s_t, buf=buf: e.tensor_scalar(
                            out=buf[:, kc, cc:cc + w], in0=s_t[:, kc, 0:w], scalar1=nwT[:, kc:kc + 1], scalar2=None, op0=ALU.mult),
                            reads=[s_k, t_nwT], writes=[tk])
                cc += w
            return buf, tk

        sm2 = sb("sm2", [128, 32], F32)
        t_sm2 = P.tok("sm2")
        st6 = sb("st6", [128, 4, 6], F32)
        t_st6 = P.tok("st6")
        Cout = arena3[:, :].rearrange("p (a b) -> p a b", a=8)
        t_Cout = P.tok("Cout")
        hnb = sb("hnb", [128, 512], F32)
        t_hnb = P.tok("hnb")
        hmb = sb("hmb", [128, 512], BF16)
        t_hmb = P.tok("hmb")
        lg = sb("lg", [128, 4, NE], F32)
        t_lg = P.tok("lg")
        P.op("pool", lambda e: e.memset(vext[:, :, :, 128:129], 1.0), writes=t_vext)

        def layer_norm_inplace(xi, tw, tb):
            xv = X[:, xi, :]
            for hf in range(2):
                P.op("dve", lambda e, hf=hf: e.bn_stats(out=st6[:, hf, :], in_=X[:, xi, hf * 512:(hf + 1) * 512]), reads=[tX[xi]], writes=[t_st6])
            P.op("dve", lambda e: e.bn_aggr(out=sm2[:, 0:2], in_=st6[:, 0:2, :].rearrange("p a b -> p (a b)")), reads=[t_st6], writes=[t_sm2])
            P.op("act", lambda e: e.activation(out=sm2[:, 2:3], in_=sm2[:, 1:2], func=AF.Sqrt, bias=eps_t[:], scale=1.0), reads=[t_sm2, t_ones], writes=[t_sm2])
            P.op("dve", lambda e: e.reciprocal(out=sm2[:, 3:4], in_=sm2[:, 2:3]), reads=[t_sm2], writes=[t_sm2])
            P.op("dve", lambda e: e.scalar_tensor_tensor(out=sm2[:, 4:5], in0=sm2[:, 0:1], scalar=-1.0, in1=sm2[:, 3:4], op0=ALU.mult, op1=ALU.mult),
                 reads=[t_sm2], writes=[t_sm2])
            P.op("act", lambda e: e.activation(out=xv, in_=xv, func=AF.Identity, scale=sm2[:, 3:4], bias=sm2[:, 4:5]), reads=[tX[xi], t_sm2], writes=[tX[xi]])
            P.op("dve", lambda e: e.tensor_tensor(out=xv, in0=xv, in1=lnw[:], op=ALU.mult), reads=[tX[xi], tw], writes=[tX[xi]])
            P.op("dve", lambda e: e.tensor_tensor(out=xv, in0=xv, in1=lnb[:], op=ALU.add), reads=[tX[xi], tb], writes=[tX[xi]])

        def mod_vectors(l, vecs):
            pmT, t_pmT = PLONG, t_PLONG
            for v in vecs:
                for sub in range(4):
                    pc = v * 4 + sub
                    s_t, s_k = stage_load_w(DI["w_ada"][l], pc * 256, 256)
                    pr, tpr = bank()
                    for kc in range(8):
                        P.op("pe", lambda e, kc=kc, s_t=s_t, pr=pr: e.matmul(pr[0:2, 0:256], lhsT=sT[:, 2 * kc:2 * kc + 2], rhs=s_t[:, kc, :],
                                                                            start=(kc == 0), stop=(kc == 7)), reads=[s_k, t_sT], writes=[tpr])
                    mr = tmpA[0:2, pc % 2, 0:256]
                    tmr = t_tmpA[pc % 2]
                    bp = tmpC[0:2, pc % 2, 0:256]
                    tbp = t_tmpC[pc % 2]
                    P.dma("sp", lambda e, pc=pc, bp=bp: e.dma_start(out=bp, in_=DI["b_ada2"][l][:, pc * 256:(pc + 1) * 256]), writes=[tbp])
                    P.op("dve", lambda e, pr=pr, mr=mr, bp=bp: e.tensor_tensor(out=mr, in0=pr[0:2, 0:256], in1=bp, op=ALU.add),
                         reads=[tpr, tbp], writes=[tmr])
                    if v in (2, 5):
                        for cond in range(2):
                            pg_, tpg_ = bank()
                            P.op("pe", lambda e, cond=cond, pg_=pg_, mr=mr: e.matmul(pg_[:, 0:256], lhsT=selr[0:2, cond, :], rhs=mr, start=True, stop=True),
                                 reads=[tmr, t_selr], writes=[tpg_])
                            P.op("act", lambda e, cond=cond, sub=sub, pg_=pg_: e.copy(out=gb[:, cond, sub * 256:(sub + 1) * 256], in_=pg_[:, 0:256]),
                                 reads=[tpg_], writes=[t_gb])
                    else:
                        for q in range(2):
                            ch = sub * 2 + q
                            c0 = (v * 8 + ch) * 2
                            P.op("pe", lambda e, c0=c0, q=q, mr=mr: e.transpose(pmT[:, c0:c0 + 2], mr[:, q * 128:(q + 1) * 128], ident_f[0:2, 0:2]),
                                 reads=[tmr, t_idf], writes=[t_pmT])
            for v in vecs:
                if v in (2, 5):
                    continue
                if v in (1, 4):
                    P.op("dve", lambda e, v=v: e.tensor_scalar(out=modT[:, v].rearrange("p a b -> p (a b)"), in0=pmT[:, v * 16:(v + 1) * 16], scalar1=1.0, scalar2=None, op0=ALU.add),
                         reads=[t_pmT], writes=[t_modT])
                else:
                    P.op("dve", lambda e, v=v: e.tensor_copy(out=modT[:, v].rearrange("p a b -> p (a b)"), in_=pmT[:, v * 16:(v + 1) * 16]),
                         reads=[t_pmT], writes=[t_modT])

        def tm_proj(slot, wbuf, twb, ncols, evac):
            pb, tpb = bank()
            for kc in range(8):
                P.op("pe", lambda e, kc=kc, pb=pb, wbuf=wbuf: e.matmul(pb[:, 0:ncols], lhsT=uT[:, kc, slot * 128:(slot + 1) * 128], rhs=wbuf[:, kc, 0:ncols],
                                                                      start=(kc == 0), stop=(kc == 7)), reads=[t_uT[slot], twb], writes=[tpb])
            evac(pb, tpb)

        gpc = sb("gpc", [128, 8, 16], BF16)
        t_gpc = P.tok("gpc")
        zpc = arena[:, 8192:12288].rearrange("p (a b) -> p a b", a=8)
        t_zpc = P.tok("zpc")

        cur_l = [0]
        t_dstg = P.toks(2, "dstg")
        okey = {n: P.tok("o_" + n) for n in ("yp", "ys", "nC", "nn", "nm")}
        out_toks.extend(okey.values())

        def process_item(l, tiles, cond, is_sample, seq_out):
            T = len(tiles)
            N = 128 * T
            for tau, xi in enumerate(tiles):
                make_uT(X[:, xi, :], tX[xi], cond, 0, 1, lambda kc, tau=tau: uT[:, kc, tau * 128:(tau + 1) * 128], t_uT[tau])
            def ev_k(pb, tpb, tau):
                P.op("act", lambda e: e.mul(out=ktm[:, tau, :], in_=pb[:, 0:512], mul=SCALE_K), reads=[tpb], writes=[t_ktm[tau]])

            def ev_v(pb, tpb, tau):
                P.op("dve", lambda e: e.tensor_copy(out=vext[:, tau, :, 0:128], in_=pb[:, 0:512].rearrange("p (h d) -> p h d", h=4)),
                     reads=[tpb], writes=[t_vext[tau]])

            def ev_o(pb, tpb, tau):
                P.op("act", lambda e: e.activation(out=og[:, tau, :], in_=pb[:, 0:512], func=AF.Sigmoid), reads=[tpb], writes=[t_og[tau]])

            def ev_g(pb, tpb, tau):
                gate_math(pb[:, 0:16], tpb, tau)

            def ev_z(pb, tpb, tau):
                P.op("act", lambda e: e.copy(out=zown[:, tau, :], in_=pb[:, 0:512]), reads=[tpb], writes=[t_zown[tau]])

            plist = [("q", 0, 512, None), ("k", 512, 512, ev_k), ("v", 1024, 512, ev_v), ("o", 1536, 512, ev_o), ("g", G_OFF, 16, ev_g)]
            if not is_sample:
                plist.append(("z", F_OFF, 512, ev_z))
            nxt = load_wpiece(DI["w_in"][l], plist[0][1], plist[0][2])
            for pi, (pname, col0, ncols, evac) in enumerate(plist):
                wbuf, twb = nxt
                if pi + 1 < len(plist):
                    nxt = load_wpiece(DI["w_in"][l], plist[pi + 1][1], plist[pi + 1][2])
                if pname in ("q", "k"):
                    for h in range(4):
                        pb, tpb = bank()
                        for kc in range(8):
                            P.op("pe", lambda e, kc=kc, h=h, pb=pb, wbuf=wbuf: e.matmul(pb[:, 0:N], lhsT=wbuf[:, kc, h * 128:(h + 1) * 128], rhs=uT[:, kc, 0:N],
                                                                                       start=(kc == 0), stop=(kc == 7)), reads=t_uT[0:T] + [twb], writes=[tpb])
                        if pname == "q":
                            P.op("act", lambda e, h=h, pb=pb: e.copy(out=qT[:, h, 0:N], in_=pb[:, 0:N]), reads=[tpb], writes=[t_qT])
                        else:
                            P.op("act", lambda e, h=h, pb=pb: e.mul(out=kT[:, h, 0:N], in_=pb[:, 0:N], mul=SCALE_K), reads=[tpb], writes=[t_kT])
                if evac is not None:
                    for tau in range(T):
                        tm_proj(tau, wbuf, twb, ncols, lambda pb, tpb, tau=tau, evac=evac: evac(pb, tpb, tau))
            if not is_sample:
                P.op("pool", lambda e: e.memset(Sst[:], 0.0), writes=t_S)
                P.op("pool", lambda e: e.memset(Sbf[:], 0.0), writes=t_Sbf)
                P.op("pool", lambda e: e.memset(mst[:], 0.0), writes=[t_mst])
            else:
                for u in range(8):
                    P.op("act", lambda e, u=u: e.copy(out=Sbf[:, u, :], in_=Sst[:, u, :]), reads=[t_S[u]], writes=[t_Sbf[u]])
            for dr in range(2):
                order = list(range(T)) if dr == 0 else list(range(T - 1, -1, -1))
                nm_, tnm_ = (NMU4, t_NMU4) if dr == 0 else (NML4, t_NML4)
                c0 = dr * 4
                for oi, tau in enumerate(order):
                    sx = (dr * T + oi) % 2
                    G_ = gt[:, tau]
                    P.op("dve", lambda e, G_=G_: e.tensor_tensor(out=sm2[:, 20:24], in0=G_[:, 5, c0:c0 + 4], in1=mst[:, c0:c0 + 4], op=ALU.max), reads=[t_gt[tau], t_mst], writes=[t_sm2])
                    P.op("dve", lambda e: e.tensor_tensor(out=sm2[:, 24:28], in0=mst[:, c0:c0 + 4], in1=sm2[:, 20:24], op=ALU.subtract), reads=[t_mst, t_sm2], writes=[t_sm2])
                    P.op("dve", lambda e, G_=G_: e.scalar_tensor_tensor(out=sm2[:, 28:32], in0=G_[:, 1, c0:c0 + 4], scalar=-1.0, in1=sm2[:, 20:24], op0=ALU.mult, op1=ALU.subtract),
                         reads=[t_gt[tau], t_sm2], writes=[t_sm2])
                    P.op("act", lambda e: e.activation(out=sm2[:, 24:32], in_=sm2[:, 24:32], func=AF.Exp), reads=[t_sm2], writes=[t_sm2])
                    for h in range(4):
                        P.op("dve", lambda e, h=h: e.tensor_scalar(out=hnb[:, h * 128:(h + 1) * 128], in0=ident_f[:], scalar1=sm2[:, 20 + h:21 + h], scalar2=-1.0, op0=ALU.mult, op1=ALU.mult),
                             reads=[t_sm2, t_idf], writes=[t_hnb])
                    pr_, tpr_ = bank()
                    P.op("pe", lambda e, pr_=pr_: e.matmul(pr_[:, 0:512], lhsT=ones_f[:], rhs=hnb[:, 0:512], start=True, stop=False), reads=[t_hnb, t_ones], writes=[tpr_])
                    P.op("pe", lambda e, pr_=pr_, nm_=nm_: e.matmul(pr_[:, 0:512], lhsT=ident_f[:], rhs=nm_[:].rearrange("p h s -> p (h s)"), start=False, stop=True),
                         reads=[tnm_, t_idf], writes=[tpr_])
                    for h in range(4):
                        P.op("act", lambda e, h=h, pr_=pr_, G_=G_: e.activation(out=scr[:, h * 128:(h + 1) * 128], in_=pr_[:, h * 128:(h + 1) * 128], func=AF.Exp,
                                                                           bias=G_[:, 0, c0 + h:c0 + h + 1], scale=1.0), reads=[tpr_, t_gt[tau]], writes=[t_scr])
                    pst, tpst = bank()
                    for h in range(4):
                        P.op("pe", lambda e, h=h, pst=pst: e.matmul(pst[:, h * 128:(h + 1) * 128], lhsT=kT[:, h, tau * 128:(tau + 1) * 128],
                                                                   rhs=qT[:, h, tau * 128:(tau + 1) * 128], start=True, stop=True),
                             reads=[t_kT, t_qT], writes=[tpst])
                    P.op("dve", lambda e, pst=pst: e.tensor_tensor(out=STx[:, sx].rearrange("p h t -> p (h t)"), in0=pst[:, 0:512], in1=scr[:, 0:512], op=ALU.mult),
                         reads=[tpst, t_scr], writes=[t_STx[sx]])
                    for h in range(4):
                        u = c0 + h
                        ni = h % 2
                        po, tpo = bank()
                        P.op("pe", lambda e, h=h, po=po: e.matmul(po[:, 0:129], lhsT=STx[:, sx, h, :], rhs=vext[:, tau, h, :], start=True, stop=True),
                             reads=[t_STx[sx], t_vext[tau]], writes=[tpo])
                        P.op("pe", lambda e, h=h, u=u, po=po: e.matmul(po[:, 132:261], lhsT=qT[:, h, tau * 128:(tau + 1) * 128], rhs=Sbf[:, u, :], start=True, stop=True),
                             reads=[t_qT, t_Sbf[u]], writes=[tpo])
                        P.op("act", lambda e, h=h, po=po, ni=ni: e.activation(out=nd[:, ni, :], in_=po[:, 132:261], func=AF.Identity, scale=sm2[:, 24 + h:25 + h]),
                             reads=[tpo, t_sm2], writes=[t_nd[ni]])
                        P.op("dve", lambda e, po=po, ni=ni: e.tensor_tensor(out=nd[:, ni, :], in0=po[:, 0:129], in1=nd[:, ni, :], op=ALU.add), reads=[tpo, t_nd[ni]], writes=[t_nd[ni]])
                        P.op("dve", lambda e, ni=ni: e.scalar_tensor_tensor(out=sm2[:, 8:9], in0=nd[:, ni, 128:129], scalar=-1.0, in1=nd[:, ni, 128:129], op0=ALU.mult, op1=ALU.max),
                             reads=[t_nd[ni]], writes=[t_sm2])
                        P.op("dve", lambda e, h=h: e.tensor_tensor(out=sm2[:, 9:10], in0=sm2[:, 8:9], in1=sm2[:, 28 + h:29 + h], op=ALU.max), reads=[t_sm2], writes=[t_sm2])
                        P.op("dve", lambda e: e.reciprocal(out=sm2[:, 10:11], in_=sm2[:, 9:10]), reads=[t_sm2], writes=[t_sm2])
                        if dr == 0:
                            P.op("dve", lambda e, h=h, ni=ni: e.tensor_scalar(out=hacc[:, tau, h * 128:(h + 1) * 128], in0=nd[:, ni, 0:128], scalar1=sm2[:, 10:11], scalar2=None, op0=ALU.mult),
                                 reads=[t_nd[ni], t_sm2], writes=[t_hacc[tau]])
                        else:
                            P.op("dve", lambda e, h=h, ni=ni: e.scalar_tensor_tensor(out=hacc[:, tau, h * 128:(h + 1) * 128], in0=nd[:, ni, 0:128], scalar=sm2[:, 10:11],
                                                                                 in1=hacc[:, tau, h * 128:(h + 1) * 128], op0=ALU.mult, op1=ALU.add),
                                 reads=[t_nd[ni], t_sm2, t_hacc[tau]], writes=[t_hacc[tau]])
                    if (not is_sample) or oi < T - 1:
                        state_update(tau, dr)
                if _DBG and l == 0 and seq_out == 1:
                    tkh = P.tok("dbgh")
                    out_toks.append(tkh)
                    P.dma("sp", lambda e, dr=dr: e.dma_start(out=DO["dbg_h1" if dr == 0 else "dbg_h2"], in_=hacc[:, 0:2, :]), reads=t_hacc[0:2], writes=[tkh])
                    if dr == 0:
                        for nm2, src, rd in [("dbg_stx", STx[:], t_STx), ("dbg_scr", scr[:], [t_scr]), ("dbg_nd", nd[:], t_nd), ("dbg_sm2", sm2[:], [t_sm2])]:
                            tkq = P.tok(nm2)
                            out_toks.append(tkq)
                            P.dma("sp", lambda e, nm2=nm2, src=src: e.dma_start(out=DO[nm2], in_=src), reads=rd, writes=[tkq])
                        tkq = P.tok("dbgqk")
                        out_toks.append(tkq)
                        P.dma("sp", lambda e: e.dma_start(out=DO["dbg_qk"][:, 0], in_=qT[:, :, 0:256]), reads=[t_qT], writes=[tkq])
                        P.dma("sp", lambda e: e.dma_start(out=DO["dbg_qk"][:, 1], in_=kT[:, :, 0:256]), reads=[t_kT], writes=[tkq])
                        tkg = P.tok("dbgg")
                        out_toks.append(tkg)
                        P.dma("sp", lambda e: e.dma_start(out=DO["dbg_gt"], in_=gt[:, 0:2].rearrange("p a b c -> p a (b c)")), reads=t_gt[0:2], writes=[tkg])
            if not is_sample:
                P.dma("sp", lambda e: e.dma_start(out=DO["nm"][seq_out, l].rearrange("(o u) -> o u", o=1), in_=mst[0:1, 0:8]), reads=[t_mst], writes=[], key=okey["nm"])
                P.dma("sp", lambda e: e.dma_start(out=DO["nC"][seq_out, l].rearrange("u d e -> d u e"), in_=Sst[:, :, 0:128]), reads=t_S, writes=[], key=okey["nC"])
                P.dma("sp", lambda e: e.dma_start(out=DO["nn"][seq_out, l].rearrange("u d -> d u"), in_=Sst[:, :, 128], allow_slow_non_contiguous=True), reads=t_S, writes=[], key=okey["nn"])
            for tau in range(T):
                for h in range(4):
                    P.op("dve", lambda e, h=h: e.bn_stats(out=st6[:, h, :], in_=hacc[:, tau, h * 128:(h + 1) * 128]), reads=[t_hacc[tau]], writes=[t_st6])
                for h in range(4):
                    P.op("dve", lambda e, h=h: e.bn_aggr(out=sm2[:, 2 * h:2 * h + 2], in_=st6[:, h, :]), reads=[t_st6], writes=[t_sm2])
                P.op("act", lambda e: e.activation(out=sm2[:, 8:12], in_=sm2[:, 0:8].rearrange("p (h t) -> p h t", t=2)[:, :, 1], func=AF.Sqrt, bias=eps_t[:], scale=1.0),
                     reads=[t_sm2, t_ones], writes=[t_sm2])
                P.op("dve", lambda e: e.reciprocal(out=sm2[:, 12:16], in_=sm2[:, 8:12]), reads=[t_sm2], writes=[t_sm2])
                P.op("dve", lambda e: e.scalar_tensor_tensor(out=sm2[:, 16:20], in0=sm2[:, 0:8].rearrange("p (h t) -> p h t", t=2)[:, :, 0], scalar=-1.0, in1=sm2[:, 12:16],
                                                             op0=ALU.mult, op1=ALU.mult), reads=[t_sm2], writes=[t_sm2])
                for h in range(4):
                    P.op("act", lambda e, h=h: e.activation(out=hnb[:, h * 128:(h + 1) * 128], in_=hacc[:, tau, h * 128:(h + 1) * 128], func=AF.Identity,
                                                            scale=sm2[:, 12 + h:13 + h], bias=sm2[:, 16 + h:17 + h]), reads=[t_hacc[tau], t_sm2], writes=[t_hnb])
                P.op("dve", lambda e: e.tensor_tensor(out=hmb[:], in0=hnb[:], in1=og[:, tau, :], op=ALU.mult), reads=[t_hnb, t_og[tau]], writes=[t_hmb])
                pb, tpb = bank()
                pbb = pb[:].bitcast(BF16)
                for h in range(4):
                    P.op("pe", lambda e, h=h, pbb=pbb: e.transpose(pbb[:, h * 128:(h + 1) * 128], hmb[:, h * 128:(h + 1) * 128], ident_b[:]), reads=[t_hmb, t_idb], writes=[tpb])
                P.op("act", lambda e, pbb=pbb: e.copy(out=hmT[:, 0:4, tau * 128:(tau + 1) * 128], in_=pbb[:, 0:512].rearrange("p (h t) -> p h t", h=4)),
                     reads=[tpb], writes=[t_hmT[tau]])
            if not is_sample:
                for g in range(4):
                    pbs = []
                    for cs, (tab, ttab) in enumerate(((c256, t_c256), (s256, t_s256))):
                        pb, tpb = bank()
                        for mc in range(2):
                            P.op("pe", lambda e, mc=mc, g=g, pb=pb, tab=tab: e.matmul(pb[:, 0:256], lhsT=zown[:, mc, g * 128:(g + 1) * 128], rhs=tab[:, mc, :],
                                                                                     start=(mc == 0), stop=(mc == 1)), reads=[t_zown[mc], ttab], writes=[tpb])
                        P.op("act" if cs else "dve", (lambda e, cs=cs, g=g, pb=pb: e.copy(out=p12[:, g % 2, cs, 0:256], in_=pb[:, 0:256])) if cs else
                             (lambda e, cs=cs, g=g, pb=pb: e.tensor_copy(out=p12[:, g % 2, cs, 0:256], in_=pb[:, 0:256])), reads=[tpb], writes=[t_p12[g % 2]])
                    py, tpy = bank()
                    P.op("pe", lambda e, g=g, py=py: e.matmul(py[:, 0:256], lhsT=consts["ccp"][0][:], rhs=p12[:, g % 2, 0, 0:256], start=True, stop=False),
                         reads=[t_p12[g % 2], consts["ccp"][1]], writes=[tpy])
                    P.op("pe", lambda e, g=g, py=py: e.matmul(py[:, 0:256], lhsT=consts["nscp"][0][:], rhs=p12[:, g % 2, 1, 0:256], start=False, stop=True),
                         reads=[t_p12[g % 2], consts["nscp"][1]], writes=[tpy])
                    P.op("act", lambda e, g=g, py=py: e.copy(out=hmT[:, 4 + g, 0:256], in_=py[:, 0:256]), reads=[tpy], writes=[t_fmT] + t_hmT[0:2])
            else:
                for gp in range(2):
                    pbs = [bank() for _ in range(4)]
                    for mc in range(16):
                        bi = mc % 2
                        P.dma("sp", lambda e, mc=mc, bi=bi: e.dma_start(out=dtab[:, bi, 0, :], in_=DI["cn_own"][mc * 128:(mc + 1) * 128, :]), writes=[t_dtab[bi]])
                        P.dma("sp", lambda e, mc=mc, bi=bi: e.dma_start(out=dtab[:, bi, 1, :], in_=DI["sn_own"][mc * 128:(mc + 1) * 128, :]), writes=[t_dtab[bi]])
                        P.dma("sp", lambda e, mc=mc, bi=bi: e.dma_start(out=zst[:, bi, :], in_=zscr.ap()[mc * 128:(mc + 1) * 128, :]), reads=[t_zscr], writes=[t_zst[bi]])
                        for gl in range(2):
                            g = gp * 2 + gl
                            for cs in range(2):
                                pb, tpb = pbs[gl * 2 + cs]
                                P.op("pe", lambda e, mc=mc, g=g, cs=cs, pb=pb, bi=bi: e.matmul(pb[:, 0:512], lhsT=zst[:, bi, g * 128:(g + 1) * 128], rhs=dtab[:, bi, cs, :],
                                                                                              start=(mc == 0), stop=(mc == 15)), reads=[t_zst[bi], t_dtab[bi]], writes=[tpb])
                    for gl in range(2):
                        g = gp * 2 + gl
                        for cs in range(2):
                            pb, tpb = pbs[gl * 2 + cs]
                            if cs:
                                P.op("act", lambda e, gl=gl, cs=cs, pb=pb: e.copy(out=p12[:, gl, cs, :], in_=pb[:, 0:512]), reads=[tpb], writes=[t_p12[gl]])
                            else:
                                P.op("dve", lambda e, gl=gl, cs=cs, pb=pb: e.tensor_copy(out=p12[:, gl, cs, :], in_=pb[:, 0:512]), reads=[tpb], writes=[t_p12[gl]])
                        py, tpy = bank()
                        P.op("pe", lambda e, gl=gl, py=py: e.matmul(py[:, 0:512], lhsT=consts["ccs"][0][:], rhs=p12[:, gl, 0, :], start=True, stop=False),
                             reads=[t_p12[gl], consts["ccs"][1]], writes=[tpy])
                        P.op("pe", lambda e, gl=gl, py=py: e.matmul(py[:, 0:512], lhsT=consts["nscs"][0][:], rhs=p12[:, gl, 1, :], start=False, stop=True),
                             reads=[t_p12[gl], consts["nscs"][1]], writes=[tpy])
                        P.op("act", lambda e, g=g, py=py: e.copy(out=hmT[:, 4 + g, 0:512], in_=py[:, 0:512]), reads=[tpy], writes=[t_fmT] + t_hmT)
            if _DBG and l == 0 and seq_out == 1:
                tkd = P.tok("dbgmix")
                out_toks.append(tkd)
                P.dma("sp", lambda e: e.dma_start(out=DO["dbg_mix"], in_=hmT[:, :, :]), reads=t_hmT + [t_fmT], writes=[tkd])
            wo = [load_wpiece(DI["w_out"][l], hf * 512, 512, row_scale=True) for hf in range(2)]
            for tau, xi in enumerate(tiles):
                for hf in range(2):
                    wbuf, twb = wo[hf]
                    pb, tpb = bank()
                    for fc in range(8):
                        P.op("pe", lambda e, fc=fc, pb=pb, wbuf=wbuf: e.matmul(pb[:, 0:512], lhsT=hmT[:, fc, tau * 128:(tau + 1) * 128], rhs=wbuf[:, fc, :],
                                                                              start=(fc == 0), stop=(fc == 7)), reads=[t_hmT[tau], t_fmT, twb], writes=[tpb])
                    P.op("dve", lambda e, hf=hf, pb=pb: e.tensor_tensor(out=xt[:, 0, hf * 512:(hf + 1) * 512], in0=pb[:, 0:512], in1=gb[:, cond, hf * 512:(hf + 1) * 512], op=ALU.mult),
                         reads=[tpb, t_gb], writes=[t_xt[0]])
                P.op("dve", lambda e, xi=xi: e.scalar_tensor_tensor(out=X[:, xi, :], in0=X[:, xi, :], scalar=ALPHA, in1=xt[:, 0, :], op0=ALU.mult, op1=ALU.add),
                     reads=[tX[xi], t_xt[0]], writes=[tX[xi]])
                layer_norm_inplace(xi, t_lnw, t_lnb)

        for l in range(NL):
            cur_l[0] = l
            if l > 0:
                moe_toks = t_gs + [t for pair in t_actT for t in pair]
                for tk in t_uT + [t_qT, t_kT, t_fmT] + t_hmT + t_ktm + t_og:
                    P.alias(tk, moe_toks)
                P.alias(t_Cout, t_GT)
            P.dma("sp", lambda e, l=l: e.dma_start(out=nwT[:], in_=DI["nwT"][l]), writes=[t_nwT])
            P.dma("sp", lambda e, l=l: e.dma_start(out=gbias[:], in_=DI["gbias"][l]), writes=[t_gbias])
            P.dma("sp", lambda e, l=l: e.dma_start(out=rw[:], in_=DI["router_w"][l].rearrange("(kc p) n -> p kc n", p=128)), writes=[t_rw])
            P.dma("sp", lambda e, l=l: e.dma_start(out=rbb[:], in_=DI["rbb"][l]), writes=[t_rbb])
            P.dma("sp", lambda e, l=l: e.dma_start(out=bguT[:], in_=DI["bguT"][l]), writes=[t_bguT])
            P.dma("sp", lambda e, l=l: e.dma_start(out=bdn[:], in_=DI["b_down"][l]), writes=[t_bdn])
            P.dma("sp", lambda e, l=l: e.dma_start(out=smb[:], in_=DI["smb"][l]), writes=[t_smb])
            P.dma("sp", lambda e, l=l: e.dma_start(out=lnw[:], in_=DI["ln1w"][l]), writes=[t_lnw])
            P.dma("sp", lambda e, l=l: e.dma_start(out=lnb[:], in_=DI["ln1b"][l]), writes=[t_lnb])
            mod_vectors(l, [0, 1, 2])

            P.dma("sp", lambda e, l=l: e.dma_start(out=Sst[:, :, 0:128], in_=DI["sC"][l].rearrange("u d e -> d u e")), writes=t_S)
            P.dma("sp", lambda e, l=l: e.dma_start(out=sm2[:, 20:28], in_=DI["sn"][l]), writes=[t_sm2])
            P.op("dve", lambda e: e.tensor_copy(out=Sst[:, :, 128], in_=sm2[:, 20:28]), reads=[t_sm2], writes=t_S)
            P.op("dve", lambda e: e.tensor_copy(out=mst[:], in_=smb[:]), reads=[t_smb], writes=[t_mst])
            kpc = load_wpiece(DI["w_in"][l], 512, 512, into=(wb[0], t_wb[0]))
            vpc = load_wpiece(DI["w_in"][l], 1024, 512, into=(wb[1], t_wb[1]))
            gpiece = load_wpiece(DI["w_in"][l], G_OFF, 16, into=(gpc, t_gpc))
            P.alias(t_zpc, t_hmT + [t_fmT] + t_gs)
            zpiece = load_wpiece(DI["w_in"][l], F_OFF, 512, into=(zpc, t_zpc))
            steps = [(0, c) for c in range(16)]
            dstg = arena[:, 4096:8192].bitcast(F32)
            for tkd in t_dstg:
                P.alias(tkd, [t_qT, t_kT])

            def front(gi):
                sp_dir, c = steps[gi]
                slot = gi % 4
                xs = gi % 2
                if l == 0:
                    P.dma("sp", lambda e: e.dma_start(out=xt[:, xs, :], in_=DI["xs_full"][c * 128:(c + 1) * 128, :]), writes=[t_xt[xs]])
                    for hf in range(2):
                        P.dma("sp", lambda e, hf=hf: e.dma_start(out=scr[:, :], in_=DI["pos_full"][c * 128:(c + 1) * 128, hf * 512:(hf + 1) * 512]), writes=[t_scr])
                        P.op("dve", lambda e, hf=hf: e.tensor_tensor(out=xt[:, xs, hf * 512:(hf + 1) * 512], in0=xt[:, xs, hf * 512:(hf + 1) * 512], in1=scr[:], op=ALU.add),
                             reads=[t_xt[xs], t_scr], writes=[t_xt[xs]])
                else:
                    P.dma("sp", lambda e: e.dma_start(out=xt[:, xs, :], in_=gouts[c % 4].ap()[(c // 4) * 128:(c // 4 + 1) * 128, :]), reads=[t_gout[c % 4]], writes=[t_xt[xs]])
                make_uT(xt[:, xs, :], t_xt[xs], 1, 0, 1, lambda kc: uT[:, kc, slot * 128:(slot + 1) * 128], t_uT[slot])

                def ev_k(pb, tpb):
                    P.op("act", lambda e: e.mul(out=ktm[:, slot, :], in_=pb[:, 0:512], mul=SCALE_K), reads=[tpb], writes=[t_ktm[slot]])

                def ev_v(pb, tpb):
                    P.op("dve", lambda e: e.tensor_copy(out=vext[:, slot, :, 0:128], in_=pb[:, 0:512].rearrange("p (h d) -> p h d", h=4)),
                         reads=[tpb], writes=[t_vext[slot]])

                def ev_g(pb, tpb):
                    gate_math(pb[:, 0:16], tpb, slot)

                def ev_z(pb, tpb):
                    zb = c % 2
                    P.op("act", lambda e: e.copy(out=zown[:, zb, :], in_=pb[:, 0:512]), reads=[tpb], writes=[t_zown[zb]])
                    P.dma("sp", lambda e: e.dma_start(out=zscr.ap()[c * 128:(c + 1) * 128, :], in_=zown[:, zb, :]), reads=[t_zown[zb]], writes=[t_zscr])

                tm_proj(slot, kpc[0], kpc[1], 512, ev_k)
                tm_proj(slot, vpc[0], vpc[1], 512, ev_v)
                tm_proj(slot, gpiece[0], gpiece[1], 16, ev_g)
                tm_proj(slot, zpiece[0], zpiece[1], 512, ev_z)

            def back(gi):
                sp_dir, c = steps[gi]
                slot = gi % 4
                state_update(slot, 0, maskf[:, c:c + 1], t_maskf)
                bi = gi % 2
                scaled_v(slot, 1)
                Dl = d_matmuls(slot, 1)
                for h in range(4):
                    dps, tdps = Dl[h]
                    P.op("act", lambda e, h=h, dps=dps: e.copy(out=dstg[:, bi * 528 + h * 129:bi * 528 + (h + 1) * 129], in_=dps), reads=[tdps], writes=[t_dstg[bi]])
                P.op("dve", lambda e: e.tensor_copy(out=dstg[:, bi * 528 + 516:bi * 528 + 520], in_=gt[:, slot, 3, 4:8]), reads=[t_gt[slot]], writes=[t_dstg[bi]])
                P.op("dve", lambda e: e.tensor_copy(out=dstg[:, bi * 528 + 520:bi * 528 + 524], in_=gt[:, slot, 2, 4:8]), reads=[t_gt[slot]], writes=[t_dstg[bi]])
                P.dma("sp", lambda e: e.dma_start(out=dscr.ap()[c], in_=dstg[:, bi * 528:(bi + 1) * 528]), reads=[t_dstg[bi]], writes=[t_dscr])

            front(0)
            for gi in range(len(steps)):
                if gi + 1 < len(steps):
                    front(gi + 1)
                back(gi)
            for ci, c in enumerate(range(15, -1, -1)):
                bi = ci % 2
                P.dma("sp", lambda e: e.dma_start(out=dstg[:, bi * 528:(bi + 1) * 528], in_=dscr.ap()[c]), reads=[t_dscr], writes=[t_dstg[bi]])
                Dl = [(dstg[:, bi * 528 + h * 129:bi * 528 + (h + 1) * 129], t_dstg[bi]) for h in range(4)]
                apply_update(1, Dl, dstg[:, bi * 528 + 516:bi * 528 + 520], dstg[:, bi * 528 + 520:bi * 528 + 524], t_dstg[bi], maskb[:, c:c + 1], t_maskb)
            P.alias(t_qT, t_dstg)
            P.alias(t_kT, t_dstg)

            for tkz in t_hmT + [t_fmT]:
                P.alias(tkz, [t_zpc])
            for tk in t_hacc:
                P.alias(tk, t_tmpA + t_tmpC)
            process_item(l, [4, 5, 6, 7], 1, True, None)
            process_item(l, [0, 1], 0, False, 0)
            process_item(l, [2, 3], 0, False, 1)

            if _DBG and l == 0:
                tkd2 = P.tok("dbgx1")
                out_toks.append(tkd2)
                P.dma("sp", lambda e: e.dma_start(out=DO["dbg_x1"], in_=X[:, :, :]), reads=tX, writes=[tkd2])
            mixer_toks = t_uT + [t_qT, t_kT, t_fmT] + t_hmT + t_ktm + t_og
            for tk in t_gs + [t for pair in t_actT for t in pair]:
                P.alias(tk, mixer_toks)
            for tk in t_tmpA + t_tmpC:
                P.alias(tk, t_hacc)
            for tk in t_GT:
                P.alias(tk, [t_Cout])
            mod_vectors(l, [3, 4, 5])
            P.dma("sp", lambda e, l=l: e.dma_start(out=lnw[:], in_=DI["ln2w"][l]), writes=[t_lnw])
            P.dma("sp", lambda e, l=l: e.dma_start(out=lnb[:], in_=DI["ln2b"][l]), writes=[t_lnb])
            for xi in range(8):
                cond = 0 if xi < 4 else 1
                for half in range(2):
                    pb, tpb = bank()
                    for q in range(4):
                        kc = half * 4 + q
                        P.op("pe", lambda e, q=q, kc=kc, pb=pb, xi=xi: e.transpose(pb[:, q * 128:(q + 1) * 128], X[:, xi, kc * 128:(kc + 1) * 128], ident_f[:]),
                             reads=[tX[xi], t_idf], writes=[tpb])
                    for q in range(4):
                        kc = half * 4 + q
                        P.op("act", lambda e, q=q, kc=kc, pb=pb, cond=cond: e.activation(out=u2f[:, kc, :], in_=pb[:, q * 128:(q + 1) * 128], func=AF.Identity,
                                                                                         scale=modT[:, 4, kc, cond:cond + 1], bias=modT[:, 3, kc, cond:cond + 1]),
                             reads=[tpb, t_modT], writes=[t_u2f])
                P.op("act", lambda e, xi=xi: e.copy(out=u2T[:, :, xi * 128:(xi + 1) * 128], in_=u2f[:]), reads=[t_u2f], writes=[t_u2T[xi]])
                pl, tpl = bank()
                for kc in range(8):
                    P.op("pe", lambda e, kc=kc, pl=pl: e.matmul(pl[:, 0:NE], lhsT=u2f[:, kc, :], rhs=rw[:, kc, :], start=(kc == 0), stop=(kc == 7)),
                         reads=[t_u2f, t_rw], writes=[tpl])
                P.op("dve", lambda e, pl=pl: e.tensor_tensor(out=lg[:, 0, :], in0=pl[:, 0:NE], in1=rbb[:], op=ALU.add), reads=[tpl, t_rbb], writes=[t_lg])
                P.op("dve", lambda e: e.max(out=sm2[:, 0:8], in_=lg[:, 0, :]), reads=[t_lg], writes=[t_sm2])
                P.op("dve", lambda e: e.tensor_scalar(out=lg[:, 1, :], in0=lg[:, 0, :], scalar1=sm2[:, 3:4], scalar2=None, op0=ALU.is_ge), reads=[t_lg, t_sm2], writes=[t_lg])
                P.op("dve", lambda e: e.tensor_scalar(out=sm2[:, 8:9], in0=sm2[:, 0:1], scalar1=-1.0, scalar2=None, op0=ALU.mult), reads=[t_sm2], writes=[t_sm2])
                P.op("act", lambda e: e.activation(out=lg[:, 2, :], in_=lg[:, 0, :], func=AF.Exp, bias=sm2[:, 8:9], scale=1.0), reads=[t_lg, t_sm2], writes=[t_lg])
                P.op("dve", lambda e: e.tensor_tensor(out=lg[:, 3, :], in0=lg[:, 2, :], in1=lg[:, 1, :], op=ALU.mult), reads=[t_lg], writes=[t_lg])
                P.op("dve", lambda e: e.reduce_sum(out=sm2[:, 9:10], in_=lg[:, 3, :], axis=mybir.AxisListType.X), reads=[t_lg], writes=[t_sm2])
                P.op("dve", lambda e: e.reciprocal(out=sm2[:, 10:11], in_=sm2[:, 9:10]), reads=[t_sm2], writes=[t_sm2])
                P.op("dve", lambda e, xi=xi: e.tensor_scalar(out=Gall[:, xi, :], in0=lg[:, 3, :], scalar1=sm2[:, 10:11], scalar2=None, op0=ALU.mult),
                     reads=[t_lg, t_sm2], writes=[t_G[xi]])
                pg2, tpg2 = bank()
                P.op("pe", lambda e, xi=xi, pg2=pg2: e.transpose(pg2[0:NE, 0:128], Gall[:, xi, :], ident_f[:]), reads=[t_G[xi], t_idf], writes=[tpg2])
                P.op("act", lambda e, xi=xi, pg2=pg2: e.copy(out=GT[:, xi * 128:(xi + 1) * 128], in_=pg2[0:NE, 0:128]), reads=[tpg2], writes=[t_GT[xi]])
                P.op("dve", lambda e, xi=xi: e.tensor_scalar(out=X[:, xi, :], in0=X[:, xi, :], scalar1=ALPHA, scalar2=None, op0=ALU.mult), reads=[tX[xi]], writes=[tX[xi]])
                for hf in range(2):
                    pbb_, tpbb_ = bank()
                    P.op("pe", lambda e, xi=xi, hf=hf, pbb_=pbb_: e.matmul(pbb_[:, 0:512], lhsT=GT[:, xi * 128:(xi + 1) * 128], rhs=bdn[:, hf * 512:(hf + 1) * 512], start=True, stop=True),
                         reads=[t_GT[xi], t_bdn], writes=[tpbb_])
                    P.op("dve", lambda e, hf=hf, pbb_=pbb_, cond=cond: e.tensor_tensor(out=xt[:, 1, hf * 512:(hf + 1) * 512], in0=pbb_[:, 0:512], in1=gb[:, cond, hf * 512:(hf + 1) * 512], op=ALU.mult),
                         reads=[tpbb_, t_gb], writes=[t_xt[1]])
                P.op("dve", lambda e, xi=xi: e.tensor_tensor(out=X[:, xi, :], in0=X[:, xi, :], in1=xt[:, 1, :], op=ALU.add), reads=[tX[xi], t_xt[1]], writes=[tX[xi]])
            pieces = []
            for ex in range(NEX):
                pieces += [("glu", ex, 0, 0), ("lin", ex, 0, 1024), ("glu", ex, 1, 512), ("lin", ex, 1, 1536), ("down", ex, 0, 0), ("down", ex, 1, 512)]

            def fetch(pc):
                kind, ex, fb, c0 = pc
                src = DI["w_down"][l, ex] if kind == "down" else DI["w_gu"][l, ex]
                return load_wpiece(src, c0, 512, eng="act")

            nxt = fetch(pieces[0]) if pieces else None
            for pi, pc in enumerate(pieces):
                kind, ex, fb, c0 = pc
                wbuf, twb = nxt
                if pi + 1 < len(pieces):
                    nxt = fetch(pieces[pi + 1])
                if kind == "glu":
                    for fcl in range(4):
                        fc = fb * 4 + fcl
                        for th in range(2):
                            pb, tpb = bank()
                            for kc in range(8):
                                P.op("pe", lambda e, kc=kc, fcl=fcl, th=th, pb=pb, wbuf=wbuf: e.matmul(pb[:, 0:512], lhsT=wbuf[:, kc, fcl * 128:(fcl + 1) * 128], rhs=u2T[:, kc, th * 512:(th + 1) * 512],
                                                                                                   start=(kc == 0), stop=(kc == 7)), reads=[twb] + t_u2T[th * 4:th * 4 + 4], writes=[tpb])
                            i2 = (fcl * 2 + th) % 2
                            P.op("dve", lambda e, fc=fc, ex=ex, pb=pb, i2=i2: e.tensor_scalar(out=tmpA[:, i2, :], in0=pb[:, 0:512], scalar1=bguT[:, fc, ex:ex + 1], scalar2=LIM, op0=ALU.add, op1=ALU.min),
                                 reads=[tpb, t_bguT], writes=[t_tmpA[i2]])
                            P.op("act", lambda e, i2=i2: e.activation(out=tmpB[:, i2, :], in_=tmpA[:, i2, :], func=AF.Sigmoid, scale=SALPHA), reads=[t_tmpA[i2]], writes=[t_tmpB[i2]])
                            P.op("dve", lambda e, fcl=fcl, th=th, i2=i2: e.tensor_tensor(out=gsb[:, fcl, th * 512:(th + 1) * 512], in0=tmpA[:, i2, :], in1=tmpB[:, i2, :], op=ALU.mult),
                                 reads=[t_tmpA[i2], t_tmpB[i2]], writes=[t_gs[fcl * 2 + th]])
                elif kind == "lin":
                    for fcl in range(4):
                        fc = fb * 4 + fcl
                        for th in range(2):
                            pb, tpb = bank()
                            for kc in range(8):
                                P.op("pe", lambda e, kc=kc, fcl=fcl, th=th, pb=pb, wbuf=wbuf: e.matmul(pb[:, 0:512], lhsT=wbuf[:, kc, fcl * 128:(fcl + 1) * 128], rhs=u2T[:, kc, th * 512:(th + 1) * 512],
                                                                                                   start=(kc == 0), stop=(kc == 7)), reads=[twb] + t_u2T[th * 4:th * 4 + 4], writes=[tpb])
                            i2 = (fcl * 2 + th) % 2
                            P.op("act", lambda e, fc=fc, ex=ex, pb=pb, i2=i2: e.activation(out=tmpC[:, i2, :], in_=pb[:, 0:512], func=AF.Identity, bias=bguT[:, 8 + fc, ex:ex + 1], scale=1.0),
                                 reads=[tpb, t_bguT], writes=[t_tmpC[i2]])
                            P.op("dve", lambda e, i2=i2: e.tensor_scalar(out=tmpC[:, i2, :], in0=tmpC[:, i2, :], scalar1=LIM, scalar2=-LIM, op0=ALU.min, op1=ALU.max),
                                 reads=[t_tmpC[i2]], writes=[t_tmpC[i2]])
                            P.op("dve", lambda e, fc=fc, fcl=fcl, th=th, i2=i2: e.scalar_tensor_tensor(out=actT[:, fc, th * 512:(th + 1) * 512], in0=tmpC[:, i2, :], scalar=1.0, in1=gsb[:, fcl, th * 512:(th + 1) * 512],
                                                                                                  op0=ALU.add, op1=ALU.mult),
                                 reads=[t_gs[fcl * 2 + th], t_tmpC[i2]], writes=[t_actT[fc][th]])
                else:
                    dh = fb
                    for xi in range(8):
                        cond = 0 if xi < 4 else 1
                        pb, tpb = bank()
                        for fc in range(8):
                            P.op("pe", lambda e, fc=fc, xi=xi, pb=pb, wbuf=wbuf: e.matmul(pb[:, 0:512], lhsT=actT[:, fc, xi * 128:(xi + 1) * 128], rhs=wbuf[:, fc, :],
                                                                                         start=(fc == 0), stop=(fc == 7)), reads=[twb, t_actT[fc][xi // 4]], writes=[tpb])
                        i2 = xi % 2
                        P.op("dve", lambda e, xi=xi, ex=ex, pb=pb, i2=i2, cond=cond, dh=dh: e.scalar_tensor_tensor(out=tmpA[:, i2, :], in0=pb[:, 0:512], scalar=Gall[:, xi, ex:ex + 1],
                                                                                                         in1=gb[:, cond, dh * 512:(dh + 1) * 512], op0=ALU.mult, op1=ALU.mult),
                             reads=[tpb, t_G[xi], t_gb], writes=[t_tmpA[i2]])
                        P.op("dve", lambda e, xi=xi, i2=i2, dh=dh: e.tensor_tensor(out=X[:, xi, dh * 512:(dh + 1) * 512], in0=X[:, xi, dh * 512:(dh + 1) * 512], in1=tmpA[:, i2, :], op=ALU.add),
                             reads=[tX[xi], t_tmpA[i2]], writes=[tX[xi]])
            for xi in range(8):
                layer_norm_inplace(xi, t_lnw, t_lnb)

            if l < NL - 1:
                for i in range(4):
                    P.dma("pool", lambda e, i=i: e.dma_start(out=gins[i].ap(), in_=X[:, 4 + i, :]), reads=[tX[4 + i]], writes=[t_gin[i]])
                    P.dma("pool", lambda e, i=i: e.collective_compute("AllGather", ALU.bypass, replica_groups=[[0, 1, 2, 3], [4, 5, 6, 7]],
                                                                      ins=[gins[i].ap().opt()], outs=[gouts[i].ap().opt()]), reads=[t_gin[i]], writes=[t_gout[i]], inc=1)
            else:
                for i in range(4):
                    P.dma("sp", lambda e, i=i: e.dma_start(out=DO["yp"][i * 128:(i + 1) * 128, :], in_=X[:, i, :]), reads=[tX[i]], writes=[], key=okey["yp"])
                    P.dma("sp", lambda e, i=i: e.dma_start(out=DO["ys"][i * 128:(i + 1) * 128, :], in_=X[:, 4 + i, :]), reads=[tX[4 + i]], writes=[], key=okey["ys"])

        P.emit(st, final_waits=out_toks)
    return nc


def _grid_pos_embed(rows, d):
    quarter = d // 4
    omega = (1.0 / (10000.0 ** (np.arange(quarter, dtype=np.float32) / np.float32(quarter)))).astype(np.float32)
    r = np.repeat(np.arange(rows, dtype=np.float32), 64)[:, None] * omega
    cc = np.tile(np.arange(64, dtype=np.float32), rows)[:, None] * omega
    return np.concatenate([np.sin(r), np.cos(r), np.sin(cc), np.cos(cc)], axis=-1).astype(np.float32)


_CACHE = {}
_DBG = False
_CFG = {"NL": 4, "NEX": NE}


def kernel(x_prompt, x_sample, state_C, state_n, state_m, c, c_ctx, w_ada, b_ada, w_in, gate_bias, mlstm_norm_w,
           w_out, ln1_w, ln1_b, router_w, router_b, w_gate_up, b_gate_up, w_down, b_down, ln2_w, ln2_b):
    f32 = np.float32
    bf = ml_dtypes.bfloat16
    A = lambda a: np.ascontiguousarray(np.asarray(a), dtype=f32)
    x_prompt, x_sample = A(x_prompt), A(x_sample)
    NL, NEX = _CFG["NL"], _CFG["NEX"]
    key = (NL, NEX)
    if key not in _CACHE:
        _CACHE[key] = build_program(NL, NEX)
    nc = _CACHE[key]
    DEPTH = NL
    pos = _grid_pos_embed(2048 // 64, D)
    n = np.arange(2048, dtype=np.float64)
    ang = 2 * np.pi * ((n[:, None] * n[None, :]) % 2048) / 2048
    CN, SN = np.cos(ang), np.sin(ang)
    n2 = np.arange(256, dtype=np.float64)
    ang2 = 2 * np.pi * ((n2[:, None] * n2[None, :]) % 256) / 256
    n3 = np.arange(128, dtype=np.float64)
    ang3 = 2 * np.pi * ((n3[:, None] * n3[None, :]) % 128) / 128
    CC, SC = np.cos(ang3), np.sin(ang3)
    sp_, ss_ = 1.0 / np.sqrt(256 * 128.0), 1.0 / np.sqrt(2048 * 128.0)
    ar = np.arange(128)
    triU = (ar[:, None] <= ar[None, :]).astype(f32)
    triL = (ar[:, None] >= ar[None, :]).astype(f32)
    selr = np.zeros((2, 2, 128), f32)
    selr[0, 0] = 1
    selr[1, 1] = 1
    sel8 = np.zeros((8, 2), f32)
    sel8[0:4, 0] = 1
    sel8[4:8, 1] = 1
    shared = {
        "pos_full": pos,
        "w_ada": A(w_ada)[:NL], "b_ada2": np.ascontiguousarray(np.broadcast_to(A(b_ada)[:NL, None, :], (DEPTH, 2, 6 * D))),
        "w_in": A(w_in)[:NL], "gbias": np.ascontiguousarray(np.broadcast_to(A(gate_bias)[:NL, None, :], (DEPTH, 128, 16))),
        "nwT": np.ascontiguousarray(A(mlstm_norm_w)[:NL].reshape(DEPTH, 4, 128).transpose(0, 2, 1)),
        "w_out": A(w_out)[:NL],
        "ln1w": np.ascontiguousarray(np.broadcast_to(A(ln1_w)[:NL, None, :], (DEPTH, 128, D))),
        "ln1b": np.ascontiguousarray(np.broadcast_to(A(ln1_b)[:NL, None, :], (DEPTH, 128, D))),
        "router_w": A(router_w)[:NL], "rbb": np.ascontiguousarray(np.broadcast_to(A(router_b)[:NL, None, :], (DEPTH, 128, NE))),
        "w_gu": A(w_gate_up)[:NL, :max(NEX, 1)], "bguT": np.ascontiguousarray(A(b_gate_up)[:NL].reshape(DEPTH, NE, 16, 128).transpose(0, 3, 2, 1)),
        "w_down": A(w_down)[:NL, :max(NEX, 1)], "b_down": A(b_down)[:NL],
        "ln2w": np.ascontiguousarray(np.broadcast_to(A(ln2_w)[:NL, None, :], (DEPTH, 128, D))),
        "ln2b": np.ascontiguousarray(np.broadcast_to(A(ln2_b)[:NL, None, :], (DEPTH, 128, D))),
        "c256": np.cos(ang2).astype(bf), "s256": np.sin(ang2).astype(bf),
        "ccp": (CC * sp_).astype(bf), "nscp": (-SC * sp_).astype(bf), "ccs": (CC * ss_).astype(bf), "nscs": (-SC * ss_).astype(bf),
        "ident_b": np.eye(128, dtype=f32).astype(bf), "ident_f": np.eye(128, dtype=f32), "triU": triU, "triL": triL,
        "selr": selr, "sel8": sel8,
        "NMU4": np.ascontiguousarray(np.broadcast_to(((triU - 1.0) * 30000.0)[:, None, :], (128, 4, 128))).astype(f32),
        "NML4": np.ascontiguousarray(np.broadcast_to(((triL - 1.0) * 30000.0)[:, None, :], (128, 4, 128))).astype(f32),
    }
    state_C, state_n, state_m, c, c_ctx = A(state_C), A(state_n), A(state_m), A(c), A(c_ctx)
    in_maps = []
    for core in range(8):
        b, j = divmod(core, 4)
        cv = np.stack([c_ctx, c[b]], 0)
        cvT = np.ascontiguousarray(cv.reshape(2, 8, 128).transpose(2, 1, 0).reshape(128, 16))
        mf = np.zeros((128, 16), f32)
        mb = np.zeros((128, 16), f32)
        mf[:, :4 * j] = 1
        mb[:, 4 * j + 4:] = 1
        m = dict(shared)
        m.update({
            "xp": np.ascontiguousarray(x_prompt[2 * core:2 * core + 2].reshape(512, D)),
            "xs_own": np.ascontiguousarray(x_sample[b, 512 * j:512 * j + 512]),
            "pos_own": np.ascontiguousarray(pos[512 * j:512 * j + 512]),
            "xs_full": np.ascontiguousarray(x_sample[b]),
            "sC": np.ascontiguousarray(state_C[b][:NL].reshape(DEPTH, 8, 128, 128)),
            "sn": np.ascontiguousarray(state_n[b][:NL].reshape(DEPTH, 8, 128).transpose(0, 2, 1)),
            "smb": np.ascontiguousarray(np.broadcast_to(state_m[b][:NL].reshape(DEPTH, 1, 8), (DEPTH, 128, 8))),
            "cvT": cvT, "maskf": mf, "maskb": mb,
            "cn_own": np.ascontiguousarray(CN[:, 512 * j:512 * j + 512]).astype(bf),
            "sn_own": np.ascontiguousarray(SN[:, 512 * j:512 * j + 512]).astype(bf),
        })
        in_maps.append(m)
    res = run_bass_kernel_spmd(nc, in_maps, core_ids=list(range(8)))
    R = res.results
    y_prompt = np.concatenate([R[k]["yp"].reshape(2, 256, D) for k in range(8)], 0).astype(f32)
    y_sample = np.stack([np.concatenate([R[4 * b + j]["ys"] for j in range(4)], 0) for b in range(2)], 0).astype(f32)
    nC = np.concatenate([R[k]["nC"] for k in range(8)], 0).reshape(16, DEPTH, 2, 4, 128, 128).astype(f32)
    nn = np.concatenate([R[k]["nn"] for k in range(8)], 0).reshape(16, DEPTH, 2, 4, 128).astype(f32)
    nm = np.concatenate([R[k]["nm"] for k in range(8)], 0).reshape(16, DEPTH, 2, 4).astype(f32)
    if _DBG:
      _CACHE["dbg2"] = {k2: [np.asarray(R[k][k2]) for k in range(8)] for k2 in ["dbg_h1", "dbg_h2", "dbg_gt", "dbg_stx", "dbg_scr", "dbg_qk", "dbg_nd", "dbg_sm2"]}
      _CACHE["dbg"] = {"mix": [np.asarray(R[k]["dbg_mix"]) for k in range(8)], "x1": [np.asarray(R[k]["dbg_x1"]) for k in range(8)]}
    return (y_prompt, y_sample, nC, nn, nm)
```

```python
from contextlib import ExitStack
import types
import numpy as np
import ml_dtypes
import concourse.bass as bass
import concourse.mybir as mybir
from concourse.bass_utils import run_bass_kernel_spmd

F32 = mybir.dt.float32
BF16 = mybir.dt.bfloat16
AF = mybir.ActivationFunctionType
ALU = mybir.AluOpType

D = 1024
DEPTH = 4
NE = 32
DH = 128
ALPHA = (2 * DEPTH) ** 0.25
LN_EPS = 1e-5
LIM = 7.0
SALPHA = 1.702
IN_COLS = 2576
G_OFF = 2048
F_OFF = 2064

COMPUTE = ("pe", "dve", "act", "pool")
ENGS = ("pe", "dve", "act", "pool", "sp")
EPOCH = 30000


class Tok:
    __slots__ = ("name", "last_w", "readers", "dma_total", "sem", "pending")

    def __init__(self, name):
        self.name = name
        self.last_w = None
        self.readers = []
        self.dma_total = 0
        self.sem = None
        self.pending = []


def _snapshot(fn):
    if getattr(fn, "__closure__", None) is None:
        return fn
    cells = []
    for c in fn.__closure__:
        try:
            cells.append(types.CellType(c.cell_contents))
        except ValueError:
            cells.append(c)
    return types.FunctionType(fn.__code__, fn.__globals__, fn.__name__, fn.__defaults__, tuple(cells))


class Op:
    __slots__ = ("eng", "fn", "waits", "signal", "sig_idx", "dma_key", "dma_val", "inc")

    def __init__(self, eng, fn):
        self.eng = eng
        self.fn = _snapshot(fn)
        self.waits = []
        self.signal = False
        self.sig_idx = None
        self.dma_key = None
        self.dma_val = None
        self.inc = 16


class Prog:
    def __init__(self, nc):
        self.nc = nc
        self.ops = {e: [] for e in ENGS}

    def tok(self, name="t"):
        return Tok(name)

    def toks(self, n, name="t"):
        return [Tok(name) for _ in range(n)]

    def alias(self, new, olds):
        for o in olds:
            if o.last_w is not None:
                new.pending.append(o.last_w)
            new.pending.extend(o.readers)

    def _add(self, eng, fn, reads, writes, dma_key=None, inc=16):
        o = Op(eng, fn)
        deps = []
        for t in reads:
            if t.last_w is not None:
                deps.append(t.last_w)
        for t in writes:
            if t.last_w is not None:
                deps.append(t.last_w)
            deps.extend(t.readers)
            if t.pending:
                deps.extend(t.pending)
                t.pending = []
        seen = set()
        for d in deps:
            if d is o or id(d) in seen:
                continue
            seen.add(id(d))
            if d.dma_key is not None:
                o.waits.append(("d", d.dma_key, d.dma_key.dma_total))
            else:
                if d.eng == "pe" and eng == "pe":
                    continue
                d.signal = True
                o.waits.append(("c", d.eng, d))
        for t in reads:
            t.readers.append(o)
        for t in writes:
            t.last_w = o
            t.readers = []
        if dma_key is not None:
            o.dma_key = dma_key
            o.inc = inc
            dma_key.dma_total += inc
            o.dma_val = dma_key.dma_total
        self.ops[eng].append(o)
        return o

    def op(self, eng, fn, reads=(), writes=()):
        return self._add(eng, fn, list(reads), list(writes))

    def dma(self, eng, fn, reads=(), writes=(), key=None, inc=16):
        if key is None:
            key = list(writes)[0]
        return self._add(eng, fn, list(reads), list(writes), dma_key=key, inc=inc)

    def emit(self, stack, final_waits=()):
        nc = self.nc
        nsig = {}
        for e in ENGS:
            k = 0
            for o in self.ops[e]:
                if o.dma_key is None and o.signal:
                    k += 1
                    o.sig_idx = k
            nsig[e] = k
        esems = {}
        for e in ENGS:
            n_ep = max(1, (nsig[e] + EPOCH - 1) // EPOCH)
            esems[e] = [stack.enter_context(nc.semaphore(f"s_{e}{i}")) for i in range(n_ep)]
        nk = 0
        for e in ENGS:
            for o in self.ops[e]:
                if o.dma_key is not None and o.dma_key.sem is None:
                    o.dma_key.sem = stack.enter_context(nc.semaphore(f"d_{nk}"))
                    nk += 1
        self.n_sems = sum(len(v) for v in esems.values()) + nk
        block = stack.enter_context(nc.Block())
        ops = self.ops

        def run(e, eng):
            waited_c = {}
            waited_d = {}
            for o in ops[e]:
                for w in o.waits:
                    if w[0] == "c":
                        d = w[2]
                        ep, v = divmod(d.sig_idx - 1, EPOCH)
                        v += 1
                        kk = (d.eng, ep)
                        if waited_c.get(kk, 0) >= v:
                            continue
                        if any(k2[0] == d.eng and k2[1] > ep for k2 in waited_c):
                            continue
                        waited_c[kk] = v
                        eng.wait_ge(esems[d.eng][ep], v)
                    else:
                        t, v = w[1], w[2]
                        if waited_d.get(id(t), 0) >= v:
                            continue
                        waited_d[id(t)] = v
                        eng.wait_ge(t.sem, v)
                ins = o.fn(eng)
                if o.dma_key is not None:
                    ins.then_inc(o.dma_key.sem, o.inc)
                elif o.signal:
                    ins.then_inc(esems[e][(o.sig_idx - 1) // EPOCH], 1)
            if e == "sp":
                for t in final_waits:
                    eng.wait_ge(t.sem, t.dma_total)

        @block.sync
        def _(eng):
            run("sp", eng)

        @block.tensor
        def _(eng):
            run("pe", eng)

        @block.vector
        def _(eng):
            run("dve", eng)

        @block.scalar
        def _(eng):
            run("act", eng)

        @block.gpsimd
        def _(eng):
            run("pool", eng)


def input_specs(DEPTH, NEX):
  return [
    ("xp", [512, D], F32), ("xs_own", [512, D], F32), ("pos_own", [512, D], F32),
    ("xs_full", [2048, D], F32), ("pos_full", [2048, D], F32),
    ("sC", [DEPTH, 8, 128, 128], F32), ("sn", [DEPTH, 128, 8], F32), ("smb", [DEPTH, 128, 8], F32),
    ("cvT", [128, 16], F32), ("maskf", [128, 16], F32), ("maskb", [128, 16], F32),
    ("w_ada", [DEPTH, D, 6 * D], F32), ("b_ada2", [DEPTH, 2, 6 * D], F32),
    ("w_in", [DEPTH, D, IN_COLS], F32), ("gbias", [DEPTH, 128, 16], F32), ("nwT", [DEPTH, 128, 4], F32),
    ("w_out", [DEPTH, D, D], F32), ("ln1w", [DEPTH, 128, D], F32), ("ln1b", [DEPTH, 128, D], F32),
    ("router_w", [DEPTH, D, NE], F32), ("rbb", [DEPTH, 128, NE], F32),
    ("w_gu", [DEPTH, NEX, D, 2 * D], F32), ("bguT", [DEPTH, 128, 16, NE], F32),
    ("w_down", [DEPTH, NEX, D, D], F32), ("b_down", [DEPTH, NE, D], F32),
    ("ln2w", [DEPTH, 128, D], F32), ("ln2b", [DEPTH, 128, D], F32),
    ("cn_own", [2048, 512], BF16), ("sn_own", [2048, 512], BF16),
    ("c256", [256, 256], BF16), ("s256", [256, 256], BF16),
    ("ccp", [128, 128], BF16), ("nscp", [128, 128], BF16), ("ccs", [128, 128], BF16), ("nscs", [128, 128], BF16),
    ("ident_b", [128, 128], BF16), ("ident_f", [128, 128], F32), ("triU", [128, 128], F32), ("triL", [128, 128], F32),
    ("selr", [2, 2, 128], F32), ("sel8", [8, 2], F32), ("NMU4", [128, 4, 128], F32), ("NML4", [128, 4, 128], F32),
  ]


def output_specs(DEPTH):
  return [
    ("yp", [512, D], F32), ("ys", [512, D], F32),
    ("nC", [2, DEPTH, 8, 128, 128], F32), ("nn", [2, DEPTH, 8, 128], F32), ("nm", [2, DEPTH, 8], F32),
  ]


def build_program(NL=DEPTH, NEX=NE, do_cc=True, stop_after=None):
    nc = bass.Bass("TRN2", target_bir_lowering=False)
    P = Prog(nc)
    DI = {n: nc.dram_tensor(n, list(s), d, kind="ExternalInput").ap() for n, s, d in input_specs(NL, max(NEX, 1))}
    DO = {n: nc.dram_tensor(n, list(s), d, kind="ExternalOutput").ap() for n, s, d in output_specs(NL)}
    if _DBG:
        DO["dbg_mix"] = nc.dram_tensor("dbg_mix", [128, 8, 512], BF16, kind="ExternalOutput").ap()
        DO["dbg_h1"] = nc.dram_tensor("dbg_h1", [128, 2, 512], F32, kind="ExternalOutput").ap()
        DO["dbg_h2"] = nc.dram_tensor("dbg_h2", [128, 2, 512], F32, kind="ExternalOutput").ap()
        DO["dbg_gt"] = nc.dram_tensor("dbg_gt", [128, 2, 48], F32, kind="ExternalOutput").ap()
        DO["dbg_stx"] = nc.dram_tensor("dbg_stx", [128, 2, 4, 128], BF16, kind="ExternalOutput").ap()
        DO["dbg_scr"] = nc.dram_tensor("dbg_scr", [128, 512], F32, kind="ExternalOutput").ap()
        DO["dbg_qk"] = nc.dram_tensor("dbg_qk", [128, 2, 4, 256], BF16, kind="ExternalOutput").ap()
        DO["dbg_nd"] = nc.dram_tensor("dbg_nd", [128, 2, 129], F32, kind="ExternalOutput").ap()
        DO["dbg_sm2"] = nc.dram_tensor("dbg_sm2", [128, 32], F32, kind="ExternalOutput").ap()
        DO["dbg_x1"] = nc.dram_tensor("dbg_x1", [128, 8, D], F32, kind="ExternalOutput").ap()
    gins = [nc.dram_tensor(f"gin{i}", [128, D], F32) for i in range(4)]
    gouts = [nc.dram_tensor(f"gout{i}", [512, D], F32) for i in range(4)]
    t_gin, t_gout = P.toks(4, "gin"), P.toks(4, "gout")
    out_toks = []

    with ExitStack() as st:
        def sb(name, shape, dt):
            return st.enter_context(nc.sbuf_tensor("sb_" + name, list(shape), dt))

        PS = [st.enter_context(nc.psum_tensor(f"ps{i}", [128, 512], F32)) for i in range(8)]
        tPS = P.toks(8, "ps")
        bank_ctr = [0]
        PLONG, t_PLONG = PS[7], tPS[7]

        def bank():
            i = bank_ctr[0] % 7
            bank_ctr[0] += 1
            return PS[i], tPS[i]

        consts = {}
        for n, shp, dt in [("ident_b", [128, 128], BF16), ("ident_f", [128, 128], F32), ("triU", [128, 128], F32),
                           ("triL", [128, 128], F32), ("ccp", [128, 128], BF16), ("nscp", [128, 128], BF16),
                           ("ccs", [128, 128], BF16), ("nscs", [128, 128], BF16), ("cvT", [128, 16], F32),
                           ("maskf", [128, 16], F32), ("maskb", [128, 16], F32), ("selr", [2, 2, 128], F32), ("sel8", [8, 2], F32), ("NMU4", [128, 4, 128], F32), ("NML4", [128, 4, 128], F32)]:
            t = sb("c_" + n, shp, dt)
            tk = P.tok(n)
            P.dma("sp", lambda e, t=t, n=n: e.dma_start(out=t[:], in_=DI[n]), writes=[tk])
            consts[n] = (t, tk)
        ident_b, t_idb = consts["ident_b"]
        ident_f, t_idf = consts["ident_f"]
        triU, t_triU = consts["triU"]
        triL, t_triL = consts["triL"]
        cvT, t_cvT = consts["cvT"]
        maskf, t_maskf = consts["maskf"]
        maskb, t_maskb = consts["maskb"]
        selr, t_selr = consts["selr"]
        sel8, t_sel8 = consts["sel8"]
        NMU4, t_NMU4 = consts["NMU4"]
        NML4, t_NML4 = consts["NML4"]
        c256 = sb("c256", [128, 2, 256], BF16)
        s256 = sb("s256", [128, 2, 256], BF16)
        t_c256, t_s256 = P.tok(), P.tok()
        P.dma("sp", lambda e: e.dma_start(out=c256[:], in_=DI["c256"].rearrange("(m p) n -> p m n", p=128)), writes=[t_c256])
        P.dma("sp", lambda e: e.dma_start(out=s256[:], in_=DI["s256"].rearrange("(m p) n -> p m n", p=128)), writes=[t_s256])
        ones_f = sb("ones_f", [128, 128], F32)
        ones_b = sb("ones_b", [128, 128], BF16)
        eps_t = sb("eps_t", [128, 1], F32)
        t_ones = P.tok()
        P.op("pool", lambda e: e.memset(ones_f[:], 1.0), writes=[t_ones])
        P.op("pool", lambda e: e.memset(ones_b[:], 1.0), writes=[t_ones])
        P.op("pool", lambda e: e.memset(eps_t[:], LN_EPS), writes=[t_ones])
        sT = sb("sT", [128, 16], F32)
        t_sT = P.tok()
        P.op("act", lambda e: e.activation(out=sT[:], in_=cvT[:], func=AF.Silu), reads=[t_cvT], writes=[t_sT])

        X = sb("X", [128, 8, D], F32)
        tX = P.toks(8, "X")
        for i in range(4):
            P.dma("sp", lambda e, i=i: e.dma_start(out=X[:, i, :], in_=DI["xp"][i * 128:(i + 1) * 128, :]), writes=[tX[i]])
        ptmp = sb("ptmp", [128, 2, D], F32)
        t_ptmp = P.toks(2)
        for i in range(4):
            P.dma("sp", lambda e, i=i: e.dma_start(out=X[:, 4 + i, :], in_=DI["xs_own"][i * 128:(i + 1) * 128, :]), writes=[tX[4 + i]])
            P.dma("sp", lambda e, i=i: e.dma_start(out=ptmp[:, i % 2, :], in_=DI["pos_own"][i * 128:(i + 1) * 128, :]), writes=[t_ptmp[i % 2]])
            P.op("pool", lambda e, i=i: e.tensor_tensor(out=X[:, 4 + i, :], in0=X[:, 4 + i, :], in1=ptmp[:, i % 2, :], op=ALU.add),
                 reads=[tX[4 + i], t_ptmp[i % 2]], writes=[tX[4 + i]])

        STG_N = 2
        stg = [sb(f"stg{i}", [128, 8, 256], F32) for i in range(STG_N)]
        t_stg = P.toks(STG_N, "stg")
        stg_ctr = [0]

        def stage_load(src_ap):
            i = stg_ctr[0] % STG_N
            stg_ctr[0] += 1
            P.dma("sp", lambda e: e.dma_start(out=stg[i][:], in_=src_ap.rearrange("(kc p) c -> p kc c", p=128)), writes=[t_stg[i]])
            return stg[i], t_stg[i]

        cast_ctr = [0]

        def cast_eng():
            cast_ctr[0] += 1
            return "dve" if cast_ctr[0] % 2 else "act"

        def do_copy(eng, out, in_, reads, writes):
            if eng == "act":
                P.op("act", lambda e: e.copy(out=out, in_=in_), reads=reads, writes=writes)
            else:
                P.op(eng, lambda e: e.tensor_copy(out=out, in_=in_), reads=reads, writes=writes)

        nwT = sb("nwT", [128, 4], F32)
        t_nwT = P.tok()
        gbias = sb("gbias", [128, 16], F32)
        t_gbias = P.tok()
        modT = sb("modT", [128, 6, 8, 2], F32)
        t_modT = P.tok("modT")
        gb = sb("gb", [128, 2, D], F32)
        t_gb = P.tok("gb")
        lnw = sb("lnw", [128, D], F32)
        lnb = sb("lnb", [128, D], F32)
        t_lnw, t_lnb = P.tok(), P.tok()
        rw = sb("rw", [128, 8, NE], F32)
        t_rw = P.tok()
        rbb = sb("rbb", [128, NE], F32)
        t_rbb = P.tok()
        bguT = sb("bguT", [128, 16, NE], F32)
        t_bguT = P.tok()
        bdn = sb("bdn", [NE, D], F32)
        t_bdn = P.tok()
        zscr = nc.dram_tensor("zscr", [2048, 512], BF16)
        t_zscr = P.tok("zscr")
        dscr = nc.dram_tensor("dscr", [16, 128, 528], F32)
        t_dscr = P.tok("dscr")
        Sst = sb("Sst", [128, 8, 129], F32)
        t_S = P.toks(8, "S")
        Sbf = sb("Sbf", [128, 8, 129], BF16)
        t_Sbf = P.toks(8, "Sbf")
        smb = sb("smb", [128, 8], F32)
        t_smb = P.tok()
        arena = sb("arena", [128, 16384], BF16)
        uT = arena[:, 0:4096].rearrange("p (a b) -> p a b", a=8)
        t_uT = P.toks(4, "uT")
        qT = arena[:, 4096:6144].rearrange("p (a b) -> p a b", a=4)
        kT = arena[:, 6144:8192].rearrange("p (a b) -> p a b", a=4)
        t_qT, t_kT = P.tok("qT"), P.tok("kT")
        ktm = arena[:, 12288:14336].rearrange("p (a b) -> p a b", a=4)
        t_ktm = P.toks(4, "ktm")
        vext = sb("vext", [128, 4, 4, 129], BF16)
        t_vext = P.toks(4, "vext")
        vsc = sb("vsc", [128, 2, 4, 129], BF16)
        t_vsc = P.tok("vsc")
        og = arena[:, 14336:16384].rearrange("p (a b) -> p a b", a=4)
        t_og = P.toks(4, "og")
        zown = sb("zown", [128, 2, 512], BF16)
        t_zown = P.toks(2, "zown")
        zst, t_zst = zown, t_zown
        gt = sb("gt", [128, 4, 6, 8], F32)
        mst = sb("mst", [128, 8], F32)
        t_mst = P.tok("mst")
        scr = sb("scr", [128, 512], F32)
        t_scr = P.tok("scr")
        nd = sb("nd", [128, 2, 129], F32)
        t_nd = P.toks(2, "nd")
        t_gt = P.toks(4, "gt")
        STx = sb("STx", [128, 2, 4, 128], BF16)
        t_STx = P.toks(2, "STx")
        arena2 = sb("arena2", [128, 2048], F32)
        hacc = arena2[:, :].rearrange("p (a b) -> p a b", a=4)
        t_hacc = P.toks(4, "hacc")
        hmT = arena[:, 8192:12288].rearrange("p (a b) -> p a b", a=8)
        t_hmT = P.toks(4, "hmT")
        t_fmT = P.tok("fmT")
        small = sb("small", [128, 64], F32)
        t_small = P.tok("small")
        xt = ptmp
        t_xt = t_ptmp
        xtb = sb("xtb", [128, 1, D], BF16)
        t_xtb = P.toks(1, "xtb")
        dtab = sb("dtab", [128, 2, 2, 512], BF16)
        t_dtab = P.toks(2, "dtab")
        p12 = sb("p12", [128, 2, 2, 512], BF16)
        t_p12 = P.toks(2, "p12")

        u2T = sb("u2T", [128, 8, 1024], BF16)
        t_u2T = P.toks(8, "u2T")
        u2f = sb("u2f", [128, 8, 128], F32)
        t_u2f = P.tok("u2f")
        Gall = sb("Gall", [128, 8, NE], F32)
        t_G = P.toks(8, "G")
        arena3 = sb("arena3", [128, 1032], F32)
        GT = arena3[0:NE, 0:1024]
        t_GT = P.toks(8, "GT")
        WB_N = 2
        wb = [sb(f"wb{i}", [128, 8, 512], BF16) for i in range(WB_N)]
        t_wb = P.toks(WB_N, "wb")
        wb_ctr = [0]
        gsb = arena[:, 8192:12288].rearrange("p (a b) -> p a b", a=4)
        t_gs = P.toks(8, "gs")
        actT = arena[:, 0:8192].rearrange("p (a b) -> p a b", a=8)
        t_actT = [P.toks(2, "actT") for _ in range(8)]
        tmpA = arena2[:, 0:1024].rearrange("p (a b) -> p a b", a=2)
        t_tmpA = P.toks(2, "tmpA")
        tmpB = sb("tmpB", [128, 2, 512], BF16)
        t_tmpB = P.toks(2, "tmpB")
        tmpC = arena2[:, 1024:2048].rearrange("p (a b) -> p a b", a=2)
        t_tmpC = P.toks(2, "tmpC")

        SCALE_K = DH ** -0.5

        def load_cast(dst, t_dst, src2d, ncols, scale_ap=None, scale_rows=0):
            c0 = 0
            while c0 < ncols:
                w = min(256, ncols - c0)
                s_t, s_k = stage_load_w(src2d, c0, w)
                eng = cast_eng()
                if scale_ap is None:
                    do_copy(eng, dst[:, :, c0:c0 + w], s_t[:, :, 0:w], [s_k], [t_dst])
                else:
                    for kc in range(8):
                        if kc < scale_rows:
                            P.op("pool", lambda e, kc=kc, c0=c0, w=w, s_t=s_t: e.tensor_scalar(
                                out=dst[:, kc, c0:c0 + w], in0=s_t[:, kc, 0:w], scalar1=scale_ap[:, kc:kc + 1], scalar2=None,
                                op0=ALU.mult), reads=[s_k, t_nwT], writes=[t_dst])
                        else:
                            do_copy("pool", dst[:, kc, c0:c0 + w], s_t[:, kc, 0:w], [s_k], [t_dst])
                c0 += w

        def stage_load_w(src2d, c0, w):
            i = stg_ctr[0] % STG_N
            stg_ctr[0] += 1
            P.dma("sp", lambda e: e.dma_start(out=stg[i][:, :, 0:w], in_=src2d[:, c0:c0 + w].rearrange("(kc p) c -> p kc c", p=128)),
                  writes=[t_stg[i]])
            return stg[i], t_stg[i]

        BIG = 30000.0

        def gate_math(pg, t_pg, slot):
            G_ = gt[:, slot]
            tk = t_gt[slot]
            P.op("dve", lambda e: e.tensor_tensor(out=small[:, 0:16], in0=pg, in1=gbias[:], op=ALU.add),
                 reads=[t_pg, t_gbias], writes=[t_small])
            P.op("act", lambda e: e.activation(out=small[:, 16:20], in_=small[:, 4:8], func=AF.Exp, scale=-1.0), reads=[t_small], writes=[t_small])
            P.op("act", lambda e: e.activation(out=small[:, 20:24], in_=small[:, 12:16], func=AF.Exp, scale=-1.0), reads=[t_small], writes=[t_small])
            P.op("act", lambda e: e.activation(out=small[:, 24:32], in_=small[:, 16:24], func=AF.Ln, bias=1.0), reads=[t_small], writes=[t_small])
            P.op("dve", lambda e: e.tensor_scalar(out=small[:, 48:56], in0=small[:, 24:32], scalar1=-1.0, scalar2=None, op0=ALU.mult),
                 reads=[t_small], writes=[t_small])
            P.op("dve", lambda e: e.tensor_copy(out=small[:, 56:60], in_=small[:, 0:4]), reads=[t_small], writes=[t_small])
            P.op("dve", lambda e: e.tensor_copy(out=small[:, 60:64], in_=small[:, 8:12]), reads=[t_small], writes=[t_small])
            pb, tpb = bank()
            P.op("pe", lambda e: e.matmul(pb[:, 0:4], lhsT=triU[:], rhs=small[:, 48:52], start=True, stop=True), reads=[t_small, t_triU], writes=[tpb])
            P.op("pe", lambda e: e.matmul(pb[:, 4:8], lhsT=triL[:], rhs=small[:, 52:56], start=True, stop=True), reads=[t_small, t_triL], writes=[tpb])
            P.op("pe", lambda e: e.matmul(pb[:, 8:16], lhsT=ones_f[:], rhs=small[:, 48:56], start=True, stop=True), reads=[t_small, t_ones], writes=[tpb])
            P.op("dve", lambda e: e.tensor_tensor(out=G_[:, 0, :], in0=small[:, 56:64], in1=pb[:, 0:8], op=ALU.subtract), reads=[t_small, tpb], writes=[tk])
            P.op("act", lambda e: e.copy(out=G_[:, 1, :], in_=pb[:, 0:8]), reads=[tpb], writes=[tk])
            P.op("act", lambda e: e.copy(out=G_[:, 2, :], in_=pb[:, 8:16]), reads=[tpb], writes=[tk])
            for dr in range(2):
                for h in range(4):
                    P.op("dve", lambda e, dr=dr, h=h: e.tensor_scalar(out=hnb[:, h * 128:(h + 1) * 128], in0=ident_f[:], scalar1=G_[:, 0, dr * 4 + h:dr * 4 + h + 1], scalar2=None, op0=ALU.mult),
                         reads=[tk, t_idf], writes=[t_hnb])
                pq, tpq = bank()
                P.op("pe", lambda e, pq=pq: e.matmul(pq[:, 0:512], lhsT=ones_f[:], rhs=hnb[:, 0:512], start=True, stop=True), reads=[t_hnb, t_ones], writes=[tpq])
                P.op("dve", lambda e, dr=dr, pq=pq: e.tensor_reduce(out=G_[:, 3, dr * 4:dr * 4 + 4], in_=pq[:, 0:512].rearrange("p (h s) -> p h s", h=4), axis=mybir.AxisListType.X, op=ALU.max),
                     reads=[tpq], writes=[tk])
                nm_, tnm_ = (NML4, t_NML4) if dr == 0 else (NMU4, t_NMU4)
                P.op("dve", lambda e, pq=pq, nm_=nm_: e.tensor_tensor(out=scr[:, 0:512], in0=pq[:, 0:512], in1=nm_[:].rearrange("p h s -> p (h s)"), op=ALU.add),
                     reads=[tpq, tnm_], writes=[t_scr])
                P.op("dve", lambda e, dr=dr: e.tensor_reduce(out=G_[:, 5, dr * 4:dr * 4 + 4], in_=scr[:, 0:512].rearrange("p (h s) -> p h s", h=4), axis=mybir.AxisListType.X, op=ALU.max),
                     reads=[t_scr], writes=[tk])
            P.op("dve", lambda e: e.tensor_tensor(out=small[:, 40:48], in0=G_[:, 0, :], in1=G_[:, 3, :], op=ALU.subtract), reads=[tk], writes=[t_small])
            P.op("act", lambda e: e.activation(out=G_[:, 4, :], in_=small[:, 40:48], func=AF.Exp), reads=[t_small], writes=[tk])

        def scaled_v(slot, dr):
            for h in range(4):
                P.op("act", lambda e, h=h: e.activation(
                    out=vsc[:, dr, h, :], in_=vext[:, slot, h, :], func=AF.Identity, scale=gt[:, slot, 4, dr * 4 + h:dr * 4 + h + 1]),
                    reads=[t_vext[slot], t_gt[slot]], writes=[t_vsc])

        def d_matmuls(slot, dr):
            res = []
            pbs = [bank() for _ in range(2)]
            for h in range(4):
                pb, tpb = pbs[h // 3]
                o = (h % 3) * 132
                P.op("pe", lambda e, pb=pb, o=o, h=h: e.matmul(pb[:, o:o + 129], lhsT=ktm[:, slot, h * 128:(h + 1) * 128], rhs=vsc[:, dr, h, :], start=True, stop=True),
                     reads=[t_ktm[slot], t_vsc], writes=[tpb])
                res.append((pb[:, o:o + 129], tpb))
            return res

        def state_update(slot, dr, mask_col=None, t_mask=None):
            scaled_v(slot, dr)
            Dl = d_matmuls(slot, dr)
            c0 = dr * 4
            apply_update(dr, Dl, gt[:, slot, 3, c0:c0 + 4], gt[:, slot, 2, c0:c0 + 4], t_gt[slot], mask_col, t_mask)

        def apply_update(dr, Dl, Gm, BLc, t_src, mask_col=None, t_mask=None):
            c0 = dr * 4
            mcur = mst[:, c0:c0 + 4]
            if mask_col is not None:
                P.op("dve", lambda e: e.tensor_scalar(out=sm2[:, 0:4], in0=Gm, scalar1=BIG, scalar2=mask_col, op0=ALU.add, op1=ALU.mult), reads=[t_src, t_mask], writes=[t_sm2])
                P.op("dve", lambda e: e.tensor_scalar(out=sm2[:, 0:4], in0=sm2[:, 0:4], scalar1=-BIG, scalar2=None, op0=ALU.add), reads=[t_sm2], writes=[t_sm2])
                P.op("dve", lambda e: e.tensor_scalar(out=sm2[:, 4:8], in0=BLc, scalar1=mask_col, scalar2=None, op0=ALU.mult), reads=[t_src, t_mask], writes=[t_sm2])
            else:
                P.op("dve", lambda e: e.tensor_copy(out=sm2[:, 0:4], in_=Gm), reads=[t_src], writes=[t_sm2])
                P.op("dve", lambda e: e.tensor_copy(out=sm2[:, 4:8], in_=BLc), reads=[t_src], writes=[t_sm2])
            P.op("dve", lambda e: e.tensor_tensor(out=sm2[:, 8:12], in0=mcur, in1=sm2[:, 0:4], op=ALU.max), reads=[t_mst, t_sm2], writes=[t_sm2])
            P.op("dve", lambda e: e.tensor_tensor(out=sm2[:, 12:16], in0=mcur, in1=sm2[:, 8:12], op=ALU.subtract), reads=[t_mst, t_sm2], writes=[t_sm2])
            P.op("dve", lambda e: e.tensor_tensor(out=sm2[:, 16:20], in0=sm2[:, 0:4], in1=sm2[:, 8:12], op=ALU.subtract), reads=[t_sm2], writes=[t_sm2])
            P.op("act", lambda e: e.activation(out=sm2[:, 12:20], in_=sm2[:, 12:20], func=AF.Exp), reads=[t_sm2], writes=[t_sm2])
            for h in range(4):
                u = c0 + h
                dps, tdps = Dl[h]
                P.op("dve", lambda e, u=u, h=h: e.tensor_scalar(out=Sst[:, u, :], in0=Sst[:, u, :], scalar1=sm2[:, 12 + h:13 + h], scalar2=None, op0=ALU.mult),
                     reads=[t_S[u], t_sm2], writes=[t_S[u]])
                P.op("dve", lambda e, u=u, h=h, dps=dps: e.scalar_tensor_tensor(out=Sst[:, u, :], in0=dps, scalar=sm2[:, 16 + h:17 + h], in1=Sst[:, u, :], op0=ALU.mult, op1=ALU.add),
                     reads=[tdps, t_S[u], t_sm2], writes=[t_S[u]])
                P.op("act", lambda e, u=u: e.copy(out=Sbf[:, u, :], in_=Sst[:, u, :]), reads=[t_S[u]], writes=[t_Sbf[u]])
            P.op("dve", lambda e: e.tensor_tensor(out=mcur, in0=sm2[:, 4:8], in1=sm2[:, 8:12], op=ALU.add), reads=[t_sm2], writes=[t_mst])

        def make_uT(src_f32, t_src, cond, vec_shift, vec_scale, dst_fn, t_dst, want_f32=None, t_f32=None):
            xb_i = 0
            P.op("dve", lambda e: e.tensor_copy(out=xtb[:, xb_i, :], in_=src_f32), reads=[t_src], writes=[t_xtb[xb_i]])
            for half in range(2):
                pb, tpb = bank()
                pbb = pb[:].bitcast(BF16)
                for q in range(4):
                    kc = half * 4 + q
                    P.op("pe", lambda e, q=q, kc=kc, pbb=pbb: e.transpose(pbb[:, q * 128:(q + 1) * 128], xtb[:, xb_i, kc * 128:(kc + 1) * 128], ident_b[:]),
                         reads=[t_xtb[xb_i], t_idb], writes=[tpb])
                for q in range(4):
                    kc = half * 4 + q
                    P.op("act", lambda e, q=q, kc=kc, pbb=pbb: e.activation(
                        out=dst_fn(kc), in_=pbb[:, q * 128:(q + 1) * 128], func=AF.Identity,
                        scale=modT[:, vec_scale, kc, cond:cond + 1], bias=modT[:, vec_shift, kc, cond:cond + 1]),
                        reads=[tpb, t_modT], writes=[t_dst])

        def load_wpiece(src2d, c0, ncols, row_scale=False, eng=None, into=None):
            if into is None:
                i = wb_ctr[0] % WB_N
                wb_ctr[0] += 1
                buf, tk = wb[i], t_wb[i]
            else:
                buf, tk = into
            cc = 0
            while cc < ncols:
                w = min(256, ncols - cc)
                s_t, s_k = stage_load_w(src2d, c0 + cc, w)
                if not row_scale:
                    do_copy(eng or cast_eng(), buf[:, :, cc:cc + w], s_t[:, :, 0:w], [s_k], [tk])
                else:
                    do_copy("act", buf[:, 4:8, cc:cc + w], s_t[:, 4:8, 0:w], [s_k], [tk])
                    for kc in range(4):
                        P.op("dve", lambda e, kc=kc, cc=cc, w=w, s_t=s_t, buf=buf: e.tensor_scalar(
                            out=buf[:, kc, cc:cc + w], in0=s_t[:, kc, 0:w], scalar1=nwT[:, kc:kc + 1], scalar2=None, op0=ALU.mult),
                            reads=[s_k, t_nwT], writes=[tk])
                cc += w
            return buf, tk

        sm2 = sb("sm2", [128, 32], F32)
        t_sm2 = P.tok("sm2")
        st6 = sb("st6", [128, 4, 6], F32)
        t_st6 = P.tok("st6")
        Cout = arena3[:, :].rearrange("p (a b) -> p a b", a=8)
        t_Cout = P.tok("Cout")
        hnb = sb("hnb", [128, 512], F32)
        t_hnb = P.tok("hnb")
        hmb = sb("hmb", [128, 512], BF16)
        t_hmb = P.tok("hmb")
        lg = sb("lg", [128, 4, NE], F32)
        t_lg = P.tok("lg")
        P.op("pool", lambda e: e.memset(vext[:, :, :, 128:129], 1.0), writes=t_vext)

        def layer_norm_inplace(xi, tw, tb):
            xv = X[:, xi, :]
            for hf in range(2):
                P.op("dve", lambda e, hf=hf: e.bn_stats(out=st6[:, hf, :], in_=X[:, xi, hf * 512:(hf + 1) * 512]), reads=[tX[xi]], writes=[t_st6])
            P.op("dve", lambda e: e.bn_aggr(out=sm2[:, 0:2], in_=st6[:, 0:2, :].rearrange("p a b -> p (a b)")), reads=[t_st6], writes=[t_sm2])
            P.op("act", lambda e: e.activation(out=sm2[:, 2:3], in_=sm2[:, 1:2], func=AF.Sqrt, bias=eps_t[:], scale=1.0), reads=[t_sm2, t_ones], writes=[t_sm2])
            P.op("dve", lambda e: e.reciprocal(out=sm2[:, 3:4], in_=sm2[:, 2:3]), reads=[t_sm2], writes=[t_sm2])
            P.op("dve", lambda e: e.scalar_tensor_tensor(out=sm2[:, 4:5], in0=sm2[:, 0:1], scalar=-1.0, in1=sm2[:, 3:4], op0=ALU.mult, op1=ALU.mult),
                 reads=[t_sm2], writes=[t_sm2])
            P.op("act", lambda e: e.activation(out=xv, in_=xv, func=AF.Identity, scale=sm2[:, 3:4], bias=sm2[:, 4:5]), reads=[tX[xi], t_sm2], writes=[tX[xi]])
            P.op("dve", lambda e: e.tensor_tensor(out=xv, in0=xv, in1=lnw[:], op=ALU.mult), reads=[tX[xi], tw], writes=[tX[xi]])
            P.op("dve", lambda e: e.tensor_tensor(out=xv, in0=xv, in1=lnb[:], op=ALU.add), reads=[tX[xi], tb], writes=[tX[xi]])

        sTb = sb("sTb", [128, 16], BF16)
        t_sTb = P.tok("sTb")
        P.op("dve", lambda e: e.tensor_copy(out=sTb[:], in_=sT[:]), reads=[t_sT], writes=[t_sTb])

        def mod_vectors(l, vecs):
            pmT, t_pmT = PLONG, t_PLONG
            plist = [(v, sub) for v in vecs for sub in range(2)]
            nxt = load_wpiece(DI["w_ada"][l], (plist[0][0] * 2 + plist[0][1]) * 512, 512)
            for pi, (v, sub) in enumerate(plist):
                pc = v * 2 + sub
                wbuf, twb = nxt
                if pi + 1 < len(plist):
                    nxt = load_wpiece(DI["w_ada"][l], (plist[pi + 1][0] * 2 + plist[pi + 1][1]) * 512, 512)
                pr, tpr = bank()
                for kc in range(8):
                    P.op("pe", lambda e, kc=kc, wbuf=wbuf, pr=pr: e.matmul(pr[0:2, 0:512], lhsT=sTb[:, 2 * kc:2 * kc + 2], rhs=wbuf[:, kc, :],
                                                                          start=(kc == 0), stop=(kc == 7)), reads=[twb, t_sTb], writes=[tpr])
                mr = tmpA[0:2, pc % 2, :]
                tmr = t_tmpA[pc % 2]
                bp = tmpC[0:2, pc % 2, :]
                tbp = t_tmpC[pc % 2]
                P.dma("sp", lambda e, pc=pc, bp=bp: e.dma_start(out=bp, in_=DI["b_ada2"][l][:, pc * 512:(pc + 1) * 512]), writes=[tbp])
                P.op("dve", lambda e, pr=pr, mr=mr, bp=bp: e.tensor_tensor(out=mr, in0=pr[0:2, 0:512], in1=bp, op=ALU.add),
                     reads=[tpr, tbp], writes=[tmr])
                if v in (2, 5):
                    for cond in range(2):
                        pg_, tpg_ = bank()
                        P.op("pe", lambda e, cond=cond, pg_=pg_, mr=mr: e.matmul(pg_[:, 0:512], lhsT=selr[0:2, cond, :], rhs=mr, start=True, stop=True),
                             reads=[tmr, t_selr], writes=[tpg_])
                        P.op("act", lambda e, cond=cond, sub=sub, pg_=pg_: e.copy(out=gb[:, cond, sub * 512:(sub + 1) * 512], in_=pg_[:, 0:512]),
                             reads=[tpg_], writes=[t_gb])
                else:
                    for q in range(4):
                        ch = sub * 4 + q
                        c0 = (v * 8 + ch) * 2
                        P.op("pe", lambda e, c0=c0, q=q, mr=mr: e.transpose(pmT[:, c0:c0 + 2], mr[:, q * 128:(q + 1) * 128], ident_f[0:2, 0:2]),
                             reads=[tmr, t_idf], writes=[t_pmT])
            for v in vecs:
                if v in (2, 5):
                    continue
                if v in (1, 4):
                    P.op("dve", lambda e, v=v: e.tensor_scalar(out=modT[:, v].rearrange("p a b -> p (a b)"), in0=pmT[:, v * 16:(v + 1) * 16], scalar1=1.0, scalar2=None, op0=ALU.add),
                         reads=[t_pmT], writes=[t_modT])
                else:
                    P.op("dve", lambda e, v=v: e.tensor_copy(out=modT[:, v].rearrange("p a b -> p (a b)"), in_=pmT[:, v * 16:(v + 1) * 16]),
                         reads=[t_pmT], writes=[t_modT])

        def tm_proj(slot, wbuf, twb, ncols, evac):
            pb, tpb = bank()
            for kc in range(8):
                P.op("pe", lambda e, kc=kc, pb=pb, wbuf=wbuf: e.matmul(pb[:, 0:ncols], lhsT=uT[:, kc, slot * 128:(slot + 1) * 128], rhs=wbuf[:, kc, 0:ncols],
                                                                      start=(kc == 0), stop=(kc == 7)), reads=[t_uT[slot], twb], writes=[tpb])
            evac(pb, tpb)

        gpc = sb("gpc", [128, 8, 16], BF16)
        t_gpc = P.tok("gpc")
        zpc = arena[:, 8192:12288].rearrange("p (a b) -> p a b", a=8)
        t_zpc = P.tok("zpc")

        cur_l = [0]
        t_dstg = P.toks(2, "dstg")
        okey = {n: P.tok("o_" + n) for n in ("yp", "ys", "nC", "nn", "nm")}
        out_toks.extend(okey.values())

        def process_item(l, tiles, cond, is_sample, seq_out):
            T = len(tiles)
            N = 128 * T
            for tau, xi in enumerate(tiles):
                make_uT(X[:, xi, :], tX[xi], cond, 0, 1, lambda kc, tau=tau: uT[:, kc, tau * 128:(tau + 1) * 128], t_uT[tau])
            def ev_k(pb, tpb, tau):
                P.op("act", lambda e: e.mul(out=ktm[:, tau, :], in_=pb[:, 0:512], mul=SCALE_K), reads=[tpb], writes=[t_ktm[tau]])

            def ev_v(pb, tpb, tau):
                P.op("dve", lambda e: e.tensor_copy(out=vext[:, tau, :, 0:128], in_=pb[:, 0:512].rearrange("p (h d) -> p h d", h=4)),
                     reads=[tpb], writes=[t_vext[tau]])

            def ev_o(pb, tpb, tau):
                P.op("act", lambda e: e.activation(out=og[:, tau, :], in_=pb[:, 0:512], func=AF.Sigmoid), reads=[tpb], writes=[t_og[tau]])

            def ev_g(pb, tpb, tau):
                gate_math(pb[:, 0:16], tpb, tau)

            def ev_z(pb, tpb, tau):
                P.op("act", lambda e: e.copy(out=zown[:, tau, :], in_=pb[:, 0:512]), reads=[tpb], writes=[t_zown[tau]])

            plist = [("q", 0, 512, None), ("k", 512, 512, ev_k), ("v", 1024, 512, ev_v), ("o", 1536, 512, ev_o), ("g", G_OFF, 16, ev_g)]
            if not is_sample:
                plist.append(("z", F_OFF, 512, ev_z))
            nxt = load_wpiece(DI["w_in"][l], plist[0][1], plist[0][2])
            for pi, (pname, col0, ncols, evac) in enumerate(plist):
                wbuf, twb = nxt
                if pi + 1 < len(plist):
                    nxt = load_wpiece(DI["w_in"][l], plist[pi + 1][1], plist[pi + 1][2])
                if pname in ("q", "k"):
                    for h in range(4):
                        pb, tpb = bank()
                        for kc in range(8):
                            P.op("pe", lambda e, kc=kc, h=h, pb=pb, wbuf=wbuf: e.matmul(pb[:, 0:N], lhsT=wbuf[:, kc, h * 128:(h + 1) * 128], rhs=uT[:, kc, 0:N],
                                                                                       start=(kc == 0), stop=(kc == 7)), reads=t_uT[0:T] + [twb], writes=[tpb])
                        if pname == "q":
                            P.op("act", lambda e, h=h, pb=pb: e.copy(out=qT[:, h, 0:N], in_=pb[:, 0:N]), reads=[tpb], writes=[t_qT])
                        else:
                            P.op("act", lambda e, h=h, pb=pb: e.mul(out=kT[:, h, 0:N], in_=pb[:, 0:N], mul=SCALE_K), reads=[tpb], writes=[t_kT])
                if evac is not None:
                    for tau in range(T):
                        tm_proj(tau, wbuf, twb, ncols, lambda pb, tpb, tau=tau, evac=evac: evac(pb, tpb, tau))
            if not is_sample:
                P.op("pool", lambda e: e.memset(Sst[:], 0.0), writes=t_S)
                P.op("pool", lambda e: e.memset(Sbf[:], 0.0), writes=t_Sbf)
                P.op("pool", lambda e: e.memset(mst[:], 0.0), writes=[t_mst])
            else:
                for u in range(8):
                    P.op("act", lambda e, u=u: e.copy(out=Sbf[:, u, :], in_=Sst[:, u, :]), reads=[t_S[u]], writes=[t_Sbf[u]])
            for dr in range(2):
                order = list(range(T)) if dr == 0 else list(range(T - 1, -1, -1))
                nm_, tnm_ = (NMU4, t_NMU4) if dr == 0 else (NML4, t_NML4)
                c0 = dr * 4
                for oi, tau in enumerate(order):
                    sx = (dr * T + oi) % 2
                    G_ = gt[:, tau]
                    P.op("dve", lambda e, G_=G_: e.tensor_tensor(out=sm2[:, 20:24], in0=G_[:, 5, c0:c0 + 4], in1=mst[:, c0:c0 + 4], op=ALU.max), reads=[t_gt[tau], t_mst], writes=[t_sm2])
                    P.op("dve", lambda e: e.tensor_tensor(out=sm2[:, 24:28], in0=mst[:, c0:c0 + 4], in1=sm2[:, 20:24], op=ALU.subtract), reads=[t_mst, t_sm2], writes=[t_sm2])
                    P.op("dve", lambda e, G_=G_: e.scalar_tensor_tensor(out=sm2[:, 28:32], in0=G_[:, 1, c0:c0 + 4], scalar=-1.0, in1=sm2[:, 20:24], op0=ALU.mult, op1=ALU.subtract),
                         reads=[t_gt[tau], t_sm2], writes=[t_sm2])
                    P.op("act", lambda e: e.activation(out=sm2[:, 24:32], in_=sm2[:, 24:32], func=AF.Exp), reads=[t_sm2], writes=[t_sm2])
                    for h in range(4):
                        P.op("dve", lambda e, h=h: e.tensor_scalar(out=hnb[:, h * 128:(h + 1) * 128], in0=ident_f[:], scalar1=sm2[:, 20 + h:21 + h], scalar2=-1.0, op0=ALU.mult, op1=ALU.mult),
                             reads=[t_sm2, t_idf], writes=[t_hnb])
                    pr_, tpr_ = bank()
                    P.op("pe", lambda e, pr_=pr_: e.matmul(pr_[:, 0:512], lhsT=ones_f[:], rhs=hnb[:, 0:512], start=True, stop=False), reads=[t_hnb, t_ones], writes=[tpr_])
                    P.op("pe", lambda e, pr_=pr_, nm_=nm_: e.matmul(pr_[:, 0:512], lhsT=ident_f[:], rhs=nm_[:].rearrange("p h s -> p (h s)"), start=False, stop=True),
                         reads=[tnm_, t_idf], writes=[tpr_])
                    for h in range(4):
                        P.op("act", lambda e, h=h, pr_=pr_, G_=G_: e.activation(out=scr[:, h * 128:(h + 1) * 128], in_=pr_[:, h * 128:(h + 1) * 128], func=AF.Exp,
                                                                           bias=G_[:, 0, c0 + h:c0 + h + 1], scale=1.0), reads=[tpr_, t_gt[tau]], writes=[t_scr])
                    pst, tpst = bank()
                    for h in range(4):
                        P.op("pe", lambda e, h=h, pst=pst: e.matmul(pst[:, h * 128:(h + 1) * 128], lhsT=kT[:, h, tau * 128:(tau + 1) * 128],
                                                                   rhs=qT[:, h, tau * 128:(tau + 1) * 128], start=True, stop=True),
                             reads=[t_kT, t_qT], writes=[tpst])
                    P.op("dve", lambda e, pst=pst: e.tensor_tensor(out=STx[:, sx].rearrange("p h t -> p (h t)"), in0=pst[:, 0:512], in1=scr[:, 0:512], op=ALU.mult),
                         reads=[tpst, t_scr], writes=[t_STx[sx]])
                    for h in range(4):
                        u = c0 + h
                        ni = h % 2
                        po, tpo = bank()
                        P.op("pe", lambda e, h=h, po=po: e.matmul(po[:, 0:129], lhsT=STx[:, sx, h, :], rhs=vext[:, tau, h, :], start=True, stop=True),
                             reads=[t_STx[sx], t_vext[tau]], writes=[tpo])
                        P.op("pe", lambda e, h=h, u=u, po=po: e.matmul(po[:, 132:261], lhsT=qT[:, h, tau * 128:(tau + 1) * 128], rhs=Sbf[:, u, :], start=True, stop=True),
                             reads=[t_qT, t_Sbf[u]], writes=[tpo])
                        P.op("act", lambda e, h=h, po=po, ni=ni: e.activation(out=nd[:, ni, :], in_=po[:, 132:261], func=AF.Identity, scale=sm2[:, 24 + h:25 + h]),
                             reads=[tpo, t_sm2], writes=[t_nd[ni]])
                        P.op("dve", lambda e, po=po, ni=ni: e.tensor_tensor(out=nd[:, ni, :], in0=po[:, 0:129], in1=nd[:, ni, :], op=ALU.add), reads=[tpo, t_nd[ni]], writes=[t_nd[ni]])
                        P.op("dve", lambda e, ni=ni: e.scalar_tensor_tensor(out=sm2[:, 8:9], in0=nd[:, ni, 128:129], scalar=-1.0, in1=nd[:, ni, 128:129], op0=ALU.mult, op1=ALU.max),
                             reads=[t_nd[ni]], writes=[t_sm2])
                        P.op("dve", lambda e, h=h: e.tensor_tensor(out=sm2[:, 9:10], in0=sm2[:, 8:9], in1=sm2[:, 28 + h:29 + h], op=ALU.max), reads=[t_sm2], writes=[t_sm2])
                        P.op("dve", lambda e: e.reciprocal(out=sm2[:, 10:11], in_=sm2[:, 9:10]), reads=[t_sm2], writes=[t_sm2])
                        if dr == 0:
                            P.op("dve", lambda e, h=h, ni=ni: e.tensor_scalar(out=hacc[:, tau, h * 128:(h + 1) * 128], in0=nd[:, ni, 0:128], scalar1=sm2[:, 10:11], scalar2=None, op0=ALU.mult),
                                 reads=[t_nd[ni], t_sm2], writes=[t_hacc[tau]])
                        else:
                            P.op("dve", lambda e, h=h, ni=ni: e.scalar_tensor_tensor(out=hacc[:, tau, h * 128:(h + 1) * 128], in0=nd[:, ni, 0:128], scalar=sm2[:, 10:11],
                                                                                 in1=hacc[:, tau, h * 128:(h + 1) * 128], op0=ALU.mult, op1=ALU.add),
                                 reads=[t_nd[ni], t_sm2, t_hacc[tau]], writes=[t_hacc[tau]])
                    if (not is_sample) or oi < T - 1:
                        state_update(tau, dr)
                if _DBG and l == 0 and seq_out == 1:
                    tkh = P.tok("dbgh")
                    out_toks.append(tkh)
                    P.dma("sp", lambda e, dr=dr: e.dma_start(out=DO["dbg_h1" if dr == 0 else "dbg_h2"], in_=hacc[:, 0:2, :]), reads=t_hacc[0:2], writes=[tkh])
                    if dr == 0:
                        for nm2, src, rd in [("dbg_stx", STx[:], t_STx), ("dbg_scr", scr[:], [t_scr]), ("dbg_nd", nd[:], t_nd), ("dbg_sm2", sm2[:], [t_sm2])]:
                            tkq = P.tok(nm2)
                            out_toks.append(tkq)
                            P.dma("sp", lambda e, nm2=nm2, src=src: e.dma_start(out=DO[nm2], in_=src), reads=rd, writes=[tkq])
                        tkq = P.tok("dbgqk")
                        out_toks.append(tkq)
                        P.dma("sp", lambda e: e.dma_start(out=DO["dbg_qk"][:, 0], in_=qT[:, :, 0:256]), reads=[t_qT], writes=[tkq])
                        P.dma("sp", lambda e: e.dma_start(out=DO["dbg_qk"][:, 1], in_=kT[:, :, 0:256]), reads=[t_kT], writes=[tkq])
                        tkg = P.tok("dbgg")
                        out_toks.append(tkg)
                        P.dma("sp", lambda e: e.dma_start(out=DO["dbg_gt"], in_=gt[:, 0:2].rearrange("p a b c -> p a (b c)")), reads=t_gt[0:2], writes=[tkg])
            if not is_sample:
                P.dma("sp", lambda e: e.dma_start(out=DO["nm"][seq_out, l].rearrange("(o u) -> o u", o=1), in_=mst[0:1, 0:8]), reads=[t_mst], writes=[], key=okey["nm"])
                P.dma("sp", lambda e: e.dma_start(out=DO["nC"][seq_out, l].rearrange("u d e -> d u e"), in_=Sst[:, :, 0:128]), reads=t_S, writes=[], key=okey["nC"])
                P.dma("sp", lambda e: e.dma_start(out=DO["nn"][seq_out, l].rearrange("u d -> d u"), in_=Sst[:, :, 128], allow_slow_non_contiguous=True), reads=t_S, writes=[], key=okey["nn"])
            for tau in range(T):
                for h in range(4):
                    P.op("dve", lambda e, h=h: e.bn_stats(out=st6[:, h, :], in_=hacc[:, tau, h * 128:(h + 1) * 128]), reads=[t_hacc[tau]], writes=[t_st6])
                for h in range(4):
                    P.op("dve", lambda e, h=h: e.bn_aggr(out=sm2[:, 2 * h:2 * h + 2], in_=st6[:, h, :]), reads=[t_st6], writes=[t_sm2])
                P.op("act", lambda e: e.activation(out=sm2[:, 8:12], in_=sm2[:, 0:8].rearrange("p (h t) -> p h t", t=2)[:, :, 1], func=AF.Sqrt, bias=eps_t[:], scale=1.0),
                     reads=[t_sm2, t_ones], writes=[t_sm2])
                P.op("dve", lambda e: e.reciprocal(out=sm2[:, 12:16], in_=sm2[:, 8:12]), reads=[t_sm2], writes=[t_sm2])
                P.op("dve", lambda e: e.scalar_tensor_tensor(out=sm2[:, 16:20], in0=sm2[:, 0:8].rearrange("p (h t) -> p h t", t=2)[:, :, 0], scalar=-1.0, in1=sm2[:, 12:16],
                                                             op0=ALU.mult, op1=ALU.mult), reads=[t_sm2], writes=[t_sm2])
                for h in range(4):
                    P.op("act", lambda e, h=h: e.activation(out=hnb[:, h * 128:(h + 1) * 128], in_=hacc[:, tau, h * 128:(h + 1) * 128], func=AF.Identity,
                                                            scale=sm2[:, 12 + h:13 + h], bias=sm2[:, 16 + h:17 + h]), reads=[t_hacc[tau], t_sm2], writes=[t_hnb])
                P.op("dve", lambda e: e.tensor_tensor(out=hmb[:], in0=hnb[:], in1=og[:, tau, :], op=ALU.mult), reads=[t_hnb, t_og[tau]], writes=[t_hmb])
                pb, tpb = bank()
                pbb = pb[:].bitcast(BF16)
                for h in range(4):
                    P.op("pe", lambda e, h=h, pbb=pbb: e.transpose(pbb[:, h * 128:(h + 1) * 128], hmb[:, h * 128:(h + 1) * 128], ident_b[:]), reads=[t_hmb, t_idb], writes=[tpb])
                P.op("act", lambda e, pbb=pbb: e.copy(out=hmT[:, 0:4, tau * 128:(tau + 1) * 128], in_=pbb[:, 0:512].rearrange("p (h t) -> p h t", h=4)),
                     reads=[tpb], writes=[t_hmT[tau]])
            if not is_sample:
                for g in range(4):
                    pbs = []
                    for cs, (tab, ttab) in enumerate(((c256, t_c256), (s256, t_s256))):
                        pb, tpb = bank()
                        for mc in range(2):
                            P.op("pe", lambda e, mc=mc, g=g, pb=pb, tab=tab: e.matmul(pb[:, 0:256], lhsT=zown[:, mc, g * 128:(g + 1) * 128], rhs=tab[:, mc, :],
                                                                                     start=(mc == 0), stop=(mc == 1)), reads=[t_zown[mc], ttab], writes=[tpb])
                        P.op("act" if cs else "dve", (lambda e, cs=cs, g=g, pb=pb: e.copy(out=p12[:, g % 2, cs, 0:256], in_=pb[:, 0:256])) if cs else
                             (lambda e, cs=cs, g=g, pb=pb: e.tensor_copy(out=p12[:, g % 2, cs, 0:256], in_=pb[:, 0:256])), reads=[tpb], writes=[t_p12[g % 2]])
                    py, tpy = bank()
                    P.op("pe", lambda e, g=g, py=py: e.matmul(py[:, 0:256], lhsT=consts["ccp"][0][:], rhs=p12[:, g % 2, 0, 0:256], start=True, stop=False),
                         reads=[t_p12[g % 2], consts["ccp"][1]], writes=[tpy])
                    P.op("pe", lambda e, g=g, py=py: e.matmul(py[:, 0:256], lhsT=consts["nscp"][0][:], rhs=p12[:, g % 2, 1, 0:256], start=False, stop=True),
                         reads=[t_p12[g % 2], consts["nscp"][1]], writes=[tpy])
                    P.op("act", lambda e, g=g, py=py: e.copy(out=hmT[:, 4 + g, 0:256], in_=py[:, 0:256]), reads=[tpy], writes=[t_fmT] + t_hmT[0:2])
            else:
                for gp in range(2):
                    pbs = [bank() for _ in range(4)]
                    for mc in range(16):
                        bi = mc % 2
                        P.dma("sp", lambda e, mc=mc, bi=bi: e.dma_start(out=dtab[:, bi, 0, :], in_=DI["cn_own"][mc * 128:(mc + 1) * 128, :]), writes=[t_dtab[bi]])
                        P.dma("sp", lambda e, mc=mc, bi=bi: e.dma_start(out=dtab[:, bi, 1, :], in_=DI["sn_own"][mc * 128:(mc + 1) * 128, :]), writes=[t_dtab[bi]])
                        P.dma("sp", lambda e, mc=mc, bi=bi: e.dma_start(out=zst[:, bi, :], in_=zscr.ap()[mc * 128:(mc + 1) * 128, :]), reads=[t_zscr], writes=[t_zst[bi]])
                        for gl in range(2):
                            g = gp * 2 + gl
                            for cs in range(2):
                                pb, tpb = pbs[gl * 2 + cs]
                                P.op("pe", lambda e, mc=mc, g=g, cs=cs, pb=pb, bi=bi: e.matmul(pb[:, 0:512], lhsT=zst[:, bi, g * 128:(g + 1) * 128], rhs=dtab[:, bi, cs, :],
                                                                                              start=(mc == 0), stop=(mc == 15)), reads=[t_zst[bi], t_dtab[bi]], writes=[tpb])
                    for gl in range(2):
                        g = gp * 2 + gl
                        for cs in range(2):
                            pb, tpb = pbs[gl * 2 + cs]
                            if cs:
                                P.op("act", lambda e, gl=gl, cs=cs, pb=pb: e.copy(out=p12[:, gl, cs, :], in_=pb[:, 0:512]), reads=[tpb], writes=[t_p12[gl]])
                            else:
                                P.op("dve", lambda e, gl=gl, cs=cs, pb=pb: e.tensor_copy(out=p12[:, gl, cs, :], in_=pb[:, 0:512]), reads=[tpb], writes=[t_p12[gl]])
                        py, tpy = bank()
                        P.op("pe", lambda e, gl=gl, py=py: e.matmul(py[:, 0:512], lhsT=consts["ccs"][0][:], rhs=p12[:, gl, 0, :], start=True, stop=False),
                             reads=[t_p12[gl], consts["ccs"][1]], writes=[tpy])
                        P.op("pe", lambda e, gl=gl, py=py: e.matmul(py[:, 0:512], lhsT=consts["nscs"][0][:], rhs=p12[:, gl, 1, :], start=False, stop=True),
                             reads=[t_p12[gl], consts["nscs"][1]], writes=[tpy])
                        P.op("act", lambda e, g=g, py=py: e.copy(out=hmT[:, 4 + g, 0:512], in_=py[:, 0:512]), reads=[tpy], writes=[t_fmT] + t_hmT)
            if _DBG and l == 0 and seq_out == 1:
                tkd = P.tok("dbgmix")
                out_toks.append(tkd)
                P.dma("sp", lambda e: e.dma_start(out=DO["dbg_mix"], in_=hmT[:, :, :]), reads=t_hmT + [t_fmT], writes=[tkd])
            wo = [load_wpiece(DI["w_out"][l], hf * 512, 512, row_scale=True) for hf in range(2)]
            for tau, xi in enumerate(tiles):
                for hf in range(2):
                    wbuf, twb = wo[hf]
                    pb, tpb = bank()
                    for fc in range(8):
                        P.op("pe", lambda e, fc=fc, pb=pb, wbuf=wbuf: e.matmul(pb[:, 0:512], lhsT=hmT[:, fc, tau * 128:(tau + 1) * 128], rhs=wbuf[:, fc, :],
                                                                              start=(fc == 0), stop=(fc == 7)), reads=[t_hmT[tau], t_fmT, twb], writes=[tpb])
                    P.op("dve", lambda e, hf=hf, pb=pb: e.tensor_tensor(out=xt[:, 0, hf * 512:(hf + 1) * 512], in0=pb[:, 0:512], in1=gb[:, cond, hf * 512:(hf + 1) * 512], op=ALU.mult),
                         reads=[tpb, t_gb], writes=[t_xt[0]])
                P.op("dve", lambda e, xi=xi: e.scalar_tensor_tensor(out=X[:, xi, :], in0=X[:, xi, :], scalar=ALPHA, in1=xt[:, 0, :], op0=ALU.mult, op1=ALU.add),
                     reads=[tX[xi], t_xt[0]], writes=[tX[xi]])
                layer_norm_inplace(xi, t_lnw, t_lnb)

        for l in range(NL):
            cur_l[0] = l
            if l > 0:
                moe_toks = t_gs + [t for pair in t_actT for t in pair]
                for tk in t_uT + [t_qT, t_kT, t_fmT] + t_hmT + t_ktm + t_og:
                    P.alias(tk, moe_toks)
                P.alias(t_Cout, t_GT)
            P.dma("sp", lambda e, l=l: e.dma_start(out=nwT[:], in_=DI["nwT"][l]), writes=[t_nwT])
            P.dma("sp", lambda e, l=l: e.dma_start(out=gbias[:], in_=DI["gbias"][l]), writes=[t_gbias])
            P.dma("sp", lambda e, l=l: e.dma_start(out=rw[:], in_=DI["router_w"][l].rearrange("(kc p) n -> p kc n", p=128)), writes=[t_rw])
            P.dma("sp", lambda e, l=l: e.dma_start(out=rbb[:], in_=DI["rbb"][l]), writes=[t_rbb])
            P.dma("sp", lambda e, l=l: e.dma_start(out=bguT[:], in_=DI["bguT"][l]), writes=[t_bguT])
            P.dma("sp", lambda e, l=l: e.dma_start(out=bdn[:], in_=DI["b_down"][l]), writes=[t_bdn])
            P.dma("sp", lambda e, l=l: e.dma_start(out=smb[:], in_=DI["smb"][l]), writes=[t_smb])
            P.dma("sp", lambda e, l=l: e.dma_start(out=lnw[:], in_=DI["ln1w"][l]), writes=[t_lnw])
            P.dma("sp", lambda e, l=l: e.dma_start(out=lnb[:], in_=DI["ln1b"][l]), writes=[t_lnb])
            mod_vectors(l, [0, 1, 2])

            P.dma("sp", lambda e, l=l: e.dma_start(out=Sst[:, :, 0:128], in_=DI["sC"][l].rearrange("u d e -> d u e")), writes=t_S)
            P.dma("sp", lambda e, l=l: e.dma_start(out=sm2[:, 20:28], in_=DI["sn"][l]), writes=[t_sm2])
            P.op("dve", lambda e: e.tensor_copy(out=Sst[:, :, 128], in_=sm2[:, 20:28]), reads=[t_sm2], writes=t_S)
            P.op("dve", lambda e: e.tensor_copy(out=mst[:], in_=smb[:]), reads=[t_smb], writes=[t_mst])
            kpc = load_wpiece(DI["w_in"][l], 512, 512, into=(wb[0], t_wb[0]))
            vpc = load_wpiece(DI["w_in"][l], 1024, 512, into=(wb[1], t_wb[1]))
            gpiece = load_wpiece(DI["w_in"][l], G_OFF, 16, into=(gpc, t_gpc))
            P.alias(t_zpc, t_hmT + [t_fmT] + t_gs)
            zpiece = load_wpiece(DI["w_in"][l], F_OFF, 512, into=(zpc, t_zpc))
            steps = [(0, c) for c in range(16)]
            dstg = arena[:, 4096:8192].bitcast(F32)
            for tkd in t_dstg:
                P.alias(tkd, [t_qT, t_kT])

            def front(gi):
                sp_dir, c = steps[gi]
                slot = gi % 4
                xs = gi % 2
                if l == 0:
                    P.dma("sp", lambda e: e.dma_start(out=xt[:, xs, :], in_=DI["xs_full"][c * 128:(c + 1) * 128, :]), writes=[t_xt[xs]])
                    for hf in range(2):
                        P.dma("sp", lambda e, hf=hf: e.dma_start(out=scr[:, :], in_=DI["pos_full"][c * 128:(c + 1) * 128, hf * 512:(hf + 1) * 512]), writes=[t_scr])
                        P.op("dve", lambda e, hf=hf: e.tensor_tensor(out=xt[:, xs, hf * 512:(hf + 1) * 512], in0=xt[:, xs, hf * 512:(hf + 1) * 512], in1=scr[:], op=ALU.add),
                             reads=[t_xt[xs], t_scr], writes=[t_xt[xs]])
                else:
                    P.dma("sp", lambda e: e.dma_start(out=xt[:, xs, :], in_=gouts[c % 4].ap()[(c // 4) * 128:(c // 4 + 1) * 128, :]), reads=[t_gout[c % 4]], writes=[t_xt[xs]])
                make_uT(xt[:, xs, :], t_xt[xs], 1, 0, 1, lambda kc: uT[:, kc, slot * 128:(slot + 1) * 128], t_uT[slot])

                def ev_k(pb, tpb):
                    P.op("act", lambda e: e.mul(out=ktm[:, slot, :], in_=pb[:, 0:512], mul=SCALE_K), reads=[tpb], writes=[t_ktm[slot]])

                def ev_v(pb, tpb):
                    P.op("dve", lambda e: e.tensor_copy(out=vext[:, slot, :, 0:128], in_=pb[:, 0:512].rearrange("p (h d) -> p h d", h=4)),
                         reads=[tpb], writes=[t_vext[slot]])

                def ev_g(pb, tpb):
                    gate_math(pb[:, 0:16], tpb, slot)

                def ev_z(pb, tpb):
                    zb = c % 2
                    P.op("act", lambda e: e.copy(out=zown[:, zb, :], in_=pb[:, 0:512]), reads=[tpb], writes=[t_zown[zb]])
                    P.dma("sp", lambda e: e.dma_start(out=zscr.ap()[c * 128:(c + 1) * 128, :], in_=zown[:, zb, :]), reads=[t_zown[zb]], writes=[t_zscr])

                tm_proj(slot, kpc[0], kpc[1], 512, ev_k)
                tm_proj(slot, vpc[0], vpc[1], 512, ev_v)
                tm_proj(slot, gpiece[0], gpiece[1], 16, ev_g)
                tm_proj(slot, zpiece[0], zpiece[1], 512, ev_z)

            def back(gi):
                sp_dir, c = steps[gi]
                slot = gi % 4
                state_update(slot, 0, maskf[:, c:c + 1], t_maskf)
                bi = gi % 2
                scaled_v(slot, 1)
                Dl = d_matmuls(slot, 1)
                for h in range(4):
                    dps, tdps = Dl[h]
                    P.op("act", lambda e, h=h, dps=dps: e.copy(out=dstg[:, bi * 528 + h * 129:bi * 528 + (h + 1) * 129], in_=dps), reads=[tdps], writes=[t_dstg[bi]])
                P.op("dve", lambda e: e.tensor_copy(out=dstg[:, bi * 528 + 516:bi * 528 + 520], in_=gt[:, slot, 3, 4:8]), reads=[t_gt[slot]], writes=[t_dstg[bi]])
                P.op("dve", lambda e: e.tensor_copy(out=dstg[:, bi * 528 + 520:bi * 528 + 524], in_=gt[:, slot, 2, 4:8]), reads=[t_gt[slot]], writes=[t_dstg[bi]])
                P.dma("sp", lambda e: e.dma_start(out=dscr.ap()[c], in_=dstg[:, bi * 528:(bi + 1) * 528]), reads=[t_dstg[bi]], writes=[t_dscr])

            front(0)
            for gi in range(len(steps)):
                if gi + 1 < len(steps):
                    front(gi + 1)
                back(gi)
            for ci, c in enumerate(range(15, -1, -1)):
                bi = ci % 2
                P.dma("sp", lambda e: e.dma_start(out=dstg[:, bi * 528:(bi + 1) * 528], in_=dscr.ap()[c]), reads=[t_dscr], writes=[t_dstg[bi]])
                Dl = [(dstg[:, bi * 528 + h * 129:bi * 528 + (h + 1) * 129], t_dstg[bi]) for h in range(4)]
                apply_update(1, Dl, dstg[:, bi * 528 + 516:bi * 528 + 520], dstg[:, bi * 528 + 520:bi * 528 + 524], t_dstg[bi], maskb[:, c:c + 1], t_maskb)
            P.alias(t_qT, t_dstg)
            P.alias(t_kT, t_dstg)

            for tkz in t_hmT + [t_fmT]:
                P.alias(tkz, [t_zpc])
            for tk in t_hacc:
                P.alias(tk, t_tmpA + t_tmpC)
            process_item(l, [4, 5, 6, 7], 1, True, None)
            process_item(l, [0, 1], 0, False, 0)
            process_item(l, [2, 3], 0, False, 1)

            if _DBG and l == 0:
                tkd2 = P.tok("dbgx1")
                out_toks.append(tkd2)
                P.dma("sp", lambda e: e.dma_start(out=DO["dbg_x1"], in_=X[:, :, :]), reads=tX, writes=[tkd2])
            mixer_toks = t_uT + [t_qT, t_kT, t_fmT] + t_hmT + t_ktm + t_og
            for tk in t_gs + [t for pair in t_actT for t in pair]:
                P.alias(tk, mixer_toks)
            for tk in t_tmpA + t_tmpC:
                P.alias(tk, t_hacc)
            for tk in t_GT:
                P.alias(tk, [t_Cout])
            mod_vectors(l, [3, 4, 5])
            P.dma("sp", lambda e, l=l: e.dma_start(out=lnw[:], in_=DI["ln2w"][l]), writes=[t_lnw])
            P.dma("sp", lambda e, l=l: e.dma_start(out=lnb[:], in_=DI["ln2b"][l]), writes=[t_lnb])
            for xi in range(8):
                cond = 0 if xi < 4 else 1
                for half in range(2):
                    pb, tpb = bank()
                    for q in range(4):
                        kc = half * 4 + q
                        P.op("pe", lambda e, q=q, kc=kc, pb=pb, xi=xi: e.transpose(pb[:, q * 128:(q + 1) * 128], X[:, xi, kc * 128:(kc + 1) * 128], ident_f[:]),
                             reads=[tX[xi], t_idf], writes=[tpb])
                    for q in range(4):
                        kc = half * 4 + q
                        P.op("act", lambda e, q=q, kc=kc, pb=pb, cond=cond: e.activation(out=u2f[:, kc, :], in_=pb[:, q * 128:(q + 1) * 128], func=AF.Identity,
                                                                                         scale=modT[:, 4, kc, cond:cond + 1], bias=modT[:, 3, kc, cond:cond + 1]),
                             reads=[tpb, t_modT], writes=[t_u2f])
                P.op("act", lambda e, xi=xi: e.copy(out=u2T[:, :, xi * 128:(xi + 1) * 128], in_=u2f[:]), reads=[t_u2f], writes=[t_u2T[xi]])
                pl, tpl = bank()
                for kc in range(8):
                    P.op("pe", lambda e, kc=kc, pl=pl: e.matmul(pl[:, 0:NE], lhsT=u2f[:, kc, :], rhs=rw[:, kc, :], start=(kc == 0), stop=(kc == 7)),
                         reads=[t_u2f, t_rw], writes=[tpl])
                P.op("dve", lambda e, pl=pl: e.tensor_tensor(out=lg[:, 0, :], in0=pl[:, 0:NE], in1=rbb[:], op=ALU.add), reads=[tpl, t_rbb], writes=[t_lg])
                P.op("dve", lambda e: e.max(out=sm2[:, 0:8], in_=lg[:, 0, :]), reads=[t_lg], writes=[t_sm2])
                P.op("dve", lambda e: e.tensor_scalar(out=lg[:, 1, :], in0=lg[:, 0, :], scalar1=sm2[:, 3:4], scalar2=None, op0=ALU.is_ge), reads=[t_lg, t_sm2], writes=[t_lg])
                P.op("dve", lambda e: e.tensor_scalar(out=sm2[:, 8:9], in0=sm2[:, 0:1], scalar1=-1.0, scalar2=None, op0=ALU.mult), reads=[t_sm2], writes=[t_sm2])
                P.op("act", lambda e: e.activation(out=lg[:, 2, :], in_=lg[:, 0, :], func=AF.Exp, bias=sm2[:, 8:9], scale=1.0), reads=[t_lg, t_sm2], writes=[t_lg])
                P.op("dve", lambda e: e.tensor_tensor(out=lg[:, 3, :], in0=lg[:, 2, :], in1=lg[:, 1, :], op=ALU.mult), reads=[t_lg], writes=[t_lg])
                P.op("dve", lambda e: e.reduce_sum(out=sm2[:, 9:10], in_=lg[:, 3, :], axis=mybir.AxisListType.X), reads=[t_lg], writes=[t_sm2])
                P.op("dve", lambda e: e.reciprocal(out=sm2[:, 10:11], in_=sm2[:, 9:10]), reads=[t_sm2], writes=[t_sm2])
                P.op("dve", lambda e, xi=xi: e.tensor_scalar(out=Gall[:, xi, :], in0=lg[:, 3, :], scalar1=sm2[:, 10:11], scalar2=None, op0=ALU.mult),
                     reads=[t_lg, t_sm2], writes=[t_G[xi]])
                pg2, tpg2 = bank()
                P.op("pe", lambda e, xi=xi, pg2=pg2: e.transpose(pg2[0:NE, 0:128], Gall[:, xi, :], ident_f[:]), reads=[t_G[xi], t_idf], writes=[tpg2])
                P.op("act", lambda e, xi=xi, pg2=pg2: e.copy(out=GT[:, xi * 128:(xi + 1) * 128], in_=pg2[0:NE, 0:128]), reads=[tpg2], writes=[t_GT[xi]])
                P.op("dve", lambda e, xi=xi: e.tensor_scalar(out=X[:, xi, :], in0=X[:, xi, :], scalar1=ALPHA, scalar2=None, op0=ALU.mult), reads=[tX[xi]], writes=[tX[xi]])
                for hf in range(2):
                    pbb_, tpbb_ = bank()
                    P.op("pe", lambda e, xi=xi, hf=hf, pbb_=pbb_: e.matmul(pbb_[:, 0:512], lhsT=GT[:, xi * 128:(xi + 1) * 128], rhs=bdn[:, hf * 512:(hf + 1) * 512], start=True, stop=True),
                         reads=[t_GT[xi], t_bdn], writes=[tpbb_])
                    P.op("dve", lambda e, hf=hf, pbb_=pbb_, cond=cond: e.tensor_tensor(out=xt[:, 1, hf * 512:(hf + 1) * 512], in0=pbb_[:, 0:512], in1=gb[:, cond, hf * 512:(hf + 1) * 512], op=ALU.mult),
                         reads=[tpbb_, t_gb], writes=[t_xt[1]])
                P.op("dve", lambda e, xi=xi: e.tensor_tensor(out=X[:, xi, :], in0=X[:, xi, :], in1=xt[:, 1, :], op=ALU.add), reads=[tX[xi], t_xt[1]], writes=[tX[xi]])
            pieces = []
            for ex in range(NEX):
                pieces += [("glu", ex, 0, 0), ("lin", ex, 0, 1024), ("glu", ex, 1, 512), ("lin", ex, 1, 1536), ("down", ex, 0, 0), ("down", ex, 1, 512)]

            def fetch(pc):
                kind, ex, fb, c0 = pc
                src = DI["w_down"][l, ex] if kind == "down" else DI["w_gu"][l, ex]
                return load_wpiece(src, c0, 512, eng="act")

            nxt = fetch(pieces[0]) if pieces else None
            for pi, pc in enumerate(pieces):
                kind, ex, fb, c0 = pc
                wbuf, twb = nxt
                if pi + 1 < len(pieces):
                    nxt = fetch(pieces[pi + 1])
                if kind == "glu":
                    for fcl in range(4):
                        fc = fb * 4 + fcl
                        for th in range(2):
                            pb, tpb = bank()
                            for kc in range(8):
                                P.op("pe", lambda e, kc=kc, fcl=fcl, th=th, pb=pb, wbuf=wbuf: e.matmul(pb[:, 0:512], lhsT=wbuf[:, kc, fcl * 128:(fcl + 1) * 128], rhs=u2T[:, kc, th * 512:(th + 1) * 512],
                                                                                                   start=(kc == 0), stop=(kc == 7)), reads=[twb] + t_u2T[th * 4:th * 4 + 4], writes=[tpb])
                            i2 = (fcl * 2 + th) % 2
                            P.op("dve", lambda e, fc=fc, ex=ex, pb=pb, i2=i2: e.tensor_scalar(out=tmpA[:, i2, :], in0=pb[:, 0:512], scalar1=bguT[:, fc, ex:ex + 1], scalar2=LIM, op0=ALU.add, op1=ALU.min),
                                 reads=[tpb, t_bguT], writes=[t_tmpA[i2]])
                            P.op("act", lambda e, i2=i2: e.activation(out=tmpB[:, i2, :], in_=tmpA[:, i2, :], func=AF.Sigmoid, scale=SALPHA), reads=[t_tmpA[i2]], writes=[t_tmpB[i2]])
                            P.op("dve", lambda e, fcl=fcl, th=th, i2=i2: e.tensor_tensor(out=gsb[:, fcl, th * 512:(th + 1) * 512], in0=tmpA[:, i2, :], in1=tmpB[:, i2, :], op=ALU.mult),
                                 reads=[t_tmpA[i2], t_tmpB[i2]], writes=[t_gs[fcl * 2 + th]])
                elif kind == "lin":
                    for fcl in range(4):
                        fc = fb * 4 + fcl
                        for th in range(2):
                            pb, tpb = bank()
                            for kc in range(8):
                                P.op("pe", lambda e, kc=kc, fcl=fcl, th=th, pb=pb, wbuf=wbuf: e.matmul(pb[:, 0:512], lhsT=wbuf[:, kc, fcl * 128:(fcl + 1) * 128], rhs=u2T[:, kc, th * 512:(th + 1) * 512],
                                                                                                   start=(kc == 0), stop=(kc == 7)), reads=[twb] + t_u2T[th * 4:th * 4 + 4], writes=[tpb])
                            i2 = (fcl * 2 + th) % 2
                            P.op("act", lambda e, fc=fc, ex=ex, pb=pb, i2=i2: e.activation(out=tmpC[:, i2, :], in_=pb[:, 0:512], func=AF.Identity, bias=bguT[:, 8 + fc, ex:ex + 1], scale=1.0),
                                 reads=[tpb, t_bguT], writes=[t_tmpC[i2]])
                            P.op("dve", lambda e, i2=i2: e.tensor_scalar(out=tmpC[:, i2, :], in0=tmpC[:, i2, :], scalar1=LIM, scalar2=-LIM, op0=ALU.min, op1=ALU.max),
                                 reads=[t_tmpC[i2]], writes=[t_tmpC[i2]])
                            P.op("dve", lambda e, fc=fc, fcl=fcl, th=th, i2=i2: e.scalar_tensor_tensor(out=actT[:, fc, th * 512:(th + 1) * 512], in0=tmpC[:, i2, :], scalar=1.0, in1=gsb[:, fcl, th * 512:(th + 1) * 512],
                                                                                                  op0=ALU.add, op1=ALU.mult),
                                 reads=[t_gs[fcl * 2 + th], t_tmpC[i2]], writes=[t_actT[fc][th]])
                else:
                    dh = fb
                    for xi in range(8):
                        cond = 0 if xi < 4 else 1
                        pb, tpb = bank()
                        for fc in range(8):
                            P.op("pe", lambda e, fc=fc, xi=xi, pb=pb, wbuf=wbuf: e.matmul(pb[:, 0:512], lhsT=actT[:, fc, xi * 128:(xi + 1) * 128], rhs=wbuf[:, fc, :],
                                                                                         start=(fc == 0), stop=(fc == 7)), reads=[twb, t_actT[fc][xi // 4]], writes=[tpb])
                        i2 = xi % 2
                        P.op("dve", lambda e, xi=xi, ex=ex, pb=pb, i2=i2, cond=cond, dh=dh: e.scalar_tensor_tensor(out=tmpA[:, i2, :], in0=pb[:, 0:512], scalar=Gall[:, xi, ex:ex + 1],
                                                                                                         in1=gb[:, cond, dh * 512:(dh + 1) * 512], op0=ALU.mult, op1=ALU.mult),
                             reads=[tpb, t_G[xi], t_gb], writes=[t_tmpA[i2]])
                        P.op("dve", lambda e, xi=xi, i2=i2, dh=dh: e.tensor_tensor(out=X[:, xi, dh * 512:(dh + 1) * 512], in0=X[:, xi, dh * 512:(dh + 1) * 512], in1=tmpA[:, i2, :], op=ALU.add),
                             reads=[tX[xi], t_tmpA[i2]], writes=[tX[xi]])
            for xi in range(8):
                layer_norm_inplace(xi, t_lnw, t_lnb)

            if l < NL - 1:
                for i in range(4):
                    P.dma("pool", lambda e, i=i: e.dma_start(out=gins[i].ap(), in_=X[:, 4 + i, :]), reads=[tX[4 + i]], writes=[t_gin[i]])
                    P.dma("pool", lambda e, i=i: e.collective_compute("AllGather", ALU.bypass, replica_groups=[[0, 1, 2, 3], [4, 5, 6, 7]],
                                                                      ins=[gins[i].ap().opt()], outs=[gouts[i].ap().opt()]), reads=[t_gin[i]], writes=[t_gout[i]], inc=1)
            else:
                for i in range(4):
                    P.dma("sp", lambda e, i=i: e.dma_start(out=DO["yp"][i * 128:(i + 1) * 128, :], in_=X[:, i, :]), reads=[tX[i]], writes=[], key=okey["yp"])
                    P.dma("sp", lambda e, i=i: e.dma_start(out=DO["ys"][i * 128:(i + 1) * 128, :], in_=X[:, 4 + i, :]), reads=[tX[4 + i]], writes=[], key=okey["ys"])

        P.emit(st, final_waits=out_toks)
    return nc


def _grid_pos_embed(rows, d):
    quarter = d // 4
    omega = (1.0 / (10000.0 ** (np.arange(quarter, dtype=np.float32) / np.float32(quarter)))).astype(np.float32)
    r = np.repeat(np.arange(rows, dtype=np.float32), 64)[:, None] * omega
    cc = np.tile(np.arange(64, dtype=np.float32), rows)[:, None] * omega
    return np.concatenate([np.sin(r), np.cos(r), np.sin(cc), np.cos(cc)], axis=-1).astype(np.float32)


_CACHE = {}
_DBG = False
_CFG = {"NL": 4, "NEX": NE}


def kernel(x_prompt, x_sample, state_C, state_n, state_m, c, c_ctx, w_ada, b_ada, w_in, gate_bias, mlstm_norm_w,
           w_out, ln1_w, ln1_b, router_w, router_b, w_gate_up, b_gate_up, w_down, b_down, ln2_w, ln2_b):
    f32 = np.float32
    bf = ml_dtypes.bfloat16
    A = lambda a: np.ascontiguousarray(np.asarray(a), dtype=f32)
    x_prompt, x_sample = A(x_prompt), A(x_sample)
    NL, NEX = _CFG["NL"], _CFG["NEX"]
    key = (NL, NEX)
    if key not in _CACHE:
        _CACHE[key] = build_program(NL, NEX)
    nc = _CACHE[key]
    DEPTH = NL
    pos = _grid_pos_embed(2048 // 64, D)
    n = np.arange(2048, dtype=np.float64)
    ang = 2 * np.pi * ((n[:, None] * n[None, :]) % 2048) / 2048
    CN, SN = np.cos(ang), np.sin(ang)
    n2 = np.arange(256, dtype=np.float64)
    ang2 = 2 * np.pi * ((n2[:, None] * n2[None, :]) % 256) / 256
    n3 = np.arange(128, dtype=np.float64)
    ang3 = 2 * np.pi * ((n3[:, None] * n3[None, :]) % 128) / 128
    CC, SC = np.cos(ang3), np.sin(ang3)
    sp_, ss_ = 1.0 / np.sqrt(256 * 128.0), 1.0 / np.sqrt(2048 * 128.0)
    ar = np.arange(128)
    triU = (ar[:, None] <= ar[None, :]).astype(f32)
    triL = (ar[:, None] >= ar[None, :]).astype(f32)
    selr = np.zeros((2, 2, 128), f32)
    selr[0, 0] = 1
    selr[1, 1] = 1
    sel8 = np.zeros((8, 2), f32)
    sel8[0:4, 0] = 1
    sel8[4:8, 1] = 1
    shared = {
        "pos_full": pos,
        "w_ada": A(w_ada)[:NL], "b_ada2": np.ascontiguousarray(np.broadcast_to(A(b_ada)[:NL, None, :], (DEPTH, 2, 6 * D))),
        "w_in": A(w_in)[:NL], "gbias": np.ascontiguousarray(np.broadcast_to(A(gate_bias)[:NL, None, :], (DEPTH, 128, 16))),
        "nwT": np.ascontiguousarray(A(mlstm_norm_w)[:NL].reshape(DEPTH, 4, 128).transpose(0, 2, 1)),
        "w_out": A(w_out)[:NL],
        "ln1w": np.ascontiguousarray(np.broadcast_to(A(ln1_w)[:NL, None, :], (DEPTH, 128, D))),
        "ln1b": np.ascontiguousarray(np.broadcast_to(A(ln1_b)[:NL, None, :], (DEPTH, 128, D))),
        "router_w": A(router_w)[:NL], "rbb": np.ascontiguousarray(np.broadcast_to(A(router_b)[:NL, None, :], (DEPTH, 128, NE))),
        "w_gu": A(w_gate_up)[:NL, :max(NEX, 1)], "bguT": np.ascontiguousarray(A(b_gate_up)[:NL].reshape(DEPTH, NE, 16, 128).transpose(0, 3, 2, 1)),
        "w_down": A(w_down)[:NL, :max(NEX, 1)], "b_down": A(b_down)[:NL],
        "ln2w": np.ascontiguousarray(np.broadcast_to(A(ln2_w)[:NL, None, :], (DEPTH, 128, D))),
        "ln2b": np.ascontiguousarray(np.broadcast_to(A(ln2_b)[:NL, None, :], (DEPTH, 128, D))),
        "c256": np.cos(ang2).astype(bf), "s256": np.sin(ang2).astype(bf),
        "ccp": (CC * sp_).astype(bf), "nscp": (-SC * sp_).astype(bf), "ccs": (CC * ss_).astype(bf), "nscs": (-SC * ss_).astype(bf),
        "ident_b": np.eye(128, dtype=f32).astype(bf), "ident_f": np.eye(128, dtype=f32), "triU": triU, "triL": triL,
        "selr": selr, "sel8": sel8,
        "NMU4": np.ascontiguousarray(np.broadcast_to(((triU - 1.0) * 30000.0)[:, None, :], (128, 4, 128))).astype(f32),
        "NML4": np.ascontiguousarray(np.broadcast_to(((triL - 1.0) * 30000.0)[:, None, :], (128, 4, 128))).astype(f32),
    }
    state_C, state_n, state_m, c, c_ctx = A(state_C), A(state_n), A(state_m), A(c), A(c_ctx)
    in_maps = []
    for core in range(8):
        b, j = divmod(core, 4)
        cv = np.stack([c_ctx, c[b]], 0)
        cvT = np.ascontiguousarray(cv.reshape(2, 8, 128).transpose(2, 1, 0).reshape(128, 16))
        mf = np.zeros((128, 16), f32)
        mb = np.zeros((128, 16), f32)
        mf[:, :4 * j] = 1
        mb[:, 4 * j + 4:] = 1
        m = dict(shared)
        m.update({
            "xp": np.ascontiguousarray(x_prompt[2 * core:2 * core + 2].reshape(512, D)),
            "xs_own": np.ascontiguousarray(x_sample[b, 512 * j:512 * j + 512]),
            "pos_own": np.ascontiguousarray(pos[512 * j:512 * j + 512]),
            "xs_full": np.ascontiguousarray(x_sample[b]),
            "sC": np.ascontiguousarray(state_C[b][:NL].reshape(DEPTH, 8, 128, 128)),
            "sn": np.ascontiguousarray(state_n[b][:NL].reshape(DEPTH, 8, 128).transpose(0, 2, 1)),
            "smb": np.ascontiguousarray(np.broadcast_to(state_m[b][:NL].reshape(DEPTH, 1, 8), (DEPTH, 128, 8))),
            "cvT": cvT, "maskf": mf, "maskb": mb,
            "cn_own": np.ascontiguousarray(CN[:, 512 * j:512 * j + 512]).astype(bf),
            "sn_own": np.ascontiguousarray(SN[:, 512 * j:512 * j + 512]).astype(bf),
        })
        in_maps.append(m)
    res = run_bass_kernel_spmd(nc, in_maps, core_ids=list(range(8)))
    R = res.results
    y_prompt = np.concatenate([R[k]["yp"].reshape(2, 256, D) for k in range(8)], 0).astype(f32)
    y_sample = np.stack([np.concatenate([R[4 * b + j]["ys"] for j in range(4)], 0) for b in range(2)], 0).astype(f32)
    nC = np.concatenate([R[k]["nC"] for k in range(8)], 0).reshape(16, DEPTH, 2, 4, 128, 128).astype(f32)
    nn = np.concatenate([R[k]["nn"] for k in range(8)], 0).reshape(16, DEPTH, 2, 4, 128).astype(f32)
    nm = np.concatenate([R[k]["nm"] for k in range(8)], 0).reshape(16, DEPTH, 2, 4).astype(f32)
    if _DBG:
      _CACHE["dbg2"] = {k2: [np.asarray(R[k][k2]) for k in range(8)] for k2 in ["dbg_h1", "dbg_h2", "dbg_gt", "dbg_stx", "dbg_scr", "dbg_qk", "dbg_nd", "dbg_sm2"]}
      _CACHE["dbg"] = {"mix": [np.asarray(R[k]["dbg_mix"]) for k in range(8)], "x1": [np.asarray(R[k]["dbg_x1"]) for k in range(8)]}
    return (y_prompt, y_sample, nC, nn, nm)
```

```python
from contextlib import ExitStack
import types
import numpy as np
import ml_dtypes
import concourse.bass as bass
import concourse.mybir as mybir
from concourse.bass_utils import run_bass_kernel_spmd

F32 = mybir.dt.float32
BF16 = mybir.dt.bfloat16
AF = mybir.ActivationFunctionType
ALU = mybir.AluOpType

D = 1024
DEPTH = 4
NE = 32
DH = 128
ALPHA = (2 * DEPTH) ** 0.25
LN_EPS = 1e-5
LIM = 7.0
SALPHA = 1.702
IN_COLS = 2576
G_OFF = 2048
F_OFF = 2064

COMPUTE = ("pe", "dve", "act", "pool")
ENGS = ("pe", "dve", "act", "pool", "sp")
EPOCH = 30000


class Tok:
    __slots__ = ("name", "last_w", "readers", "dma_total", "sem", "pending")

    def __init__(self, name):
        self.name = name
        self.last_w = None
        self.readers = []
        self.dma_total = 0
        self.sem = None
        self.pending = []


def _snapshot(fn):
    if getattr(fn, "__closure__", None) is None:
        return fn
    cells = []
    for c in fn.__closure__:
        try:
            cells.append(types.CellType(c.cell_contents))
        except ValueError:
            cells.append(c)
    return types.FunctionType(fn.__code__, fn.__globals__, fn.__name__, fn.__defaults__, tuple(cells))


class Op:
    __slots__ = ("eng", "fn", "waits", "signal", "sig_idx", "dma_key", "dma_val", "inc")

    def __init__(self, eng, fn):
        self.eng = eng
        self.fn = _snapshot(fn)
        self.waits = []
        self.signal = False
        self.sig_idx = None
        self.dma_key = None
        self.dma_val = None
        self.inc = 16


class Prog:
    def __init__(self, nc):
        self.nc = nc
        self.ops = {e: [] for e in ENGS}

    def tok(self, name="t"):
        return Tok(name)

    def toks(self, n, name="t"):
        return [Tok(name) for _ in range(n)]

    def alias(self, new, olds):
        for o in olds:
            if o.last_w is not None:
                new.pending.append(o.last_w)
            new.pending.extend(o.readers)

    def _add(self, eng, fn, reads, writes, dma_key=None, inc=16):
        o = Op(eng, fn)
        deps = []
        for t in reads:
            if t.last_w is not None:
                deps.append(t.last_w)
        for t in writes:
            if t.last_w is not None:
                deps.append(t.last_w)
            deps.extend(t.readers)
            if t.pending:
                deps.extend(t.pending)
                t.pending = []
        seen = set()
        for d in deps:
            if d is o or id(d) in seen:
                continue
            seen.add(id(d))
            if d.dma_key is not None:
                o.waits.append(("d", d.dma_key, d.dma_key.dma_total))
            else:
                if d.eng == "pe" and eng == "pe":
                    continue
                d.signal = True
                o.waits.append(("c", d.eng, d))
        for t in reads:
            t.readers.append(o)
        for t in writes:
            t.last_w = o
            t.readers = []
        if dma_key is not None:
            o.dma_key = dma_key
            o.inc = inc
            dma_key.dma_total += inc
            o.dma_val = dma_key.dma_total
        self.ops[eng].append(o)
        return o

    def op(self, eng, fn, reads=(), writes=()):
        return self._add(eng, fn, list(reads), list(writes))

    def dma(self, eng, fn, reads=(), writes=(), key=None, inc=16):
        if key is None:
            key = list(writes)[0]
        return self._add(eng, fn, list(reads), list(writes), dma_key=key, inc=inc)

    def emit(self, stack, final_waits=()):
        nc = self.nc
        nsig = {}
        for e in ENGS:
            k = 0
            for o in self.ops[e]:
                if o.dma_key is None and o.signal:
                    k += 1
                    o.sig_idx = k
            nsig[e] = k
        esems = {}
        for e in ENGS:
            n_ep = max(1, (nsig[e] + EPOCH - 1) // EPOCH)
            esems[e] = [stack.enter_context(nc.semaphore(f"s_{e}{i}")) for i in range(n_ep)]
        nk = 0
        for e in ENGS:
            for o in self.ops[e]:
                if o.dma_key is not None and o.dma_key.sem is None:
                    o.dma_key.sem = stack.enter_context(nc.semaphore(f"d_{nk}"))
                    nk += 1
        self.n_sems = sum(len(v) for v in esems.values()) + nk
        block = stack.enter_context(nc.Block())
        ops = self.ops

        def run(e, eng):
            waited_c = {}
            waited_d = {}
            for o in ops[e]:
                for w in o.waits:
                    if w[0] == "c":
                        d = w[2]
                        ep, v = divmod(d.sig_idx - 1, EPOCH)
                        v += 1
                        kk = (d.eng, ep)
                        if waited_c.get(kk, 0) >= v:
                            continue
                        if any(k2[0] == d.eng and k2[1] > ep for k2 in waited_c):
                            continue
                        waited_c[kk] = v
                        eng.wait_ge(esems[d.eng][ep], v)
                    else:
                        t, v = w[1], w[2]
                        if waited_d.get(id(t), 0) >= v:
                            continue
                        waited_d[id(t)] = v
                        eng.wait_ge(t.sem, v)
                ins = o.fn(eng)
                if o.dma_key is not None:
                    ins.then_inc(o.dma_key.sem, o.inc)
                elif o.signal:
                    ins.then_inc(esems[e][(o.sig_idx - 1) // EPOCH], 1)
            if e == "sp":
                for t in final_waits:
                    eng.wait_ge(t.sem, t.dma_total)

        @block.sync
        def _(eng):
            run("sp", eng)

        @block.tensor
        def _(eng):
            run("pe", eng)

        @block.vector
        def _(eng):
            run("dve", eng)

        @block.scalar
        def _(eng):
            run("act", eng)

        @block.gpsimd
        def _(eng):
            run("pool", eng)


def input_specs(DEPTH, NEX):
  return [
    ("xp", [512, D], F32), ("xs_own", [512, D], F32), ("pos_own", [512, D], F32),
    ("xs_full", [2048, D], F32), ("pos_full", [2048, D], F32),
    ("sC", [DEPTH, 8, 128, 128], F32), ("sn", [DEPTH, 128, 8], F32), ("smb", [DEPTH, 128, 8], F32),
    ("cvT", [128, 16], F32), ("maskf", [128, 16], F32), ("maskb", [128, 16], F32),
    ("w_ada", [DEPTH, D, 6 * D], F32), ("b_ada2", [DEPTH, 2, 6 * D], F32),
    ("w_in", [DEPTH, D, IN_COLS], F32), ("gbias", [DEPTH, 128, 16], F32), ("nwT", [DEPTH, 128, 4], F32),
    ("w_out", [DEPTH, D, D], F32), ("ln1w", [DEPTH, 128, D], F32), ("ln1b", [DEPTH, 128, D], F32),
    ("router_w", [DEPTH, D, NE], F32), ("rbb", [DEPTH, 128, NE], F32),
    ("w_gu", [DEPTH, NEX, D, 2 * D], F32), ("bguT", [DEPTH, 128, 16, NE], F32),
    ("w_down", [DEPTH, NEX, D, D], F32), ("b_down", [DEPTH, NE, D], F32),
    ("ln2w", [DEPTH, 128, D], F32), ("ln2b", [DEPTH, 128, D], F32),
    ("cn_own", [2048, 512], BF16), ("sn_own", [2048, 512], BF16),
    ("c256", [256, 256], BF16), ("s256", [256, 256], BF16),
    ("ccp", [128, 128], BF16), ("nscp", [128, 128], BF16), ("ccs", [128, 128], BF16), ("nscs", [128, 128], BF16),
    ("ident_b", [128, 128], BF16), ("ident_f", [128, 128], F32), ("triU", [128, 128], F32), ("triL", [128, 128], F32),
    ("selr", [2, 2, 128], F32), ("sel8", [8, 2], F32), ("NMU4", [128, 4, 128], F32), ("NML4", [128, 4, 128], F32),
  ]


def output_specs(DEPTH):
  return [
    ("yp", [512, D], F32), ("ys", [512, D], F32),
    ("nC", [2, DEPTH, 8, 128, 128], F32), ("nn", [2, DEPTH, 8, 128], F32), ("nm", [2, DEPTH, 8], F32),
  ]


def build_program(NL=DEPTH, NEX=NE, do_cc=True, stop_after=None):
    nc = bass.Bass("TRN2", target_bir_lowering=False)
    P = Prog(nc)
    DI = {n: nc.dram_tensor(n, list(s), d, kind="ExternalInput").ap() for n, s, d in input_specs(NL, max(NEX, 1))}
    DO = {n: nc.dram_tensor(n, list(s), d, kind="ExternalOutput").ap() for n, s, d in output_specs(NL)}
    if _DBG:
        DO["dbg_mix"] = nc.dram_tensor("dbg_mix", [128, 8, 512], BF16, kind="ExternalOutput").ap()
        DO["dbg_h1"] = nc.dram_tensor("dbg_h1", [128, 2, 512], F32, kind="ExternalOutput").ap()
        DO["dbg_h2"] = nc.dram_tensor("dbg_h2", [128, 2, 512], F32, kind="ExternalOutput").ap()
        DO["dbg_gt"] = nc.dram_tensor("dbg_gt", [128, 2, 48], F32, kind="ExternalOutput").ap()
        DO["dbg_stx"] = nc.dram_tensor("dbg_stx", [128, 2, 4, 128], BF16, kind="ExternalOutput").ap()
        DO["dbg_scr"] = nc.dram_tensor("dbg_scr", [128, 512], F32, kind="ExternalOutput").ap()
        DO["dbg_qk"] = nc.dram_tensor("dbg_qk", [128, 2, 4, 256], BF16, kind="ExternalOutput").ap()
        DO["dbg_nd"] = nc.dram_tensor("dbg_nd", [128, 2, 129], F32, kind="ExternalOutput").ap()
        DO["dbg_sm2"] = nc.dram_tensor("dbg_sm2", [128, 32], F32, kind="ExternalOutput").ap()
        DO["dbg_x1"] = nc.dram_tensor("dbg_x1", [128, 8, D], F32, kind="ExternalOutput").ap()
    gins = [nc.dram_tensor(f"gin{i}", [128, D], F32) for i in range(4)]
    gouts = [nc.dram_tensor(f"gout{i}", [512, D], F32) for i in range(4)]
    t_gin, t_gout = P.toks(4, "gin"), P.toks(4, "gout")
    out_toks = []

    with ExitStack() as st:
        def sb(name, shape, dt):
            return st.enter_context(nc.sbuf_tensor("sb_" + name, list(shape), dt))

        PS = [st.enter_context(nc.psum_tensor(f"ps{i}", [128, 512], F32)) for i in range(8)]
        tPS = P.toks(8, "ps")
        bank_ctr = [0]
        PLONG, t_PLONG = PS[7], tPS[7]

        def bank():
            i = bank_ctr[0] % 7
            bank_ctr[0] += 1
            return PS[i], tPS[i]

        consts = {}
        for n, shp, dt in [("ident_b", [128, 128], BF16), ("ident_f", [128, 128], F32), ("triU", [128, 128], F32),
                           ("triL", [128, 128], F32), ("ccp", [128, 128], BF16), ("nscp", [128, 128], BF16),
                           ("ccs", [128, 128], BF16), ("nscs", [128, 128], BF16), ("cvT", [128, 16], F32),
                           ("maskf", [128, 16], F32), ("maskb", [128, 16], F32), ("selr", [2, 2, 128], F32), ("sel8", [8, 2], F32), ("NMU4", [128, 4, 128], F32), ("NML4", [128, 4, 128], F32)]:
            t = sb("c_" + n, shp, dt)
            tk = P.tok(n)
            P.dma("sp", lambda e, t=t, n=n: e.dma_start(out=t[:], in_=DI[n]), writes=[tk])
            consts[n] = (t, tk)
        ident_b, t_idb = consts["ident_b"]
        ident_f, t_idf = consts["ident_f"]
        triU, t_triU = consts["triU"]
        triL, t_triL = consts["triL"]
        cvT, t_cvT = consts["cvT"]
        maskf, t_maskf = consts["maskf"]
        maskb, t_maskb = consts["maskb"]
        selr, t_selr = consts["selr"]
        sel8, t_sel8 = consts["sel8"]
        NMU4, t_NMU4 = consts["NMU4"]
        NML4, t_NML4 = consts["NML4"]
        c256 = sb("c256", [128, 2, 256], BF16)
        s256 = sb("s256", [128, 2, 256], BF16)
        t_c256, t_s256 = P.tok(), P.tok()
        P.dma("sp", lambda e: e.dma_start(out=c256[:], in_=DI["c256"].rearrange("(m p) n -> p m n", p=128)), writes=[t_c256])
        P.dma("sp", lambda e: e.dma_start(out=s256[:], in_=DI["s256"].rearrange("(m p) n -> p m n", p=128)), writes=[t_s256])
        ones_f = sb("ones_f", [128, 128], F32)
        ones_b = sb("ones_b", [128, 128], BF16)
        eps_t = sb("eps_t", [128, 1], F32)
        t_ones = P.tok()
        P.op("pool", lambda e: e.memset(ones_f[:], 1.0), writes=[t_ones])
        P.op("pool", lambda e: e.memset(ones_b[:], 1.0), writes=[t_ones])
        P.op("pool", lambda e: e.memset(eps_t[:], LN_EPS), writes=[t_ones])
        sT = sb("sT", [128, 16], F32)
        t_sT = P.tok()
        P.op("act", lambda e: e.activation(out=sT[:], in_=cvT[:], func=AF.Silu), reads=[t_cvT], writes=[t_sT])

        X = sb("X", [128, 8, D], F32)
        tX = P.toks(8, "X")
        for i in range(4):
            P.dma("sp", lambda e, i=i: e.dma_start(out=X[:, i, :], in_=DI["xp"][i * 128:(i + 1) * 128, :]), writes=[tX[i]])
        ptmp = sb("ptmp", [128, 2, D], F32)
        t_ptmp = P.toks(2)
        for i in range(4):
            P.dma("sp", lambda e, i=i: e.dma_start(out=X[:, 4 + i, :], in_=DI["xs_own"][i * 128:(i + 1) * 128, :]), writes=[tX[4 + i]])
            P.dma("sp", lambda e, i=i: e.dma_start(out=ptmp[:, i % 2, :], in_=DI["pos_own"][i * 128:(i + 1) * 128, :]), writes=[t_ptmp[i % 2]])
            P.op("pool", lambda e, i=i: e.tensor_tensor(out=X[:, 4 + i, :], in0=X[:, 4 + i, :], in1=ptmp[:, i % 2, :], op=ALU.add),
                 reads=[tX[4 + i], t_ptmp[i % 2]], writes=[tX[4 + i]])

        STG_N = 2
        stg = [sb(f"stg{i}", [128, 8, 256], F32) for i in range(STG_N)]
        t_stg = P.toks(STG_N, "stg")
        stg_ctr = [0]

        def stage_load(src_ap):
            i = stg_ctr[0] % STG_N
            stg_ctr[0] += 1
            P.dma("sp", lambda e: e.dma_start(out=stg[i][:], in_=src_ap.rearrange("(kc p) c -> p kc c", p=128)), writes=[t_stg[i]])
            return stg[i], t_stg[i]

        cast_ctr = [0]

        def cast_eng():
            cast_ctr[0] += 1
            return "dve" if cast_ctr[0] % 2 else "act"

        def do_copy(eng, out, in_, reads, writes):
            if eng == "act":
                P.op("act", lambda e: e.copy(out=out, in_=in_), reads=reads, writes=writes)
            else:
                P.op(eng, lambda e: e.tensor_copy(out=out, in_=in_), reads=reads, writes=writes)

        nwT = sb("nwT", [128, 4], F32)
        t_nwT = P.tok()
        gbias = sb("gbias", [128, 16], F32)
        t_gbias = P.tok()
        modT = sb("modT", [128, 6, 8, 2], F32)
        t_modT = P.tok("modT")
        gb = sb("gb", [128, 2, D], F32)
        t_gb = P.tok("gb")
        lnw = sb("lnw", [128, D], F32)
        lnb = sb("lnb", [128, D], F32)
        t_lnw, t_lnb = P.tok(), P.tok()
        rw = sb("rw", [128, 8, NE], F32)
        t_rw = P.tok()
        rbb = sb("rbb", [128, NE], F32)
        t_rbb = P.tok()
        bguT = sb("bguT", [128, 16, NE], F32)
        t_bguT = P.tok()
        bdn = sb("bdn", [NE, D], F32)
        t_bdn = P.tok()
        zscr = nc.dram_tensor("zscr", [2048, 512], BF16)
        t_zscr = P.tok("zscr")
        dscr = nc.dram_tensor("dscr", [16, 128, 528], F32)
        t_dscr = P.tok("dscr")
        Sst = sb("Sst", [128, 8, 129], F32)
        t_S = P.toks(8, "S")
        Sbf = sb("Sbf", [128, 8, 129], BF16)
        t_Sbf = P.toks(8, "Sbf")
        smb = sb("smb", [128, 8], F32)
        t_smb = P.tok()
        arena = sb("arena", [128, 16384], BF16)
        uT = arena[:, 0:4096].rearrange("p (a b) -> p a b", a=8)
        t_uT = P.toks(4, "uT")
        qT = arena[:, 4096:6144].rearrange("p (a b) -> p a b", a=4)
        kT = arena[:, 6144:8192].rearrange("p (a b) -> p a b", a=4)
        t_qT, t_kT = P.tok("qT"), P.tok("kT")
        ktm = arena[:, 12288:14336].rearrange("p (a b) -> p a b", a=4)
        t_ktm = P.toks(4, "ktm")
        vext = sb("vext", [128, 4, 4, 129], BF16)
        t_vext = P.toks(4, "vext")
        vsc = sb("vsc", [128, 2, 4, 129], BF16)
        t_vsc = P.tok("vsc")
        og = arena[:, 14336:16384].rearrange("p (a b) -> p a b", a=4)
        t_og = P.toks(4, "og")
        zown = sb("zown", [128, 2, 512], BF16)
        t_zown = P.toks(2, "zown")
        zst, t_zst = zown, t_zown
        gt = sb("gt", [128, 4, 6, 8], F32)
        mst = sb("mst", [128, 8], F32)
        t_mst = P.tok("mst")
        scr = sb("scr", [128, 512], F32)
        t_scr = P.tok("scr")
        nd = sb("nd", [128, 2, 129], F32)
        t_nd = P.toks(2, "nd")
        t_gt = P.toks(4, "gt")
        STx = sb("STx", [128, 2, 4, 128], BF16)
        t_STx = P.toks(2, "STx")
        arena2 = sb("arena2", [128, 2048], F32)
        hacc = arena2[:, :].rearrange("p (a b) -> p a b", a=4)
        t_hacc = P.toks(4, "hacc")
        hmT = arena[:, 8192:12288].rearrange("p (a b) -> p a b", a=8)
        t_hmT = P.toks(4, "hmT")
        t_fmT = P.tok("fmT")
        small = sb("small", [128, 64], F32)
        t_small = P.tok("small")
        xt = ptmp
        t_xt = t_ptmp
        xtb = sb("xtb", [128, 1, D], BF16)
        t_xtb = P.toks(1, "xtb")
        dtab = sb("dtab", [128, 2, 2, 512], BF16)
        t_dtab = P.toks(2, "dtab")
        p12 = sb("p12", [128, 2, 2, 512], BF16)
        t_p12 = P.toks(2, "p12")

        u2T = sb("u2T", [128, 8, 1024], BF16)
        t_u2T = P.toks(8, "u2T")
        u2f = sb("u2f", [128, 8, 128], F32)
        t_u2f = P.tok("u2f")
        Gall = sb("Gall", [128, 8, NE], F32)
        t_G = P.toks(8, "G")
        arena3 = sb("arena3", [128, 1032], F32)
        GT = arena3[0:NE, 0:1024]
        t_GT = P.toks(8, "GT")
        WB_N = 2
        wb = [sb(f"wb{i}", [128, 8, 512], BF16) for i in range(WB_N)]
        t_wb = P.toks(WB_N, "wb")
        wb_ctr = [0]
        gsb = arena[:, 8192:12288].rearrange("p (a b) -> p a b", a=4)
        t_gs = P.toks(8, "gs")
        actT = arena[:, 0:8192].rearrange("p (a b) -> p a b", a=8)
        t_actT = [P.toks(2, "actT") for _ in range(8)]
        tmpA = arena2[:, 0:1024].rearrange("p (a b) -> p a b", a=2)
        t_tmpA = P.toks(2, "tmpA")
        tmpB = sb("tmpB", [128, 2, 512], BF16)
        t_tmpB = P.toks(2, "tmpB")
        tmpC = arena2[:, 1024:2048].rearrange("p (a b) -> p a b", a=2)
        t_tmpC = P.toks(2, "tmpC")

        SCALE_K = DH ** -0.5

        def load_cast(dst, t_dst, src2d, ncols, scale_ap=None, scale_rows=0):
            c0 = 0
            while c0 < ncols:
                w = min(256, ncols - c0)
                s_t, s_k = stage_load_w(src2d, c0, w)
                eng = cast_eng()
                if scale_ap is None:
                    do_copy(eng, dst[:, :, c0:c0 + w], s_t[:, :, 0:w], [s_k], [t_dst])
                else:
                    for kc in range(8):
                        if kc < scale_rows:
                            P.op("pool", lambda e, kc=kc, c0=c0, w=w, s_t=s_t: e.tensor_scalar(
                                out=dst[:, kc, c0:c0 + w], in0=s_t[:, kc, 0:w], scalar1=scale_ap[:, kc:kc + 1], scalar2=None,
                                op0=ALU.mult), reads=[s_k, t_nwT], writes=[t_dst])
                        else:
                            do_copy("pool", dst[:, kc, c0:c0 + w], s_t[:, kc, 0:w], [s_k], [t_dst])
                c0 += w

        def stage_load_w(src2d, c0, w):
            i = stg_ctr[0] % STG_N
            stg_ctr[0] += 1
            P.dma("sp", lambda e: e.dma_start(out=stg[i][:, :, 0:w], in_=src2d[:, c0:c0 + w].rearrange("(kc p) c -> p kc c", p=128)),
                  writes=[t_stg[i]])
            return stg[i], t_stg[i]

        BIG = 30000.0

        def gate_math(pg, t_pg, slot):
            G_ = gt[:, slot]
            tk = t_gt[slot]
            P.op("dve", lambda e: e.tensor_tensor(out=small[:, 0:16], in0=pg, in1=gbias[:], op=ALU.add),
                 reads=[t_pg, t_gbias], writes=[t_small])
            P.op("act", lambda e: e.activation(out=small[:, 16:20], in_=small[:, 4:8], func=AF.Exp, scale=-1.0), reads=[t_small], writes=[t_small])
            P.op("act", lambda e: e.activation(out=small[:, 20:24], in_=small[:, 12:16], func=AF.Exp, scale=-1.0), reads=[t_small], writes=[t_small])
            P.op("act", lambda e: e.activation(out=small[:, 24:32], in_=small[:, 16:24], func=AF.Ln, bias=1.0), reads=[t_small], writes=[t_small])
            P.op("dve", lambda e: e.tensor_scalar(out=small[:, 48:56], in0=small[:, 24:32], scalar1=-1.0, scalar2=None, op0=ALU.mult),
                 reads=[t_small], writes=[t_small])
            P.op("dve", lambda e: e.tensor_copy(out=small[:, 56:60], in_=small[:, 0:4]), reads=[t_small], writes=[t_small])
            P.op("dve", lambda e: e.tensor_copy(out=small[:, 60:64], in_=small[:, 8:12]), reads=[t_small], writes=[t_small])
            pb, tpb = bank()
            P.op("pe", lambda e: e.matmul(pb[:, 0:4], lhsT=triU[:], rhs=small[:, 48:52], start=True, stop=True), reads=[t_small, t_triU], writes=[tpb])
            P.op("pe", lambda e: e.matmul(pb[:, 4:8], lhsT=triL[:], rhs=small[:, 52:56], start=True, stop=True), reads=[t_small, t_triL], writes=[tpb])
            P.op("pe", lambda e: e.matmul(pb[:, 8:16], lhsT=ones_f[:], rhs=small[:, 48:56], start=True, stop=True), reads=[t_small, t_ones], writes=[tpb])
            P.op("dve", lambda e: e.tensor_tensor(out=G_[:, 0, :], in0=small[:, 56:64], in1=pb[:, 0:8], op=ALU.subtract), reads=[t_small, tpb], writes=[tk])
            P.op("act", lambda e: e.copy(out=G_[:, 1, :], in_=pb[:, 0:8]), reads=[tpb], writes=[tk])
            P.op("act", lambda e: e.copy(out=G_[:, 2, :], in_=pb[:, 8:16]), reads=[tpb], writes=[tk])
            for dr in range(2):
                for h in range(4):
                    P.op("dve", lambda e, dr=dr, h=h: e.tensor_scalar(out=hnb[:, h * 128:(h + 1) * 128], in0=ident_f[:], scalar1=G_[:, 0, dr * 4 + h:dr * 4 + h + 1], scalar2=None, op0=ALU.mult),
                         reads=[tk, t_idf], writes=[t_hnb])
                pq, tpq = bank()
                P.op("pe", lambda e, pq=pq: e.matmul(pq[:, 0:512], lhsT=ones_f[:], rhs=hnb[:, 0:512], start=True, stop=True), reads=[t_hnb, t_ones], writes=[tpq])
                P.op("dve", lambda e, dr=dr, pq=pq: e.tensor_reduce(out=G_[:, 3, dr * 4:dr * 4 + 4], in_=pq[:, 0:512].rearrange("p (h s) -> p h s", h=4), axis=mybir.AxisListType.X, op=ALU.max),
                     reads=[tpq], writes=[tk])
                nm_, tnm_ = (NML4, t_NML4) if dr == 0 else (NMU4, t_NMU4)
                P.op("dve", lambda e, pq=pq, nm_=nm_: e.tensor_tensor(out=scr[:, 0:512], in0=pq[:, 0:512], in1=nm_[:].rearrange("p h s -> p (h s)"), op=ALU.add),
                     reads=[tpq, tnm_], writes=[t_scr])
                P.op("dve", lambda e, dr=dr: e.tensor_reduce(out=G_[:, 5, dr * 4:dr * 4 + 4], in_=scr[:, 0:512].rearrange("p (h s) -> p h s", h=4), axis=mybir.AxisListType.X, op=ALU.max),
                     reads=[t_scr], writes=[tk])
            P.op("dve", lambda e: e.tensor_tensor(out=small[:, 40:48], in0=G_[:, 0, :], in1=G_[:, 3, :], op=ALU.subtract), reads=[tk], writes=[t_small])
            P.op("act", lambda e: e.activation(out=G_[:, 4, :], in_=small[:, 40:48], func=AF.Exp), reads=[t_small], writes=[tk])

        def scaled_v(slot, dr):
            for h in range(4):
                P.op("act", lambda e, h=h: e.activation(
                    out=vsc[:, dr, h, :], in_=vext[:, slot, h, :], func=AF.Identity, scale=gt[:, slot, 4, dr * 4 + h:dr * 4 + h + 1]),
                    reads=[t_vext[slot], t_gt[slot]], writes=[t_vsc])

        def d_matmuls(slot, dr):
            res = []
            pbs = [bank() for _ in range(2)]
            for h in range(4):
                pb, tpb = pbs[h // 3]
                o = (h % 3) * 132
                P.op("pe", lambda e, pb=pb, o=o, h=h: e.matmul(pb[:, o:o + 129], lhsT=ktm[:, slot, h * 128:(h + 1) * 128], rhs=vsc[:, dr, h, :], start=True, stop=True),
                     reads=[t_ktm[slot], t_vsc], writes=[tpb])
                res.append((pb[:, o:o + 129], tpb))
            return res

        def state_update(slot, dr, mask_col=None, t_mask=None):
            scaled_v(slot, dr)
            Dl = d_matmuls(slot, dr)
            c0 = dr * 4
            apply_update(dr, Dl, gt[:, slot, 3, c0:c0 + 4], gt[:, slot, 2, c0:c0 + 4], t_gt[slot], mask_col, t_mask)

        def apply_update(dr, Dl, Gm, BLc, t_src, mask_col=None, t_mask=None):
            c0 = dr * 4
            mcur = mst[:, c0:c0 + 4]
            if mask_col is not None:
                P.op("dve", lambda e: e.tensor_scalar(out=sm2[:, 0:4], in0=Gm, scalar1=BIG, scalar2=mask_col, op0=ALU.add, op1=ALU.mult), reads=[t_src, t_mask], writes=[t_sm2])
                P.op("dve", lambda e: e.tensor_scalar(out=sm2[:, 0:4], in0=sm2[:, 0:4], scalar1=-BIG, scalar2=None, op0=ALU.add), reads=[t_sm2], writes=[t_sm2])
                P.op("dve", lambda e: e.tensor_scalar(out=sm2[:, 4:8], in0=BLc, scalar1=mask_col, scalar2=None, op0=ALU.mult), reads=[t_src, t_mask], writes=[t_sm2])
            else:
                P.op("dve", lambda e: e.tensor_copy(out=sm2[:, 0:4], in_=Gm), reads=[t_src], writes=[t_sm2])
                P.op("dve", lambda e: e.tensor_copy(out=sm2[:, 4:8], in_=BLc), reads=[t_src], writes=[t_sm2])
            P.op("dve", lambda e: e.tensor_tensor(out=sm2[:, 8:12], in0=mcur, in1=sm2[:, 0:4], op=ALU.max), reads=[t_mst, t_sm2], writes=[t_sm2])
            P.op("dve", lambda e: e.tensor_tensor(out=sm2[:, 12:16], in0=mcur, in1=sm2[:, 8:12], op=ALU.subtract), reads=[t_mst, t_sm2], writes=[t_sm2])
            P.op("dve", lambda e: e.tensor_tensor(out=sm2[:, 16:20], in0=sm2[:, 0:4], in1=sm2[:, 8:12], op=ALU.subtract), reads=[t_sm2], writes=[t_sm2])
            P.op("act", lambda e: e.activation(out=sm2[:, 12:20], in_=sm2[:, 12:20], func=AF.Exp), reads=[t_sm2], writes=[t_sm2])
            for h in range(4):
                u = c0 + h
                dps, tdps = Dl[h]
                P.op("dve", lambda e, u=u, h=h: e.tensor_scalar(out=Sst[:, u, :], in0=Sst[:, u, :], scalar1=sm2[:, 12 + h:13 + h], scalar2=None, op0=ALU.mult),
                     reads=[t_S[u], t_sm2], writes=[t_S[u]])
                P.op("dve", lambda e, u=u, h=h, dps=dps: e.scalar_tensor_tensor(out=Sst[:, u, :], in0=dps, scalar=sm2[:, 16 + h:17 + h], in1=Sst[:, u, :], op0=ALU.mult, op1=ALU.add),
                     reads=[tdps, t_S[u], t_sm2], writes=[t_S[u]])
                P.op("act", lambda e, u=u: e.copy(out=Sbf[:, u, :], in_=Sst[:, u, :]), reads=[t_S[u]], writes=[t_Sbf[u]])
            P.op("dve", lambda e: e.tensor_tensor(out=mcur, in0=sm2[:, 4:8], in1=sm2[:, 8:12], op=ALU.add), reads=[t_sm2], writes=[t_mst])

        def make_uT(src_f32, t_src, cond, vec_shift, vec_scale, dst_fn, t_dst, want_f32=None, t_f32=None):
            xb_i = 0
            P.op("dve", lambda e: e.tensor_copy(out=xtb[:, xb_i, :], in_=src_f32), reads=[t_src], writes=[t_xtb[xb_i]])
            for half in range(2):
                pb, tpb = bank()
                pbb = pb[:].bitcast(BF16)
                for q in range(4):
                    kc = half * 4 + q
                    P.op("pe", lambda e, q=q, kc=kc, pbb=pbb: e.transpose(pbb[:, q * 128:(q + 1) * 128], xtb[:, xb_i, kc * 128:(kc + 1) * 128], ident_b[:]),
                         reads=[t_xtb[xb_i], t_idb], writes=[tpb])
                for q in range(4):
                    kc = half * 4 + q
                    P.op("act", lambda e, q=q, kc=kc, pbb=pbb: e.activation(
                        out=dst_fn(kc), in_=pbb[:, q * 128:(q + 1) * 128], func=AF.Identity,
                        scale=modT[:, vec_scale, kc, cond:cond + 1], bias=modT[:, vec_shift, kc, cond:cond + 1]),
                        reads=[tpb, t_modT], writes=[t_dst])

        def load_wpiece(src2d, c0, ncols, row_scale=False, eng=None, into=None):
            if into is None:
                i = wb_ctr[0] % WB_N
                wb_ctr[0] += 1
                buf, tk = wb[i], t_wb[i]
            else:
                buf, tk = into
            cc = 0
            while cc < ncols:
                w = min(256, ncols - cc)
                s_t, s_k = stage_load_w(src2d, c0 + cc, w)
                if not row_scale:
                    do_copy(eng or cast_eng(), buf[:, :, cc:cc + w], s_t[:, :, 0:w], [s_k], [tk])
                else:
                    do_copy("act", buf[:, 4:8, cc:cc + w], s_t[:, 4:8, 0:w], [s_k], [tk])
                    for kc in range(4):
                        P.op("dve", lambda e, kc=kc, cc=cc, w=w, s_t=s_t, buf=buf: e.tensor_scalar(
                            out=buf[:, kc, cc:cc + w], in0=s_t[:, kc, 0:w], scalar1=nwT[:, kc:kc + 1], scalar2=None, op0=ALU.mult),
                            reads=[s_k, t_nwT], writes=[tk])
                cc += w
            return buf, tk

        sm2 = sb("sm2", [128, 32], F32)
        t_sm2 = P.tok("sm2")
        st6 = sb("st6", [128, 4, 6], F32)
        t_st6 = P.tok("st6")
        Cout = arena3[:, :].rearrange("p (a b) -> p a b", a=8)
        t_Cout = P.tok("Cout")
        hnb = sb("hnb", [128, 512], F32)
        t_hnb = P.tok("hnb")
        hmb = sb("hmb", [128, 512], BF16)
        t_hmb = P.tok("hmb")
        lg = sb("lg", [128, 4, NE], F32)
        t_lg = P.tok("lg")
        P.op("pool", lambda e: e.memset(vext[:, :, :, 128:129], 1.0), writes=t_vext)

        def layer_norm_inplace(xi, tw, tb):
            xv = X[:, xi, :]
            for hf in range(2):
                P.op("dve", lambda e, hf=hf: e.bn_stats(out=st6[:, hf, :], in_=X[:, xi, hf * 512:(hf + 1) * 512]), reads=[tX[xi]], writes=[t_st6])
            P.op("dve", lambda e: e.bn_aggr(out=sm2[:, 0:2], in_=st6[:, 0:2, :].rearrange("p a b -> p (a b)")), reads=[t_st6], writes=[t_sm2])
            P.op("act", lambda e: e.activation(out=sm2[:, 2:3], in_=sm2[:, 1:2], func=AF.Sqrt, bias=eps_t[:], scale=1.0), reads=[t_sm2, t_ones], writes=[t_sm2])
            P.op("dve", lambda e: e.reciprocal(out=sm2[:, 3:4], in_=sm2[:, 2:3]), reads=[t_sm2], writes=[t_sm2])
            P.op("dve", lambda e: e.scalar_tensor_tensor(out=sm2[:, 4:5], in0=sm2[:, 0:1], scalar=-1.0, in1=sm2[:, 3:4], op0=ALU.mult, op1=ALU.mult),
                 reads=[t_sm2], writes=[t_sm2])
            P.op("act", lambda e: e.activation(out=xv, in_=xv, func=AF.Identity, scale=sm2[:, 3:4], bias=sm2[:, 4:5]), reads=[tX[xi], t_sm2], writes=[tX[xi]])
            P.op("dve", lambda e: e.tensor_tensor(out=xv, in0=xv, in1=lnw[:], op=ALU.mult), reads=[tX[xi], tw], writes=[tX[xi]])
            P.op("dve", lambda e: e.tensor_tensor(out=xv, in0=xv, in1=lnb[:], op=ALU.add), reads=[tX[xi], tb], writes=[tX[xi]])

        sTb = sb("sTb", [128, 16], BF16)
        t_sTb = P.tok("sTb")
        P.op("dve", lambda e: e.tensor_copy(out=sTb[:], in_=sT[:]), reads=[t_sT], writes=[t_sTb])

        def mod_vectors(l, vecs):
            pmT, t_pmT = PLONG, t_PLONG
            plist = [(v, sub) for v in vecs for sub in range(2)]
            nxt = load_wpiece(DI["w_ada"][l], (plist[0][0] * 2 + plist[0][1]) * 512, 512)
            for pi, (v, sub) in enumerate(plist):
                pc = v * 2 + sub
                wbuf, twb = nxt
                if pi + 1 < len(plist):
                    nxt = load_wpiece(DI["w_ada"][l], (plist[pi + 1][0] * 2 + plist[pi + 1][1]) * 512, 512)
                pr, tpr = bank()
                for kc in range(8):
                    P.op("pe", lambda e, kc=kc, wbuf=wbuf, pr=pr: e.matmul(pr[0:2, 0:512], lhsT=sTb[:, 2 * kc:2 * kc + 2], rhs=wbuf[:, kc, :],
                                                                          start=(kc == 0), stop=(kc == 7)), reads=[twb, t_sTb], writes=[tpr])
                mr = tmpA[0:2, pc % 2, :]
                tmr = t_tmpA[pc % 2]
                bp = tmpC[0:2, pc % 2, :]
                tbp = t_tmpC[pc % 2]
                P.dma("sp", lambda e, pc=pc, bp=bp: e.dma_start(out=bp, in_=DI["b_ada2"][l][:, pc * 512:(pc + 1) * 512]), writes=[tbp])
                P.op("dve", lambda e, pr=pr, mr=mr, bp=bp: e.tensor_tensor(out=mr, in0=pr[0:2, 0:512], in1=bp, op=ALU.add),
                     reads=[tpr, tbp], writes=[tmr])
                if v in (2, 5):
                    for cond in range(2):
                        pg_, tpg_ = bank()
                        P.op("pe", lambda e, cond=cond, pg_=pg_, mr=mr: e.matmul(pg_[:, 0:512], lhsT=selr[0:2, cond, :], rhs=mr, start=True, stop=True),
                             reads=[tmr, t_selr], writes=[tpg_])
                        P.op("act", lambda e, cond=cond, sub=sub, pg_=pg_: e.copy(out=gb[:, cond, sub * 512:(sub + 1) * 512], in_=pg_[:, 0:512]),
                             reads=[tpg_], writes=[t_gb])
                else:
                    for q in range(4):
                        ch = sub * 4 + q
                        c0 = (v * 8 + ch) * 2
                        P.op("pe", lambda e, c0=c0, q=q, mr=mr: e.transpose(pmT[:, c0:c0 + 2], mr[:, q * 128:(q + 1) * 128], ident_f[0:2, 0:2]),
                             reads=[tmr, t_idf], writes=[t_pmT])
            for v in vecs:
                if v in (2, 5):
                    continue
                if v in (1, 4):
                    P.op("dve", lambda e, v=v: e.tensor_scalar(out=modT[:, v].rearrange("p a b -> p (a b)"), in0=pmT[:, v * 16:(v + 1) * 16], scalar1=1.0, scalar2=None, op0=ALU.add),
                         reads=[t_pmT], writes=[t_modT])
                else:
                    P.op("dve", lambda e, v=v: e.tensor_copy(out=modT[:, v].rearrange("p a b -> p (a b)"), in_=pmT[:, v * 16:(v + 1) * 16]),
                         reads=[t_pmT], writes=[t_modT])

        def tm_proj(slot, wbuf, twb, ncols, evac):
            pb, tpb = bank()
            for kc in range(8):
                P.op("pe", lambda e, kc=kc, pb=pb, wbuf=wbuf: e.matmul(pb[:, 0:ncols], lhsT=uT[:, kc, slot * 128:(slot + 1) * 128], rhs=wbuf[:, kc, 0:ncols],
                                                                      start=(kc == 0), stop=(kc == 7)), reads=[t_uT[slot], twb], writes=[tpb])
            evac(pb, tpb)

        gpc = sb("gpc", [128, 8, 16], BF16)
        t_gpc = P.tok("gpc")
        zpc = arena[:, 8192:12288].rearrange("p (a b) -> p a b", a=8)
        t_zpc = P.tok("zpc")

        cur_l = [0]
        t_dstg = P.toks(2, "dstg")
        okey = {n: P.tok("o_" + n) for n in ("yp", "ys", "nC", "nn", "nm")}
        out_toks.extend(okey.values())

        def process_item(l, tiles, cond, is_sample, seq_out):
            T = len(tiles)
            N = 128 * T
            for tau, xi in enumerate(tiles):
                make_uT(X[:, xi, :], tX[xi], cond, 0, 1, lambda kc, tau=tau: uT[:, kc, tau * 128:(tau + 1) * 128], t_uT[tau])
            def ev_k(pb, tpb, tau):
                P.op("act", lambda e: e.mul(out=ktm[:, tau, :], in_=pb[:, 0:512], mul=SCALE_K), reads=[tpb], writes=[t_ktm[tau]])

            def ev_v(pb, tpb, tau):
                P.op("dve", lambda e: e.tensor_copy(out=vext[:, tau, :, 0:128], in_=pb[:, 0:512].rearrange("p (h d) -> p h d", h=4)),
                     reads=[tpb], writes=[t_vext[tau]])

            def ev_o(pb, tpb, tau):
                P.op("act", lambda e: e.activation(out=og[:, tau, :], in_=pb[:, 0:512], func=AF.Sigmoid), reads=[tpb], writes=[t_og[tau]])

            def ev_g(pb, tpb, tau):
                gate_math(pb[:, 0:16], tpb, tau)

            def ev_z(pb, tpb, tau):
                P.op("act", lambda e: e.copy(out=zown[:, tau, :], in_=pb[:, 0:512]), reads=[tpb], writes=[t_zown[tau]])

            plist = [("q", 0, 512, None), ("k", 512, 512, ev_k), ("v", 1024, 512, ev_v), ("o", 1536, 512, ev_o), ("g", G_OFF, 16, ev_g)]
            if not is_sample:
                plist.append(("z", F_OFF, 512, ev_z))
            nxt = load_wpiece(DI["w_in"][l], plist[0][1], plist[0][2])
            for pi, (pname, col0, ncols, evac) in enumerate(plist):
                wbuf, twb = nxt
                if pi + 1 < len(plist):
                    nxt = load_wpiece(DI["w_in"][l], plist[pi + 1][1], plist[pi + 1][2])
                if pname in ("q", "k"):
                    for h in range(4):
                        pb, tpb = bank()
                        for kc in range(8):
                            P.op("pe", lambda e, kc=kc, h=h, pb=pb, wbuf=wbuf: e.matmul(pb[:, 0:N], lhsT=wbuf[:, kc, h * 128:(h + 1) * 128], rhs=uT[:, kc, 0:N],
                                                                                       start=(kc == 0), stop=(kc == 7)), reads=t_uT[0:T] + [twb], writes=[tpb])
                        if pname == "q":
                            P.op("act", lambda e, h=h, pb=pb: e.copy(out=qT[:, h, 0:N], in_=pb[:, 0:N]), reads=[tpb], writes=[t_qT])
                        else:
                            P.op("act", lambda e, h=h, pb=pb: e.mul(out=kT[:, h, 0:N], in_=pb[:, 0:N], mul=SCALE_K), reads=[tpb], writes=[t_kT])
                if evac is not None:
                    for tau in range(T):
                        tm_proj(tau, wbuf, twb, ncols, lambda pb, tpb, tau=tau, evac=evac: evac(pb, tpb, tau))
            if not is_sample:
                P.op("pool", lambda e: e.memset(Sst[:], 0.0), writes=t_S)
                P.op("pool", lambda e: e.memset(Sbf[:], 0.0), writes=t_Sbf)
                P.op("pool", lambda e: e.memset(mst[:], 0.0), writes=[t_mst])
            else:
                for u in range(8):
                    P.op("act", lambda e, u=u: e.copy(out=Sbf[:, u, :], in_=Sst[:, u, :]), reads=[t_S[u]], writes=[t_Sbf[u]])
            for dr in range(2):
                order = list(range(T)) if dr == 0 else list(range(T - 1, -1, -1))
                nm_, tnm_ = (NMU4, t_NMU4) if dr == 0 else (NML4, t_NML4)
                c0 = dr * 4
                for oi, tau in enumerate(order):
                    sx = (dr * T + oi) % 2
                    G_ = gt[:, tau]
                    P.op("dve", lambda e, G_=G_: e.tensor_tensor(out=sm2[:, 20:24], in0=G_[:, 5, c0:c0 + 4], in1=mst[:, c0:c0 + 4], op=ALU.max), reads=[t_gt[tau], t_mst], writes=[t_sm2])
                    P.op("dve", lambda e: e.tensor_tensor(out=sm2[:, 24:28], in0=mst[:, c0:c0 + 4], in1=sm2[:, 20:24], op=ALU.subtract), reads=[t_mst, t_sm2], writes=[t_sm2])
                    P.op("dve", lambda e, G_=G_: e.scalar_tensor_tensor(out=sm2[:, 28:32], in0=G_[:, 1, c0:c0 + 4], scalar=-1.0, in1=sm2[:, 20:24], op0=ALU.mult, op1=ALU.subtract),
                         reads=[t_gt[tau], t_sm2], writes=[t_sm2])
                    P.op("act", lambda e: e.activation(out=sm2[:, 24:32], in_=sm2[:, 24:32], func=AF.Exp), reads=[t_sm2], writes=[t_sm2])
                    for h in range(4):
                        P.op("dve", lambda e, h=h: e.tensor_scalar(out=hnb[:, h * 128:(h + 1) * 128], in0=ident_f[:], scalar1=sm2[:, 20 + h:21 + h], scalar2=-1.0, op0=ALU.mult, op1=ALU.mult),
                             reads=[t_sm2, t_idf], writes=[t_hnb])
                    pr_, tpr_ = bank()
                    P.op("pe", lambda e, pr_=pr_: e.matmul(pr_[:, 0:512], lhsT=ones_f[:], rhs=hnb[:, 0:512], start=True, stop=False), reads=[t_hnb, t_ones], writes=[tpr_])
                    P.op("pe", lambda e, pr_=pr_, nm_=nm_: e.matmul(pr_[:, 0:512], lhsT=ident_f[:], rhs=nm_[:].rearrange("p h s -> p (h s)"), start=False, stop=True),
                         reads=[tnm_, t_idf], writes=[tpr_])
                    for h in range(4):
                        P.op("act", lambda e, h=h, pr_=pr_, G_=G_: e.activation(out=scr[:, h * 128:(h + 1) * 128], in_=pr_[:, h * 128:(h + 1) * 128], func=AF.Exp,
                                                                           bias=G_[:, 0, c0 + h:c0 + h + 1], scale=1.0), reads=[tpr_, t_gt[tau]], writes=[t_scr])
                    pst, tpst = bank()
                    for h in range(4):
                        P.op("pe", lambda e, h=h, pst=pst: e.matmul(pst[:, h * 128:(h + 1) * 128], lhsT=kT[:, h, tau * 128:(tau + 1) * 128],
                                                                   rhs=qT[:, h, tau * 128:(tau + 1) * 128], start=True, stop=True),
                             reads=[t_kT, t_qT], writes=[tpst])
                    P.op("dve", lambda e, pst=pst: e.tensor_tensor(out=STx[:, sx].rearrange("p h t -> p (h t)"), in0=pst[:, 0:512], in1=scr[:, 0:512], op=ALU.mult),
                         reads=[tpst, t_scr], writes=[t_STx[sx]])
                    for h in range(4):
                        u = c0 + h
                        ni = h % 2
                        po, tpo = bank()
                        P.op("pe", lambda e, h=h, po=po: e.matmul(po[:, 0:129], lhsT=STx[:, sx, h, :], rhs=vext[:, tau, h, :], start=True, stop=True),
                             reads=[t_STx[sx], t_vext[tau]], writes=[tpo])
                        P.op("pe", lambda e, h=h, u=u, po=po: e.matmul(po[:, 132:261], lhsT=qT[:, h, tau * 128:(tau + 1) * 128], rhs=Sbf[:, u, :], start=True, stop=True),
                             reads=[t_qT, t_Sbf[u]], writes=[tpo])
                        P.op("act", lambda e, h=h, po=po, ni=ni: e.activation(out=nd[:, ni, :], in_=po[:, 132:261], func=AF.Identity, scale=sm2[:, 24 + h:25 + h]),
                             reads=[tpo, t_sm2], writes=[t_nd[ni]])
                        P.op("dve", lambda e, po=po, ni=ni: e.tensor_tensor(out=nd[:, ni, :], in0=po[:, 0:129], in1=nd[:, ni, :], op=ALU.add), reads=[tpo, t_nd[ni]], writes=[t_nd[ni]])
                        P.op("dve", lambda e, ni=ni: e.scalar_tensor_tensor(out=sm2[:, 8:9], in0=nd[:, ni, 128:129], scalar=-1.0, in1=nd[:, ni, 128:129], op0=ALU.mult, op1=ALU.max),
                             reads=[t_nd[ni]], writes=[t_sm2])
                        P.op("dve", lambda e, h=h: e.tensor_tensor(out=sm2[:, 9:10], in0=sm2[:, 8:9], in1=sm2[:, 28 + h:29 + h], op=ALU.max), reads=[t_sm2], writes=[t_sm2])
                        P.op("dve", lambda e: e.reciprocal(out=sm2[:, 10:11], in_=sm2[:, 9:10]), reads=[t_sm2], writes=[t_sm2])
                        if dr == 0:
                            P.op("dve", lambda e, h=h, ni=ni: e.tensor_scalar(out=hacc[:, tau, h * 128:(h + 1) * 128], in0=nd[:, ni, 0:128], scalar1=sm2[:, 10:11], scalar2=None, op0=ALU.mult),
                                 reads=[t_nd[ni], t_sm2], writes=[t_hacc[tau]])
                        else:
                            P.op("dve", lambda e, h=h, ni=ni: e.scalar_tensor_tensor(out=hacc[:, tau, h * 128:(h + 1) * 128], in0=nd[:, ni, 0:128], scalar=sm2[:, 10:11],
                                                                                 in1=hacc[:, tau, h * 128:(h + 1) * 128], op0=ALU.mult, op1=ALU.add),
                                 reads=[t_nd[ni], t_sm2, t_hacc[tau]], writes=[t_hacc[tau]])
                    if (not is_sample) or oi < T - 1:
                        state_update(tau, dr)
                if _DBG and l == 0 and seq_out == 1:
                    tkh = P.tok("dbgh")
                    out_toks.append(tkh)
                    P.dma("sp", lambda e, dr=dr: e.dma_start(out=DO["dbg_h1" if dr == 0 else "dbg_h2"], in_=hacc[:, 0:2, :]), reads=t_hacc[0:2], writes=[tkh])
                    if dr == 0:
                        for nm2, src, rd in [("dbg_stx", STx[:], t_STx), ("dbg_scr", scr[:], [t_scr]), ("dbg_nd", nd[:], t_nd), ("dbg_sm2", sm2[:], [t_sm2])]:
                            tkq = P.tok(nm2)
                            out_toks.append(tkq)
                            P.dma("sp", lambda e, nm2=nm2, src=src: e.dma_start(out=DO[nm2], in_=src), reads=rd, writes=[tkq])
                        tkq = P.tok("dbgqk")
                        out_toks.append(tkq)
                        P.dma("sp", lambda e: e.dma_start(out=DO["dbg_qk"][:, 0], in_=qT[:, :, 0:256]), reads=[t_qT], writes=[tkq])
                        P.dma("sp", lambda e: e.dma_start(out=DO["dbg_qk"][:, 1], in_=kT[:, :, 0:256]), reads=[t_kT], writes=[tkq])
                        tkg = P.tok("dbgg")
                        out_toks.append(tkg)
                        P.dma("sp", lambda e: e.dma_start(out=DO["dbg_gt"], in_=gt[:, 0:2].rearrange("p a b c -> p a (b c)")), reads=t_gt[0:2], writes=[tkg])
            if not is_sample:
                P.dma("sp", lambda e: e.dma_start(out=DO["nm"][seq_out, l].rearrange("(o u) -> o u", o=1), in_=mst[0:1, 0:8]), reads=[t_mst], writes=[], key=okey["nm"])
                P.dma("sp", lambda e: e.dma_start(out=DO["nC"][seq_out, l].rearrange("u d e -> d u e"), in_=Sst[:, :, 0:128]), reads=t_S, writes=[], key=okey["nC"])
                P.dma("sp", lambda e: e.dma_start(out=DO["nn"][seq_out, l].rearrange("u d -> d u"), in_=Sst[:, :, 128], allow_slow_non_contiguous=True), reads=t_S, writes=[], key=okey["nn"])
            for tau in range(T):
                for h in range(4):
                    P.op("dve", lambda e, h=h: e.bn_stats(out=st6[:, h, :], in_=hacc[:, tau, h * 128:(h + 1) * 128]), reads=[t_hacc[tau]], writes=[t_st6])
                for h in range(4):
                    P.op("dve", lambda e, h=h: e.bn_aggr(out=sm2[:, 2 * h:2 * h + 2], in_=st6[:, h, :]), reads=[t_st6], writes=[t_sm2])
                P.op("act", lambda e: e.activation(out=sm2[:, 8:12], in_=sm2[:, 0:8].rearrange("p (h t) -> p h t", t=2)[:, :, 1], func=AF.Sqrt, bias=eps_t[:], scale=1.0),
                     reads=[t_sm2, t_ones], writes=[t_sm2])
                P.op("dve", lambda e: e.reciprocal(out=sm2[:, 12:16], in_=sm2[:, 8:12]), reads=[t_sm2], writes=[t_sm2])
                P.op("dve", lambda e: e.scalar_tensor_tensor(out=sm2[:, 16:20], in0=sm2[:, 0:8].rearrange("p (h t) -> p h t", t=2)[:, :, 0], scalar=-1.0, in1=sm2[:, 12:16],
                                                             op0=ALU.mult, op1=ALU.mult), reads=[t_sm2], writes=[t_sm2])
                for h in range(4):
                    P.op("act", lambda e, h=h: e.activation(out=hnb[:, h * 128:(h + 1) * 128], in_=hacc[:, tau, h * 128:(h + 1) * 128], func=AF.Identity,
                                                            scale=sm2[:, 12 + h:13 + h], bias=sm2[:, 16 + h:17 + h]), reads=[t_hacc[tau], t_sm2], writes=[t_hnb])
                P.op("dve", lambda e: e.tensor_tensor(out=hmb[:], in0=hnb[:], in1=og[:, tau, :], op=ALU.mult), reads=[t_hnb, t_og[tau]], writes=[t_hmb])
                pb, tpb = bank()
                pbb = pb[:].bitcast(BF16)
                for h in range(4):
                    P.op("pe", lambda e, h=h, pbb=pbb: e.transpose(pbb[:, h * 128:(h + 1) * 128], hmb[:, h * 128:(h + 1) * 128], ident_b[:]), reads=[t_hmb, t_idb], writes=[tpb])
                P.op("act", lambda e, pbb=pbb: e.copy(out=hmT[:, 0:4, tau * 128:(tau + 1) * 128], in_=pbb[:, 0:512].rearrange("p (h t) -> p h t", h=4)),
                     reads=[tpb], writes=[t_hmT[tau]])
            if not is_sample:
                for g in range(4):
                    pbs = []
                    for cs, (tab, ttab) in enumerate(((c256, t_c256), (s256, t_s256))):
                        pb, tpb = bank()
                        for mc in range(2):
                            P.op("pe", lambda e, mc=mc, g=g, pb=pb, tab=tab: e.matmul(pb[:, 0:256], lhsT=zown[:, mc, g * 128:(g + 1) * 128], rhs=tab[:, mc, :],
                                                                                     start=(mc == 0), stop=(mc == 1)), reads=[t_zown[mc], ttab], writes=[tpb])
                        P.op("act" if cs else "dve", (lambda e, cs=cs, g=g, pb=pb: e.copy(out=p12[:, g % 2, cs, 0:256], in_=pb[:, 0:256])) if cs else
                             (lambda e, cs=cs, g=g, pb=pb: e.tensor_copy(out=p12[:, g % 2, cs, 0:256], in_=pb[:, 0:256])), reads=[tpb], writes=[t_p12[g % 2]])
                    py, tpy = bank()
                    P.op("pe", lambda e, g=g, py=py: e.matmul(py[:, 0:256], lhsT=consts["ccp"][0][:], rhs=p12[:, g % 2, 0, 0:256], start=True, stop=False),
                         reads=[t_p12[g % 2], consts["ccp"][1]], writes=[tpy])
                    P.op("pe", lambda e, g=g, py=py: e.matmul(py[:, 0:256], lhsT=consts["nscp"][0][:], rhs=p12[:, g % 2, 1, 0:256], start=False, stop=True),
                         reads=[t_p12[g % 2], consts["nscp"][1]], writes=[tpy])
                    P.op("act", lambda e, g=g, py=py: e.copy(out=hmT[:, 4 + g, 0:256], in_=py[:, 0:256]), reads=[tpy], writes=[t_fmT] + t_hmT[0:2])
            else:
                for gp in range(2):
                    pbs = [bank() for _ in range(4)]
                    for mc in range(16):
                        bi = mc % 2
                        P.dma("sp", lambda e, mc=mc, bi=bi: e.dma_start(out=dtab[:, bi, 0, :], in_=DI["cn_own"][mc * 128:(mc + 1) * 128, :]), writes=[t_dtab[bi]])
                        P.dma("sp", lambda e, mc=mc, bi=bi: e.dma_start(out=dtab[:, bi, 1, :], in_=DI["sn_own"][mc * 128:(mc + 1) * 128, :]), writes=[t_dtab[bi]])
                        P.dma("sp", lambda e, mc=mc, bi=bi: e.dma_start(out=zst[:, bi, :], in_=zscr.ap()[mc * 128:(mc + 1) * 128, :]), reads=[t_zscr], writes=[t_zst[bi]])
                        for gl in range(2):
                            g = gp * 2 + gl
                            for cs in range(2):
                                pb, tpb = pbs[gl * 2 + cs]
                                P.op("pe", lambda e, mc=mc, g=g, cs=cs, pb=pb, bi=bi: e.matmul(pb[:, 0:512], lhsT=zst[:, bi, g * 128:(g + 1) * 128], rhs=dtab[:, bi, cs, :],
                                                                                              start=(mc == 0), stop=(mc == 15)), reads=[t_zst[bi], t_dtab[bi]], writes=[tpb])
                    for gl in range(2):
                        g = gp * 2 + gl
                        for cs in range(2):
                            pb, tpb = pbs[gl * 2 + cs]
                            if cs:
                                P.op("act", lambda e, gl=gl, cs=cs, pb=pb: e.copy(out=p12[:, gl, cs, :], in_=pb[:, 0:512]), reads=[tpb], writes=[t_p12[gl]])
                            else:
                                P.op("dve", lambda e, gl=gl, cs=cs, pb=pb: e.tensor_copy(out=p12[:, gl, cs, :], in_=pb[:, 0:512]), reads=[tpb], writes=[t_p12[gl]])
                        py, tpy = bank()
                        P.op("pe", lambda e, gl=gl, py=py: e.matmul(py[:, 0:512], lhsT=consts["ccs"][0][:], rhs=p12[:, gl, 0, :], start=True, stop=False),
                             reads=[t_p12[gl], consts["ccs"][1]], writes=[tpy])
                        P.op("pe", lambda e, gl=gl, py=py: e.matmul(py[:, 0:512], lhsT=consts["nscs"][0][:], rhs=p12[:, gl, 1, :], start=False, stop=True),
                             reads=[t_p12[gl], consts["nscs"][1]], writes=[tpy])
                        P.op("act", lambda e, g=g, py=py: e.copy(out=hmT[:, 4 + g, 0:512], in_=py[:, 0:512]), reads=[tpy], writes=[t_fmT] + t_hmT)
            if _DBG and l == 0 and seq_out == 1:
                tkd = P.tok("dbgmix")
                out_toks.append(tkd)
                P.dma("sp", lambda e: e.dma_start(out=DO["dbg_mix"], in_=hmT[:, :, :]), reads=t_hmT + [t_fmT], writes=[tkd])
            wo = [load_wpiece(DI["w_out"][l], hf * 512, 512, row_scale=True) for hf in range(2)]
            for tau, xi in enumerate(tiles):
                for hf in range(2):
                    wbuf, twb = wo[hf]
                    pb, tpb = bank()
                    for fc in range(8):
                        P.op("pe", lambda e, fc=fc, pb=pb, wbuf=wbuf: e.matmul(pb[:, 0:512], lhsT=hmT[:, fc, tau * 128:(tau + 1) * 128], rhs=wbuf[:, fc, :],
                                                                              start=(fc == 0), stop=(fc == 7)), reads=[t_hmT[tau], t_fmT, twb], writes=[tpb])
                    P.op("dve", lambda e, hf=hf, pb=pb: e.tensor_tensor(out=xt[:, 0, hf * 512:(hf + 1) * 512], in0=pb[:, 0:512], in1=gb[:, cond, hf * 512:(hf + 1) * 512], op=ALU.mult),
                         reads=[tpb, t_gb], writes=[t_xt[0]])
                P.op("dve", lambda e, xi=xi: e.scalar_tensor_tensor(out=X[:, xi, :], in0=X[:, xi, :], scalar=ALPHA, in1=xt[:, 0, :], op0=ALU.mult, op1=ALU.add),
                     reads=[tX[xi], t_xt[0]], writes=[tX[xi]])
                layer_norm_inplace(xi, t_lnw, t_lnb)

        for l in range(NL):
            cur_l[0] = l
            if l > 0:
                moe_toks = t_gs + [t for pair in t_actT for t in pair]
                for tk in t_uT + [t_qT, t_kT, t_fmT] + t_hmT + t_ktm + t_og:
                    P.alias(tk, moe_toks)
                P.alias(t_Cout, t_GT)
            P.dma("sp", lambda e, l=l: e.dma_start(out=nwT[:], in_=DI["nwT"][l]), writes=[t_nwT])
            P.dma("sp", lambda e, l=l: e.dma_start(out=gbias[:], in_=DI["gbias"][l]), writes=[t_gbias])
            P.dma("sp", lambda e, l=l: e.dma_start(out=rw[:], in_=DI["router_w"][l].rearrange("(kc p) n -> p kc n", p=128)), writes=[t_rw])
            P.dma("sp", lambda e, l=l: e.dma_start(out=rbb[:], in_=DI["rbb"][l]), writes=[t_rbb])
            P.dma("sp", lambda e, l=l: e.dma_start(out=bguT[:], in_=DI["bguT"][l]), writes=[t_bguT])
            P.dma("sp", lambda e, l=l: e.dma_start(out=bdn[:], in_=DI["b_down"][l]), writes=[t_bdn])
            P.dma("sp", lambda e, l=l: e.dma_start(out=smb[:], in_=DI["smb"][l]), writes=[t_smb])
            P.dma("sp", lambda e, l=l: e.dma_start(out=lnw[:], in_=DI["ln1w"][l]), writes=[t_lnw])
            P.dma("sp", lambda e, l=l: e.dma_start(out=lnb[:], in_=DI["ln1b"][l]), writes=[t_lnb])
            mod_vectors(l, [0, 1, 2])

            for tk in t_hacc:
                P.alias(tk, t_tmpA + t_tmpC)
            process_item(l, [0, 1], 0, False, 0)
            process_item(l, [2, 3], 0, False, 1)
            P.dma("sp", lambda e, l=l: e.dma_start(out=Sst[:, :, 0:128], in_=DI["sC"][l].rearrange("u d e -> d u e")), writes=t_S)
            P.dma("sp", lambda e, l=l: e.dma_start(out=sm2[:, 20:28], in_=DI["sn"][l]), writes=[t_sm2])
            P.op("dve", lambda e: e.tensor_copy(out=Sst[:, :, 128], in_=sm2[:, 20:28]), reads=[t_sm2], writes=t_S)
            P.op("dve", lambda e: e.tensor_copy(out=mst[:], in_=smb[:]), reads=[t_smb], writes=[t_mst])
            kpc = load_wpiece(DI["w_in"][l], 512, 512, into=(wb[0], t_wb[0]))
            vpc = load_wpiece(DI["w_in"][l], 1024, 512, into=(wb[1], t_wb[1]))
            gpiece = load_wpiece(DI["w_in"][l], G_OFF, 16, into=(gpc, t_gpc))
            P.alias(t_zpc, t_hmT + [t_fmT] + t_gs)
            zpiece = load_wpiece(DI["w_in"][l], F_OFF, 512, into=(zpc, t_zpc))
            steps = [(0, c) for c in range(16)]
            dstg = arena[:, 4096:8192].bitcast(F32)
            for tkd in t_dstg:
                P.alias(tkd, [t_qT, t_kT])

            def front(gi):
                sp_dir, c = steps[gi]
                slot = gi % 4
                xs = gi % 2
                if l == 0:
                    P.dma("sp", lambda e: e.dma_start(out=xt[:, xs, :], in_=DI["xs_full"][c * 128:(c + 1) * 128, :]), writes=[t_xt[xs]])
                    for hf in range(2):
                        P.dma("sp", lambda e, hf=hf: e.dma_start(out=scr[:, :], in_=DI["pos_full"][c * 128:(c + 1) * 128, hf * 512:(hf + 1) * 512]), writes=[t_scr])
                        P.op("dve", lambda e, hf=hf: e.tensor_tensor(out=xt[:, xs, hf * 512:(hf + 1) * 512], in0=xt[:, xs, hf * 512:(hf + 1) * 512], in1=scr[:], op=ALU.add),
                             reads=[t_xt[xs], t_scr], writes=[t_xt[xs]])
                else:
                    P.dma("sp", lambda e: e.dma_start(out=xt[:, xs, :], in_=gouts[c % 4].ap()[(c // 4) * 128:(c // 4 + 1) * 128, :]), reads=[t_gout[c % 4]], writes=[t_xt[xs]])
                make_uT(xt[:, xs, :], t_xt[xs], 1, 0, 1, lambda kc: uT[:, kc, slot * 128:(slot + 1) * 128], t_uT[slot])

                def ev_k(pb, tpb):
                    P.op("act", lambda e: e.mul(out=ktm[:, slot, :], in_=pb[:, 0:512], mul=SCALE_K), reads=[tpb], writes=[t_ktm[slot]])

                def ev_v(pb, tpb):
                    P.op("dve", lambda e: e.tensor_copy(out=vext[:, slot, :, 0:128], in_=pb[:, 0:512].rearrange("p (h d) -> p h d", h=4)),
                         reads=[tpb], writes=[t_vext[slot]])

                def ev_g(pb, tpb):
                    gate_math(pb[:, 0:16], tpb, slot)

                def ev_z(pb, tpb):
                    zb = c % 2
                    P.op("act", lambda e: e.copy(out=zown[:, zb, :], in_=pb[:, 0:512]), reads=[tpb], writes=[t_zown[zb]])
                    P.dma("sp", lambda e: e.dma_start(out=zscr.ap()[c * 128:(c + 1) * 128, :], in_=zown[:, zb, :]), reads=[t_zown[zb]], writes=[t_zscr])

                tm_proj(slot, kpc[0], kpc[1], 512, ev_k)
                tm_proj(slot, vpc[0], vpc[1], 512, ev_v)
                tm_proj(slot, gpiece[0], gpiece[1], 16, ev_g)
                tm_proj(slot, zpiece[0], zpiece[1], 512, ev_z)

            def back(gi):
                sp_dir, c = steps[gi]
                slot = gi % 4
                state_update(slot, 0, maskf[:, c:c + 1], t_maskf)
                bi = gi % 2
                scaled_v(slot, 1)
                Dl = d_matmuls(slot, 1)
                for h in range(4):
                    dps, tdps = Dl[h]
                    P.op("act", lambda e, h=h, dps=dps: e.copy(out=dstg[:, bi * 528 + h * 129:bi * 528 + (h + 1) * 129], in_=dps), reads=[tdps], writes=[t_dstg[bi]])
                P.op("dve", lambda e: e.tensor_copy(out=dstg[:, bi * 528 + 516:bi * 528 + 520], in_=gt[:, slot, 3, 4:8]), reads=[t_gt[slot]], writes=[t_dstg[bi]])
                P.op("dve", lambda e: e.tensor_copy(out=dstg[:, bi * 528 + 520:bi * 528 + 524], in_=gt[:, slot, 2, 4:8]), reads=[t_gt[slot]], writes=[t_dstg[bi]])
                P.dma("sp", lambda e: e.dma_start(out=dscr.ap()[c], in_=dstg[:, bi * 528:(bi + 1) * 528]), reads=[t_dstg[bi]], writes=[t_dscr])

            front(0)
            for gi in range(len(steps)):
                if gi + 1 < len(steps):
                    front(gi + 1)
                back(gi)
            for ci, c in enumerate(range(15, -1, -1)):
                bi = ci % 2
                P.dma("sp", lambda e: e.dma_start(out=dstg[:, bi * 528:(bi + 1) * 528], in_=dscr.ap()[c]), reads=[t_dscr], writes=[t_dstg[bi]])
                Dl = [(dstg[:, bi * 528 + h * 129:bi * 528 + (h + 1) * 129], t_dstg[bi]) for h in range(4)]
                apply_update(1, Dl, dstg[:, bi * 528 + 516:bi * 528 + 520], dstg[:, bi * 528 + 520:bi * 528 + 524], t_dstg[bi], maskb[:, c:c + 1], t_maskb)
            P.alias(t_qT, t_dstg)
            P.alias(t_kT, t_dstg)

            for tkz in t_hmT + [t_fmT]:
                P.alias(tkz, [t_zpc])
            process_item(l, [4, 5, 6, 7], 1, True, None)

            if _DBG and l == 0:
                tkd2 = P.tok("dbgx1")
                out_toks.append(tkd2)
                P.dma("sp", lambda e: e.dma_start(out=DO["dbg_x1"], in_=X[:, :, :]), reads=tX, writes=[tkd2])
            mixer_toks = t_uT + [t_qT, t_kT, t_fmT] + t_hmT + t_ktm + t_og
            for tk in t_gs + [t for pair in t_actT for t in pair]:
                P.alias(tk, mixer_toks)
            for tk in t_tmpA + t_tmpC:
                P.alias(tk, t_hacc)
            for tk in t_GT:
                P.alias(tk, [t_Cout])
            mod_vectors(l, [3, 4, 5])
            P.dma("sp", lambda e, l=l: e.dma_start(out=lnw[:], in_=DI["ln2w"][l]), writes=[t_lnw])
            P.dma("sp", lambda e, l=l: e.dma_start(out=lnb[:], in_=DI["ln2b"][l]), writes=[t_lnb])
            for xi in range(8):
                cond = 0 if xi < 4 else 1
                for half in range(2):
                    pb, tpb = bank()
                    for q in range(4):
                        kc = half * 4 + q
                        P.op("pe", lambda e, q=q, kc=kc, pb=pb, xi=xi: e.transpose(pb[:, q * 128:(q + 1) * 128], X[:, xi, kc * 128:(kc + 1) * 128], ident_f[:]),
                             reads=[tX[xi], t_idf], writes=[tpb])
                    for q in range(4):
                        kc = half * 4 + q
                        P.op("act", lambda e, q=q, kc=kc, pb=pb, cond=cond: e.activation(out=u2f[:, kc, :], in_=pb[:, q * 128:(q + 1) * 128], func=AF.Identity,
                                                                                         scale=modT[:, 4, kc, cond:cond + 1], bias=modT[:, 3, kc, cond:cond + 1]),
                             reads=[tpb, t_modT], writes=[t_u2f])
                P.op("act", lambda e, xi=xi: e.copy(out=u2T[:, :, xi * 128:(xi + 1) * 128], in_=u2f[:]), reads=[t_u2f], writes=[t_u2T[xi]])
                pl, tpl = bank()
                for kc in range(8):
                    P.op("pe", lambda e, kc=kc, pl=pl: e.matmul(pl[:, 0:NE], lhsT=u2f[:, kc, :], rhs=rw[:, kc, :], start=(kc == 0), stop=(kc == 7)),
                         reads=[t_u2f, t_rw], writes=[tpl])
                P.op("dve", lambda e, pl=pl: e.tensor_tensor(out=lg[:, 0, :], in0=pl[:, 0:NE], in1=rbb[:], op=ALU.add), reads=[tpl, t_rbb], writes=[t_lg])
                P.op("dve", lambda e: e.max(out=sm2[:, 0:8], in_=lg[:, 0, :]), reads=[t_lg], writes=[t_sm2])
                P.op("dve", lambda e: e.tensor_scalar(out=lg[:, 1, :], in0=lg[:, 0, :], scalar1=sm2[:, 3:4], scalar2=None, op0=ALU.is_ge), reads=[t_lg, t_sm2], writes=[t_lg])
                P.op("dve", lambda e: e.tensor_scalar(out=sm2[:, 8:9], in0=sm2[:, 0:1], scalar1=-1.0, scalar2=None, op0=ALU.mult), reads=[t_sm2], writes=[t_sm2])
                P.op("act", lambda e: e.activation(out=lg[:, 2, :], in_=lg[:, 0, :], func=AF.Exp, bias=sm2[:, 8:9], scale=1.0), reads=[t_lg, t_sm2], writes=[t_lg])
                P.op("dve", lambda e: e.tensor_tensor(out=lg[:, 3, :], in0=lg[:, 2, :], in1=lg[:, 1, :], op=ALU.mult), reads=[t_lg], writes=[t_lg])
                P.op("dve", lambda e: e.reduce_sum(out=sm2[:, 9:10], in_=lg[:, 3, :], axis=mybir.AxisListType.X), reads=[t_lg], writes=[t_sm2])
                P.op("dve", lambda e: e.reciprocal(out=sm2[:, 10:11], in_=sm2[:, 9:10]), reads=[t_sm2], writes=[t_sm2])
                P.op("dve", lambda e, xi=xi: e.tensor_scalar(out=Gall[:, xi, :], in0=lg[:, 3, :], scalar1=sm2[:, 10:11], scalar2=None, op0=ALU.mult),
                     reads=[t_lg, t_sm2], writes=[t_G[xi]])
                pg2, tpg2 = bank()
                P.op("pe", lambda e, xi=xi, pg2=pg2: e.transpose(pg2[0:NE, 0:128], Gall[:, xi, :], ident_f[:]), reads=[t_G[xi], t_idf], writes=[tpg2])
                P.op("act", lambda e, xi=xi, pg2=pg2: e.copy(out=GT[:, xi * 128:(xi + 1) * 128], in_=pg2[0:NE, 0:128]), reads=[tpg2], writes=[t_GT[xi]])
                P.op("dve", lambda e, xi=xi: e.tensor_scalar(out=X[:, xi, :], in0=X[:, xi, :], scalar1=ALPHA, scalar2=None, op0=ALU.mult), reads=[tX[xi]], writes=[tX[xi]])
                for hf in range(2):
                    pbb_, tpbb_ = bank()
                    P.op("pe", lambda e, xi=xi, hf=hf, pbb_=pbb_: e.matmul(pbb_[:, 0:512], lhsT=GT[:, xi * 128:(xi + 1) * 128], rhs=bdn[:, hf * 512:(hf + 1) * 512], start=True, stop=True),
                         reads=[t_GT[xi], t_bdn], writes=[tpbb_])
                    P.op("dve", lambda e, hf=hf, pbb_=pbb_, cond=cond: e.tensor_tensor(out=xt[:, 1, hf * 512:(hf + 1) * 512], in0=pbb_[:, 0:512], in1=gb[:, cond, hf * 512:(hf + 1) * 512], op=ALU.mult),
                         reads=[tpbb_, t_gb], writes=[t_xt[1]])
                P.op("dve", lambda e, xi=xi: e.tensor_tensor(out=X[:, xi, :], in0=X[:, xi, :], in1=xt[:, 1, :], op=ALU.add), reads=[tX[xi], t_xt[1]], writes=[tX[xi]])
            pieces = []
            for ex in range(NEX):
                pieces += [("glu", ex, 0, 0), ("lin", ex, 0, 1024), ("glu", ex, 1, 512), ("lin", ex, 1, 1536), ("down", ex, 0, 0), ("down", ex, 1, 512)]

            def fetch(pc):
                kind, ex, fb, c0 = pc
                src = DI["w_down"][l, ex] if kind == "down" else DI["w_gu"][l, ex]
                return load_wpiece(src, c0, 512, eng="act")

            nxt = fetch(pieces[0]) if pieces else None
            for pi, pc in enumerate(pieces):
                kind, ex, fb, c0 = pc
                wbuf, twb = nxt
                if pi + 1 < len(pieces):
                    nxt = fetch(pieces[pi + 1])
                if kind == "glu":
                    for fcl in range(4):
                        fc = fb * 4 + fcl
                        for th in range(2):
                            pb, tpb = bank()
                            for kc in range(8):
                                P.op("pe", lambda e, kc=kc, fcl=fcl, th=th, pb=pb, wbuf=wbuf: e.matmul(pb[:, 0:512], lhsT=wbuf[:, kc, fcl * 128:(fcl + 1) * 128], rhs=u2T[:, kc, th * 512:(th + 1) * 512],
                                                                                                   start=(kc == 0), stop=(kc == 7)), reads=[twb] + t_u2T[th * 4:th * 4 + 4], writes=[tpb])
                            i2 = (fcl * 2 + th) % 2
                            P.op("dve", lambda e, fc=fc, ex=ex, pb=pb, i2=i2: e.tensor_scalar(out=tmpA[:, i2, :], in0=pb[:, 0:512], scalar1=bguT[:, fc, ex:ex + 1], scalar2=LIM, op0=ALU.add, op1=ALU.min),
                                 reads=[tpb, t_bguT], writes=[t_tmpA[i2]])
                            P.op("act", lambda e, i2=i2: e.activation(out=tmpB[:, i2, :], in_=tmpA[:, i2, :], func=AF.Sigmoid, scale=SALPHA), reads=[t_tmpA[i2]], writes=[t_tmpB[i2]])
                            P.op("dve", lambda e, fcl=fcl, th=th, i2=i2: e.tensor_tensor(out=gsb[:, fcl, th * 512:(th + 1) * 512], in0=tmpA[:, i2, :], in1=tmpB[:, i2, :], op=ALU.mult),
                                 reads=[t_tmpA[i2], t_tmpB[i2]], writes=[t_gs[fcl * 2 + th]])
                elif kind == "lin":
                    for fcl in range(4):
                        fc = fb * 4 + fcl
                        for th in range(2):
                            pb, tpb = bank()
                            for kc in range(8):
                                P.op("pe", lambda e, kc=kc, fcl=fcl, th=th, pb=pb, wbuf=wbuf: e.matmul(pb[:, 0:512], lhsT=wbuf[:, kc, fcl * 128:(fcl + 1) * 128], rhs=u2T[:, kc, th * 512:(th + 1) * 512],
                                                                                                   start=(kc == 0), stop=(kc == 7)), reads=[twb] + t_u2T[th * 4:th * 4 + 4], writes=[tpb])
                            i2 = (fcl * 2 + th) % 2
                            P.op("act", lambda e, fc=fc, ex=ex, pb=pb, i2=i2: e.activation(out=tmpC[:, i2, :], in_=pb[:, 0:512], func=AF.Identity, bias=bguT[:, 8 + fc, ex:ex + 1], scale=1.0),
                                 reads=[tpb, t_bguT], writes=[t_tmpC[i2]])
                            P.op("dve", lambda e, i2=i2: e.tensor_scalar(out=tmpC[:, i2, :], in0=tmpC[:, i2, :], scalar1=LIM, scalar2=-LIM, op0=ALU.min, op1=ALU.max),
                                 reads=[t_tmpC[i2]], writes=[t_tmpC[i2]])
                            P.op("dve", lambda e, fc=fc, fcl=fcl, th=th, i2=i2: e.scalar_tensor_tensor(out=actT[:, fc, th * 512:(th + 1) * 512], in0=tmpC[:, i2, :], scalar=1.0, in1=gsb[:, fcl, th * 512:(th + 1) * 512],
                                                                                                  op0=ALU.add, op1=ALU.mult),
                                 reads=[t_gs[fcl * 2 + th], t_tmpC[i2]], writes=[t_actT[fc][th]])
                else:
                    dh = fb
                    for xi in range(8):
                        cond = 0 if xi < 4 else 1
                        pb, tpb = bank()
                        for fc in range(8):
                            P.op("pe", lambda e, fc=fc, xi=xi, pb=pb, wbuf=wbuf: e.matmul(pb[:, 0:512], lhsT=actT[:, fc, xi * 128:(xi + 1) * 128], rhs=wbuf[:, fc, :],
                                                                                         start=(fc == 0), stop=(fc == 7)), reads=[twb, t_actT[fc][xi // 4]], writes=[tpb])
                        i2 = xi % 2
                        P.op("dve", lambda e, xi=xi, ex=ex, pb=pb, i2=i2, cond=cond, dh=dh: e.scalar_tensor_tensor(out=tmpA[:, i2, :], in0=pb[:, 0:512], scalar=Gall[:, xi, ex:ex + 1],
                                                                                                         in1=gb[:, cond, dh * 512:(dh + 1) * 512], op0=ALU.mult, op1=ALU.mult),
                             reads=[tpb, t_G[xi], t_gb], writes=[t_tmpA[i2]])
                        P.op("dve", lambda e, xi=xi, i2=i2, dh=dh: e.tensor_tensor(out=X[:, xi, dh * 512:(dh + 1) * 512], in0=X[:, xi, dh * 512:(dh + 1) * 512], in1=tmpA[:, i2, :], op=ALU.add),
                             reads=[tX[xi], t_tmpA[i2]], writes=[tX[xi]])
            for xi in range(8):
                layer_norm_inplace(xi, t_lnw, t_lnb)

            if l < NL - 1:
                for i in range(4):
                    P.dma("pool", lambda e, i=i: e.dma_start(out=gins[i].ap(), in_=X[:, 4 + i, :]), reads=[tX[4 + i]], writes=[t_gin[i]])
                    P.dma("pool", lambda e, i=i: e.collective_compute("AllGather", ALU.bypass, replica_groups=[[0, 1, 2, 3], [4, 5, 6, 7]],
                                                                      ins=[gins[i].ap().opt()], outs=[gouts[i].ap().opt()]), reads=[t_gin[i]], writes=[t_gout[i]], inc=1)
            else:
                for i in range(4):
                    P.dma("sp", lambda e, i=i: e.dma_start(out=DO["yp"][i * 128:(i + 1) * 128, :], in_=X[:, i, :]), reads=[tX[i]], writes=[], key=okey["yp"])
                    P.dma("sp", lambda e, i=i: e.dma_start(out=DO["ys"][i * 128:(i + 1) * 128, :], in_=X[:, 4 + i, :]), reads=[tX[4 + i]], writes=[], key=okey["ys"])

        P.emit(st, final_waits=out_toks)
    return nc


def _grid_pos_embed(rows, d):
    quarter = d // 4
    omega = (1.0 / (10000.0 ** (np.arange(quarter, dtype=np.float32) / np.float32(quarter)))).astype(np.float32)
    r = np.repeat(np.arange(rows, dtype=np.float32), 64)[:, None] * omega
    cc = np.tile(np.arange(64, dtype=np.float32), rows)[:, None] * omega
    return np.concatenate([np.sin(r), np.cos(r), np.sin(cc), np.cos(cc)], axis=-1).astype(np.float32)


_CACHE = {}
_DBG = False
_CFG = {"NL": 4, "NEX": NE}


def kernel(x_prompt, x_sample, state_C, state_n, state_m, c, c_ctx, w_ada, b_ada, w_in, gate_bias, mlstm_norm_w,
           w_out, ln1_w, ln1_b, router_w, router_b, w_gate_up, b_gate_up, w_down, b_down, ln2_w, ln2_b):
    f32 = np.float32
    bf = ml_dtypes.bfloat16
    A = lambda a: np.ascontiguousarray(np.asarray(a), dtype=f32)
    x_prompt, x_sample = A(x_prompt), A(x_sample)
    NL, NEX = _CFG["NL"], _CFG["NEX"]
    key = (NL, NEX)
    if key not in _CACHE:
        _CACHE[key] = build_program(NL, NEX)
    nc = _CACHE[key]
    DEPTH = NL
    pos = _grid_pos_embed(2048 // 64, D)
    n = np.arange(2048, dtype=np.float64)
    ang = 2 * np.pi * ((n[:, None] * n[None, :]) % 2048) / 2048
    CN, SN = np.cos(ang), np.sin(ang)
    n2 = np.arange(256, dtype=np.float64)
    ang2 = 2 * np.pi * ((n2[:, None] * n2[None, :]) % 256) / 256
    n3 = np.arange(128, dtype=np.float64)
    ang3 = 2 * np.pi * ((n3[:, None] * n3[None, :]) % 128) / 128
    CC, SC = np.cos(ang3), np.sin(ang3)
    sp_, ss_ = 1.0 / np.sqrt(256 * 128.0), 1.0 / np.sqrt(2048 * 128.0)
    ar = np.arange(128)
    triU = (ar[:, None] <= ar[None, :]).astype(f32)
    triL = (ar[:, None] >= ar[None, :]).astype(f32)
    selr = np.zeros((2, 2, 128), f32)
    selr[0, 0] = 1
    selr[1, 1] = 1
    sel8 = np.zeros((8, 2), f32)
    sel8[0:4, 0] = 1
    sel8[4:8, 1] = 1
    shared = {
        "pos_full": pos,
        "w_ada": A(w_ada)[:NL], "b_ada2": np.ascontiguousarray(np.broadcast_to(A(b_ada)[:NL, None, :], (DEPTH, 2, 6 * D))),
        "w_in": A(w_in)[:NL], "gbias": np.ascontiguousarray(np.broadcast_to(A(gate_bias)[:NL, None, :], (DEPTH, 128, 16))),
        "nwT": np.ascontiguousarray(A(mlstm_norm_w)[:NL].reshape(DEPTH, 4, 128).transpose(0, 2, 1)),
        "w_out": A(w_out)[:NL],
        "ln1w": np.ascontiguousarray(np.broadcast_to(A(ln1_w)[:NL, None, :], (DEPTH, 128, D))),
        "ln1b": np.ascontiguousarray(np.broadcast_to(A(ln1_b)[:NL, None, :], (DEPTH, 128, D))),
        "router_w": A(router_w)[:NL], "rbb": np.ascontiguousarray(np.broadcast_to(A(router_b)[:NL, None, :], (DEPTH, 128, NE))),
        "w_gu": A(w_gate_up)[:NL, :max(NEX, 1)], "bguT": np.ascontiguousarray(A(b_gate_up)[:NL].reshape(DEPTH, NE, 16, 128).transpose(0, 3, 2, 1)),
        "w_down": A(w_down)[:NL, :max(NEX, 1)], "b_down": A(b_down)[:NL],
        "ln2w": np.ascontiguousarray(np.broadcast_to(A(ln2_w)[:NL, None, :], (DEPTH, 128, D))),
        "ln2b": np.ascontiguousarray(np.broadcast_to(A(ln2_b)[:NL, None, :], (DEPTH, 128, D))),
        "c256": np.cos(ang2).astype(bf), "s256": np.sin(ang2).astype(bf),
        "ccp": (CC * sp_).astype(bf), "nscp": (-SC * sp_).astype(bf), "ccs": (CC * ss_).astype(bf), "nscs": (-SC * ss_).astype(bf),
        "ident_b": np.eye(128, dtype=f32).astype(bf), "ident_f": np.eye(128, dtype=f32), "triU": triU, "triL": triL,
        "selr": selr, "sel8": sel8,
        "NMU4": np.ascontiguousarray(np.broadcast_to(((triU - 1.0) * 30000.0)[:, None, :], (128, 4, 128))).astype(f32),
        "NML4": np.ascontiguousarray(np.broadcast_to(((triL - 1.0) * 30000.0)[:, None, :], (128, 4, 128))).astype(f32),
    }
    state_C, state_n, state_m, c, c_ctx = A(state_C), A(state_n), A(state_m), A(c), A(c_ctx)
    in_maps = []
    for core in range(8):
        b, j = divmod(core, 4)
        cv = np.stack([c_ctx, c[b]], 0)
        cvT = np.ascontiguousarray(cv.reshape(2, 8, 128).transpose(2, 1, 0).reshape(128, 16))
        mf = np.zeros((128, 16), f32)
        mb = np.zeros((128, 16), f32)
        mf[:, :4 * j] = 1
        mb[:, 4 * j + 4:] = 1
        m = dict(shared)
        m.update({
            "xp": np.ascontiguousarray(x_prompt[2 * core:2 * core + 2].reshape(512, D)),
            "xs_own": np.ascontiguousarray(x_sample[b, 512 * j:512 * j + 512]),
            "pos_own": np.ascontiguousarray(pos[512 * j:512 * j + 512]),
            "xs_full": np.ascontiguousarray(x_sample[b]),
            "sC": np.ascontiguousarray(state_C[b][:NL].reshape(DEPTH, 8, 128, 128)),
            "sn": np.ascontiguousarray(state_n[b][:NL].reshape(DEPTH, 8, 128).transpose(0, 2, 1)),
            "smb": np.ascontiguousarray(np.broadcast_to(state_m[b][:NL].reshape(DEPTH, 1, 8), (DEPTH, 128, 8))),
            "cvT": cvT, "maskf": mf, "maskb": mb,
            "cn_own": np.ascontiguousarray(CN[:, 512 * j:512 * j + 512]).astype(bf),
            "sn_own": np.ascontiguousarray(SN[:, 512 * j:512 * j + 512]).astype(bf),
        })
        in_maps.append(m)
    res = run_bass_kernel_spmd(nc, in_maps, core_ids=list(range(8)))
    R = res.results
    y_prompt = np.concatenate([R[k]["yp"].reshape(2, 256, D) for k in range(8)], 0).astype(f32)
    y_sample = np.stack([np.concatenate([R[4 * b + j]["ys"] for j in range(4)], 0) for b in range(2)], 0).astype(f32)
    nC = np.concatenate([R[k]["nC"] for k in range(8)], 0).reshape(16, DEPTH, 2, 4, 128, 128).astype(f32)
    nn = np.concatenate([R[k]["nn"] for k in range(8)], 0).reshape(16, DEPTH, 2, 4, 128).astype(f32)
    nm = np.concatenate([R[k]["nm"] for k in range(8)], 0).reshape(16, DEPTH, 2, 4).astype(f32)
    if _DBG:
      _CACHE["dbg2"] = {k2: [np.asarray(R[k][k2]) for k in range(8)] for k2 in ["dbg_h1", "dbg_h2", "dbg_gt", "dbg_stx", "dbg_scr", "dbg_qk", "dbg_nd", "dbg_sm2"]}
      _CACHE["dbg"] = {"mix": [np.asarray(R[k]["dbg_mix"]) for k in range(8)], "x1": [np.asarray(R[k]["dbg_x1"]) for k in range(8)]}
    return (y_prompt, y_sample, nC, nn, nm)
```
